# Optimizing a Trainium2 kernel written in Bass

```python
import math
import jax, jax.numpy as jnp
from jax import lax
import numpy as np

D_MODEL = 1024
BATCH = 16
SEQ = 2048
DEPTH = 2

CHUNK = 64
Q_BLOCK = 128
ATT_HEADS = 8
ATT_HEAD_DIM = 64
ATT_WIDTH = ATT_HEADS * ATT_HEAD_DIM
IDX_HEADS = 8
IDX_DIM = 32
TOPK_MAX = 256
TOPK_FRACTION = 4
SSM_WIDTH = D_MODEL // 4
SSM_GROUP = 16
SSM_GROUPS = SSM_WIDTH // SSM_GROUP
SSM_STATE = 64
DT_MIN = 1e-3
DT_MAX = 1e-1
CONV_WIDTH = D_MODEL // 4
CONV_KSIZE = 31
MIX_WIDTH = ATT_WIDTH + SSM_WIDTH + CONV_WIDTH
D_FF = 2816
N_MOD = 9
NORM_EPS = 1e-6
IN_COLS = (ATT_WIDTH, ATT_HEAD_DIM, ATT_HEAD_DIM, IDX_HEADS * IDX_DIM, IDX_DIM, IDX_HEADS, SSM_WIDTH, 2 * CONV_WIDTH)
IN_WIDTH = sum(IN_COLS)
IN_SPLITS = tuple(int(s) for s in np.cumsum(IN_COLS)[:-1])

kernel_name = 'chunk_causal_hybrid_dsa_s5_conformer'


def rms_norm(x, g):
    xf = x.astype(jnp.float32)
    y = xf * lax.rsqrt(jnp.mean(xf * xf, axis=-1, keepdims=True) + NORM_EPS)
    return (y * g.astype(jnp.float32)).astype(x.dtype)


def layer_norm(x, g, b):
    xf = x.astype(jnp.float32)
    mu = jnp.mean(xf, axis=-1, keepdims=True)
    var = jnp.mean(jnp.square(xf - mu), axis=-1, keepdims=True)
    y = (xf - mu) * lax.rsqrt(var + NORM_EPS) * g.astype(jnp.float32) + b.astype(jnp.float32)
    return y.astype(x.dtype)


def modulate(x, shift, scale):
    return x * (1.0 + scale) + shift


def swiglu_ffn(h, w_gu, w_down):
    gate, up = jnp.split(h @ w_gu, 2, axis=-1)
    return (jax.nn.silu(gate) * up) @ w_down


def dsa_attention(q, k, v, qi, ki, wi):
    bsz, seq_len = q.shape[0], q.shape[1]
    topk = min(TOPK_MAX, seq_len // TOPK_FRACTION)
    n_blocks = seq_len // Q_BLOCK
    key_chunk = jnp.arange(seq_len) // CHUNK
    ki_f = ki.astype(jnp.float32)

    def to_blocks(t):
        return jnp.moveaxis(t.reshape((bsz, n_blocks, Q_BLOCK) + t.shape[2:]), 1, 0)

    def one_block(args):
        qb, qib, wib, blk = args
        t_chunk = (blk * Q_BLOCK + jnp.arange(Q_BLOCK)) // CHUNK
        admissible = key_chunk[None, :] <= t_chunk[:, None]
        s = jnp.einsum('bqhd,bsd->bqhs', qib.astype(jnp.float32), ki_f) * (IDX_DIM ** -0.5)
        w_h = wib.astype(jnp.float32) * (IDX_HEADS ** -0.5)
        idx_score = jnp.einsum('bqhs,bqh->bqs', jax.nn.relu(s), w_h)
        idx_score = jnp.where(admissible[None], idx_score, -jnp.inf)
        _, sel = lax.top_k(idx_score, topk)
        k_sel = jax.vmap(lambda kk, ii: kk[ii])(k, sel)
        v_sel = jax.vmap(lambda vv, ii: vv[ii])(v, sel)
        valid = key_chunk[sel] <= t_chunk[None, :, None]
        logits = jnp.einsum('bqhd,bqkd->bqhk', qb, k_sel).astype(jnp.float32) * (ATT_HEAD_DIM ** -0.5)
        logits = jnp.where(valid[:, :, None, :], logits, -jnp.inf)
        p = jax.nn.softmax(logits, axis=-1).astype(v.dtype)
        o = jnp.einsum('bqhk,bqkd->bqhd', p, v_sel)
        return o.reshape(bsz, Q_BLOCK, ATT_WIDTH)

    out = lax.map(one_block, (to_blocks(q), to_blocks(qi), to_blocks(wi), jnp.arange(n_blocks)))
    return jnp.moveaxis(out, 0, 1).reshape(bsz, seq_len, ATT_WIDTH)


def _cmul(ar, ai, br, bi):
    return ar * br - ai * bi, ar * bi + ai * br


def _ssm_combine(e1, e2):
    a1r, a1i, b1r, b1i = e1
    a2r, a2i, b2r, b2i = e2
    ar, ai = _cmul(a2r, a2i, a1r, a1i)
    br, bi = _cmul(a2r, a2i, b1r, b1i)
    return ar, ai, br + b2r, bi + b2i


def s5_mixer(u, a_re, a_im, log_dt, b_re, b_im, c_re, c_im, d_skip, w_glu):
    f32 = jnp.float32
    bsz, seq_len, _ = u.shape
    uf = u.astype(f32).reshape(bsz, seq_len, SSM_GROUPS, SSM_GROUP)
    dt = jnp.exp(log_dt.astype(f32))[:, None]
    ar, ai = a_re.astype(f32), a_im.astype(f32)
    mag = jnp.exp(dt * ar)
    abr, abi = mag * jnp.cos(dt * ai), mag * jnp.sin(dt * ai)
    den = ar * ar + ai * ai
    nr, ni = abr - 1.0, abi
    qr, qi = (nr * ar + ni * ai) / den, (ni * ar - nr * ai) / den
    bbr, bbi = _cmul(qr[..., None], qi[..., None], b_re.astype(f32), b_im.astype(f32))
    bu_re = jnp.einsum('blgc,gpc->blgp', uf, bbr)
    bu_im = jnp.einsum('blgc,gpc->blgp', uf, bbi)
    abr_l = jnp.broadcast_to(abr, (seq_len,) + abr.shape)
    abi_l = jnp.broadcast_to(abi, (seq_len,) + abi.shape)

    def scan_one(br, bi):
        _, _, xr, xi = lax.associative_scan(_ssm_combine, (abr_l, abi_l, br, bi), axis=0)
        return xr, xi

    xr, xi = jax.vmap(scan_one)(bu_re, bu_im)
    y = (jnp.einsum('blgp,gcp->blgc', xr, c_re.astype(f32))
         - jnp.einsum('blgp,gcp->blgc', xi, c_im.astype(f32))
         + d_skip.astype(f32) * uf)
    y = jax.nn.gelu(y).reshape(bsz, seq_len, SSM_WIDTH).astype(u.dtype)
    a, g = jnp.split(y @ w_glu, 2, axis=-1)
    return a * jax.nn.sigmoid(g)


def conformer_conv(h, w_dw, b_dw, ln_g, ln_b, w_pw):
    a, g = jnp.split(h, 2, axis=-1)
    u = a * jax.nn.sigmoid(g)
    y = lax.conv_general_dilated(u, w_dw[:, None, :], window_strides=(1,),
                                 padding=((CONV_KSIZE - 1, 0),),
                                 dimension_numbers=('NWC', 'WIO', 'NWC'),
                                 feature_group_count=CONV_WIDTH) + b_dw
    y = jax.nn.silu(layer_norm(y, ln_g, ln_b))
    return y @ w_pw


def hybrid_mixer(h, w_in, w_out, att_out_norm, ssm_out_norm, conv_out_norm,
                 a_re, a_im, log_dt, b_re, b_im, c_re, c_im, d_skip, w_glu,
                 w_dw, b_dw, ln_g, ln_b, w_pw):
    bsz, seq_len, _ = h.shape
    proj = h @ w_in
    q, k, v, qi, ki, wi, u_ssm, u_conv = jnp.split(proj, IN_SPLITS, axis=-1)
    y_att = dsa_attention(q.reshape(bsz, seq_len, ATT_HEADS, ATT_HEAD_DIM), k, v,
                          qi.reshape(bsz, seq_len, IDX_HEADS, IDX_DIM), ki, wi)
    y_ssm = s5_mixer(u_ssm, a_re, a_im, log_dt, b_re, b_im, c_re, c_im, d_skip, w_glu)
    y_conv = conformer_conv(u_conv, w_dw, b_dw, ln_g, ln_b, w_pw)
    merged = jnp.concatenate([rms_norm(y_att, att_out_norm),
                              rms_norm(y_ssm, ssm_out_norm),
                              rms_norm(y_conv, conv_out_norm)], axis=-1)
    return merged @ w_out


def setup_inputs(seed: int = 0) -> dict:
    key = jax.random.key(seed)
    ks = iter(jax.random.split(key, 40))
    f32 = jnp.float32

    def nrm(shape, scale):
        return jax.random.normal(next(ks), shape, f32) * scale

    def gain(shape):
        return 1.0 + nrm(shape, 0.02)

    n_idx = jnp.arange(SSM_STATE, dtype=f32)
    return {
        'x': nrm((BATCH, SEQ, D_MODEL), 1.0),
        'c': nrm((BATCH, D_MODEL), 1.0),
        'mod_w': nrm((DEPTH, D_MODEL, N_MOD * D_MODEL), 0.5 * D_MODEL ** -0.5),
        'mod_b': nrm((DEPTH, N_MOD * D_MODEL), 0.01),
        'ffn1_norm': gain((DEPTH, D_MODEL)),
        'ffn1_w_gu': nrm((DEPTH, D_MODEL, 2 * D_FF), D_MODEL ** -0.5),
        'ffn1_w_down': nrm((DEPTH, D_FF, D_MODEL), D_FF ** -0.5),
        'mix_norm': gain((DEPTH, D_MODEL)),
        'w_in': nrm((DEPTH, D_MODEL, IN_WIDTH), D_MODEL ** -0.5),
        'w_out': nrm((DEPTH, MIX_WIDTH, D_MODEL), MIX_WIDTH ** -0.5),
        'att_out_norm': gain((DEPTH, ATT_WIDTH)),
        'ssm_out_norm': gain((DEPTH, SSM_WIDTH)),
        'conv_out_norm': gain((DEPTH, CONV_WIDTH)),
        'ssm_a_re': -0.5 + nrm((DEPTH, SSM_GROUPS, SSM_STATE), 0.01),
        'ssm_a_im': jnp.pi * n_idx + nrm((DEPTH, SSM_GROUPS, SSM_STATE), 0.01),
        'ssm_log_dt': jax.random.uniform(next(ks), (DEPTH, SSM_GROUPS), f32,
                                         math.log(DT_MIN), math.log(DT_MAX)),
        'ssm_b_re': nrm((DEPTH, SSM_GROUPS, SSM_STATE, SSM_GROUP), (2 * SSM_GROUP) ** -0.5),
        'ssm_b_im': nrm((DEPTH, SSM_GROUPS, SSM_STATE, SSM_GROUP), (2 * SSM_GROUP) ** -0.5),
        'ssm_c_re': nrm((DEPTH, SSM_GROUPS, SSM_GROUP, SSM_STATE), (2 * SSM_STATE) ** -0.5),
        'ssm_c_im': nrm((DEPTH, SSM_GROUPS, SSM_GROUP, SSM_STATE), (2 * SSM_STATE) ** -0.5),
        'ssm_d': nrm((DEPTH, SSM_GROUPS, SSM_GROUP), 1.0),
        'ssm_w_glu': nrm((DEPTH, SSM_WIDTH, 2 * SSM_WIDTH), SSM_WIDTH ** -0.5),
        'conv_w_dw': nrm((DEPTH, CONV_KSIZE, CONV_WIDTH), CONV_KSIZE ** -0.5),
        'conv_b_dw': nrm((DEPTH, CONV_WIDTH), 0.01),
        'conv_ln_g': gain((DEPTH, CONV_WIDTH)),
        'conv_ln_b': nrm((DEPTH, CONV_WIDTH), 0.01),
        'conv_w_pw': nrm((DEPTH, CONV_WIDTH, CONV_WIDTH), CONV_WIDTH ** -0.5),
        'ffn2_norm': gain((DEPTH, D_MODEL)),
        'ffn2_w_gu': nrm((DEPTH, D_MODEL, 2 * D_FF), D_MODEL ** -0.5),
        'ffn2_w_down': nrm((DEPTH, D_FF, D_MODEL), D_FF ** -0.5),
        'final_norm': gain((D_MODEL,)),
    }


def reference(x, c, mod_w, mod_b, ffn1_norm, ffn1_w_gu, ffn1_w_down, mix_norm, w_in, w_out,
              att_out_norm, ssm_out_norm, conv_out_norm, ssm_a_re, ssm_a_im, ssm_log_dt,
              ssm_b_re, ssm_b_im, ssm_c_re, ssm_c_im, ssm_d, ssm_w_glu, conv_w_dw, conv_b_dw,
              conv_ln_g, conv_ln_b, conv_w_pw, ffn2_norm, ffn2_w_gu, ffn2_w_down, final_norm):
    cond = jax.nn.silu(c)[:, None, :]
    for l in range(DEPTH):
        mod = cond @ mod_w[l] + mod_b[l]
        sh1, sc1, g1, sh2, sc2, g2, sh3, sc3, g3 = jnp.split(mod, N_MOD, axis=-1)
        h = modulate(rms_norm(x, ffn1_norm[l]), sh1, sc1)
        x = x + 0.5 * g1 * swiglu_ffn(h, ffn1_w_gu[l], ffn1_w_down[l])
        h = modulate(rms_norm(x, mix_norm[l]), sh2, sc2)
        x = x + g2 * hybrid_mixer(h, w_in[l], w_out[l], att_out_norm[l], ssm_out_norm[l],
                                  conv_out_norm[l], ssm_a_re[l], ssm_a_im[l], ssm_log_dt[l],
                                  ssm_b_re[l], ssm_b_im[l], ssm_c_re[l], ssm_c_im[l], ssm_d[l],
                                  ssm_w_glu[l], conv_w_dw[l], conv_b_dw[l], conv_ln_g[l],
                                  conv_ln_b[l], conv_w_pw[l])
        h = modulate(rms_norm(x, ffn2_norm[l]), sh3, sc3)
        x = x + 0.5 * g3 * swiglu_ffn(h, ffn2_w_gu[l], ffn2_w_down[l])
    return rms_norm(x, final_norm)
```

```python
import numpy as np
from contextlib import ExitStack
import concourse.bass as bass
import concourse.mybir as mybir
from concourse.bass_utils import run_bass_kernel_spmd

F32 = mybir.dt.float32
BF16 = mybir.dt.bfloat16
ALU = mybir.AluOpType
AF = mybir.ActivationFunctionType
AX = mybir.AxisListType

ROT = 2048
DMA_SLOTS = 12
DMA_ROT = 120

D = 1024
SEQ = 2048
DFF = 2816
NF = 22
NWIN = 1864
C_Q, C_KK, C_KI, C_QI, C_A, C_G, C_VW, C_SSM = 0, 512, 640, 768, 1024, 1280, 1536, 1608
EPS = 1e-6


class Op:
    __slots__ = ("eng", "fn", "reads", "writes", "dma", "deps", "inc", "cnt",
                 "dslot", "dgen", "dval", "waits", "clock", "idx")


class Sched:
    def __init__(self, nc):
        self.nc = nc
        self.ops = []
        self.last_w = {}
        self.readers = {}
        self.dma_n = 0
        self.dma_prev = {}

    def add(self, eng, fn, reads=(), writes=(), dma=False):
        op = Op()
        op.eng, op.fn, op.dma = eng, fn, dma
        op.reads, op.writes = tuple(reads), tuple(writes)
        op.inc = False
        op.idx = len(self.ops)
        deps = {}
        for k in op.reads:
            w = self.last_w.get(k)
            if w is not None:
                deps[w.idx] = (w, True)
        for k in op.writes:
            w = self.last_w.get(k)
            if w is not None and w.idx not in deps:
                deps[w.idx] = (w, False)
            for r in self.readers.get(k, ()):
                if r.idx not in deps:
                    deps[r.idx] = (r, False)
        if dma:
            slot = self.dma_n % DMA_SLOTS
            self.dma_n += 1
            prev = self.dma_prev.get(slot)
            if prev is not None and prev.idx not in deps:
                deps[prev.idx] = (prev, True)
            self.dma_prev[slot] = op
            op.dslot = slot
            op.inc = True
        need = []
        for d, raw in deps.values():
            if d is op:
                continue
            if d.dma or op.dma or d.eng != eng:
                need.append(d)
            elif raw and eng != "pe":
                need.append(d)
        for d in need:
            d.inc = True
        op.deps = need
        for k in op.reads:
            self.readers.setdefault(k, []).append(op)
        for k in op.writes:
            self.last_w[k] = op
            self.readers[k] = []
        self.ops.append(op)
        return op

    def barrier(self):
        keys = set(self.last_w.keys()) | set(self.readers.keys())
        keys.discard("BAR")
        self.add("dve", lambda e: e.nop(), reads=list(keys), writes=["BAR"])
        for en in ("pe", "act", "pool", "sp"):
            self.add(en, lambda e: e.nop(), reads=["BAR"], writes=[("BARL", en)])
        bar = self.last_w["BAR"]
        self.last_w = {"BAR": bar}
        self.readers = {}
        self._bar = bar

    def finish(self, eng="sp"):
        keys = [k for k, w in self.last_w.items() if w.dma]
        self.add(eng, lambda e: e.nop(), reads=keys)

    def emit(self, stack):
        nc = self.nc
        engs = ["pe", "act", "dve", "pool", "sp"]
        cnt = {e: 0 for e in engs}
        dcount = {}
        for op in self.ops:
            if op.dma:
                g = dcount.get(op.dslot, 0)
                op.dgen, op.dval = g // DMA_ROT, 16 * (g % DMA_ROT + 1)
                dcount[op.dslot] = g + 1
            elif op.inc:
                cnt[op.eng] += 1
                op.cnt = cnt[op.eng]
        sems = {}

        def sem(name):
            if name not in sems:
                sems[name] = stack.enter_context(nc.semaphore(name))
            return sems[name]

        known = {e: {} for e in engs}
        for op in self.ops:
            kn = known[op.eng]
            waits = []
            for d in sorted(op.deps, key=lambda o: o.idx):
                if d.dma:
                    key = ("d", d.dslot, d.dgen)
                    val = d.dval
                else:
                    key = d.eng
                    val = d.cnt
                if kn.get(key, 0) >= val:
                    continue
                waits.append(d)
                for k2, v2 in d.clock.items():
                    if kn.get(k2, 0) < v2:
                        kn[k2] = v2
                kn[key] = val
            op.waits = waits
            if op.inc:
                op.clock = dict(kn)
                if op.dma:
                    op.clock[("d", op.dslot, op.dgen)] = op.dval
                else:
                    op.clock[op.eng] = op.cnt
            else:
                op.clock = None

        for op in self.ops:
            if op.inc:
                if op.dma:
                    sem(f"d{op.dslot}_{op.dgen}")
                else:
                    sem(f"{op.eng}_{(op.cnt - 1) // ROT}")

        def emit_engine(ename, e):
            for op in self.ops:
                if op.eng != ename:
                    continue
                for d in op.waits:
                    if d.dma:
                        e.wait_ge(sem(f"d{d.dslot}_{d.dgen}"), d.dval)
                    else:
                        c = d.cnt - 1
                        e.wait_ge(sem(f"{d.eng}_{c // ROT}"), c % ROT + 1)
                ins = op.fn(e)
                if op.inc:
                    if op.dma:
                        ins.then_inc(sem(f"d{op.dslot}_{op.dgen}"), 16)
                    else:
                        c = op.cnt - 1
                        ins.then_inc(sem(f"{op.eng}_{c // ROT}"), 1)

        block = stack.enter_context(nc.Block())

        @block.tensor
        def _(e):
            emit_engine("pe", e)

        @block.scalar
        def _(e):
            emit_engine("act", e)

        @block.vector
        def _(e):
            emit_engine("dve", e)

        @block.gpsimd
        def _(e):
            emit_engine("pool", e)

        @block.sync
        def _(e):
            emit_engine("sp", e)
        return len(sems)


class Mem:
    def __init__(self, pool, nbytes):
        self.pool, self.nbytes, self.off = pool, nbytes, 0

    def alloc(self, shape, dtype):
        isz = 4 if dtype == F32 else 2
        n = int(np.prod(shape))
        nb = (n * isz + 63) // 64 * 64
        assert self.off + nb <= self.nbytes, (self.off, nb, self.nbytes)
        v = self.pool[:, self.off // 2:(self.off + n * isz) // 2]
        self.off += nb
        if dtype == F32:
            v = v.bitcast(F32)
        if len(shape) > 1:
            names = [f"a{i}" for i in range(len(shape))]
            kw = {n: int(d) for n, d in zip(names[:-1], shape[:-1])}
            v = v.rearrange(f"p ({' '.join(names)}) -> p {' '.join(names)}", **kw)
        return v


def _fm(v, nch):
    return np.ascontiguousarray(v.reshape(nch, 128).T)


class Pack:
    def __init__(self):
        self.cols, self.off, self.n = [], {}, 0

    def add(self, name, arr):
        arr = np.asarray(arr, np.float32).reshape(128, -1)
        self.off[name] = (self.n, arr.shape[1])
        self.cols.append(arr)
        self.n += arr.shape[1]

    def array(self):
        return np.ascontiguousarray(np.concatenate(self.cols, axis=1))


def pack_small(inp, core):
    P = Pack()
    b0 = 2 * core
    P.add("cT", np.stack([_fm(inp["c"][b0 + s], 8) for s in range(2)], axis=-1))
    for l in range(2):
        P.add(f"modb{l}", _fm(inp["mod_b"][l], 72))
        for j, nm in enumerate(("ffn1_norm", "mix_norm", "ffn2_norm")):
            P.add(f"ng{l}{j}", _fm(inp[nm][l], 8))
    P.add("fin", _fm(inp["final_norm"], 8))
    for l in range(2):
        P.add(f"cw{l}", inp["conv_w_dw"][l].T.reshape(2, 128, 31).transpose(1, 0, 2))
        P.add(f"cb{l}", _fm(inp["conv_b_dw"][l], 2))
        P.add(f"clg{l}", _fm(inp["conv_ln_g"][l], 2))
        P.add(f"clb{l}", _fm(inp["conv_ln_b"][l], 2))
        P.add(f"cgn{l}", _fm(inp["conv_out_norm"][l], 2))
        P.add(f"sgn{l}", _fm(inp["ssm_out_norm"][l], 2))
        P.add(f"agn{l}", _fm(inp["att_out_norm"][l], 4))
        dup = lambda a: np.concatenate([a, a], axis=0)
        P.add(f"sare{l}", dup(inp["ssm_a_re"][l].T))
        P.add(f"saim{l}", dup(inp["ssm_a_im"][l].T))
        P.add(f"sldt{l}", np.broadcast_to(inp["ssm_log_dt"][l][None, :], (128, 16)))
        bre = inp["ssm_b_re"][l].transpose(1, 0, 2).reshape(64, 256)
        bim = inp["ssm_b_im"][l].transpose(1, 0, 2).reshape(64, 256)
        P.add(f"sbp{l}", np.concatenate([bre, bim], axis=0))
        P.add(f"sbq{l}", np.concatenate([bim, bre], axis=0))
        cre = inp["ssm_c_re"][l].transpose(2, 0, 1).reshape(64, 256)
        cim = inp["ssm_c_im"][l].transpose(2, 0, 1).reshape(64, 256)
        P.add(f"scp{l}", np.concatenate([cre, cim], axis=0))
        P.add(f"scq{l}", np.concatenate([cim, cre], axis=0))
        P.add(f"sdcol{l}", np.tile(inp["ssm_d"][l].T, (8, 1)))
    jall = np.concatenate([np.arange(-7, 9), np.arange(7, -1, -1), 8 * 2 ** np.arange(8)]).astype(np.float32)
    P.add("sjall", np.broadcast_to(jall[None, :], (128, 32)))
    sg = np.ones((128, 1), np.float32); sg[:64] = -1
    P.add("ssgnA", sg)
    P.add("ssgnC", -sg)
    sig = np.arange(128) // 16
    P.add("stm", (sig[None, :] >= sig[:, None]).astype(np.float32))
    sw = np.zeros((128, 128), np.float32); sw[np.arange(128), (np.arange(128) + 64) % 128] = 1
    P.add("sswap", sw)
    return P


def build(phases, first, last, poff, npar):
    nc = bass.Bass("TRN2", target_bir_lowering=False)
    x_d = nc.dram_tensor("x", [2, SEQ, D], F32, kind="ExternalInput").ap()
    par_d = nc.dram_tensor("par", [128, npar], F32, kind="ExternalInput").ap()
    modw_d = nc.dram_tensor("mod_w", [2, D, 9 * D], F32, kind="ExternalInput").ap()
    kinds = {k for _, k in phases}
    wgu_d = {j: nc.dram_tensor(f"wgu{j}", [2, NF, 128, 2048], F32, kind="ExternalInput").ap()
             for j in (1, 2) if f"ffn{j}" in kinds}
    wdn_d = {j: nc.dram_tensor(f"wdn{j}", [2, NF, 128, D], F32, kind="ExternalInput").ap()
             for j in (1, 2) if f"ffn{j}" in kinds}
    y_d = nc.dram_tensor("y", [2, SEQ, D], F32, kind="ExternalOutput").ap()
    mixl = sorted({l for l, k in phases if k == "mix"})
    win_d = {l: nc.dram_tensor(f"win{l}", [128, 8, NWIN], F32, kind="ExternalInput").ap() for l in mixl}
    wout_d = {l: nc.dram_tensor(f"wout{l}", [128, 8, D], F32, kind="ExternalInput").ap() for l in mixl}
    wpw_d = {l: nc.dram_tensor(f"wpw{l}", [128, 2, 256], F32, kind="ExternalInput").ap() for l in mixl}
    wglu_d = {l: nc.dram_tensor(f"wglu{l}", [128, 2, 512], F32, kind="ExternalInput").ap() for l in mixl}

    st = ExitStack()
    with st:
        S = Sched(nc)
        POOLB = 206 * 1024
        pool = st.enter_context(nc.sbuf_tensor("pool", [128, POOLB // 2], BF16))
        psum = st.enter_context(nc.psum_tensor("psum", [128, 8, 512], F32))
        M = Mem(pool, POOLB)

        def PS(b):
            return psum[:, b, :]

        XT = M.alloc([8, SEQ], F32)
        HT = M.alloc([8, SEQ], BF16)
        identf = M.alloc([128], F32)
        identb = M.alloc([128], BF16)
        onesb = M.alloc([128], BF16)
        PAR = M.alloc([npar], F32)
        MOD = M.alloc([2, 72, 2], F32)
        DER = M.alloc([2, 2, 3, 3, 8], F32)
        condb = M.alloc([8, 2], BF16)
        sq = [M.alloc([512], BF16) for _ in range(2)]
        rs = M.alloc([512], F32)
        rstd = M.alloc([512], F32)
        tmpf = [M.alloc([512], F32) for _ in range(2)]
        sgf = [M.alloc([512], F32) for _ in range(2)]
        big0 = M.off

        def par(name):
            o, n = poff[name]
            return PAR[:, o:o + n]

        S.add("sp", lambda e: e.dma_start(out=PAR, in_=par_d), writes=["PAR"], dma=True)
        S.add("pool", lambda e: e.memset(identf, 0.0), writes=["identf"])
        S.add("pool", lambda e: e.affine_select(out=identf, in_=identf, pattern=[[-1, 128]],
                                                compare_op=ALU.not_equal, fill=1.0, base=0,
                                                channel_multiplier=1), reads=["identf"], writes=["identf"])
        S.add("dve", lambda e: e.tensor_copy(out=identb, in_=identf), reads=["identf"], writes=["identb"])
        S.add("dve", lambda e: e.memset(onesb, 1.0), writes=["onesb"])

        S.add("act", lambda e: e.activation(out=condb, in_=par("cT").rearrange("p (a b) -> p a b", a=8),
                                            func=AF.Silu), reads=["PAR"], writes=["condb"])
        layers = sorted({l for l, _ in phases})
        mslab = [M.alloc([8, 1024], BF16) for _ in range(2)]
        for l in layers:
            for sl in range(9):
                buf = mslab[sl % 2]
                S.add("pool", lambda e, buf=buf, l=l, sl=sl: e.dma_start(
                    out=buf, in_=modw_d[l, :, sl * 1024:(sl + 1) * 1024].rearrange("(k p) n -> p k n", p=128)),
                    writes=[("mslab", sl % 2)], dma=True)
                for fc in range(8):
                    ch = sl * 8 + fc
                    for k in range(8):
                        S.add("pe", lambda e, buf=buf, fc=fc, k=k, ch=ch: e.matmul(
                            psum[:, 0, 2 * ch:2 * ch + 2], buf[:, k, fc * 128:(fc + 1) * 128], condb[:, k, :],
                            start=(k == 0), stop=(k == 7)),
                            reads=[("mslab", sl % 2), "condb"], writes=[("ps", 0)])
            for s in range(2):
                S.add("dve", lambda e, l=l, s=s: e.tensor_tensor(
                    out=MOD[:, l, :, s], in0=psum[:, 0, s:144:2], in1=par(f"modb{l}"), op=ALU.add),
                    reads=["PAR"], writes=[("ps", 0), "MOD"])
            for s in range(2):
                for j in range(3):
                    a_, sh_, g_ = DER[:, l, s, j, 0, :], DER[:, l, s, j, 1, :], DER[:, l, s, j, 2, :]
                    c0 = 3 * j * 8
                    S.add("dve", lambda e, a_=a_, l=l, s=s, j=j, c0=c0: e.scalar_tensor_tensor(
                        out=a_, in0=MOD[:, l, c0 + 8:c0 + 16, s], scalar=1.0, in1=par(f"ng{l}{j}"),
                        op0=ALU.add, op1=ALU.mult), reads=["MOD", "PAR"], writes=["DER"])
                    S.add("dve", lambda e, sh_=sh_, l=l, s=s, c0=c0: e.tensor_copy(
                        out=sh_, in_=MOD[:, l, c0:c0 + 8, s]), reads=["MOD"], writes=["DER"])
                    S.add("dve", lambda e, g_=g_, l=l, s=s, j=j, c0=c0: e.tensor_scalar(
                        out=g_, in0=MOD[:, l, c0 + 16:c0 + 24, s], scalar1=(1.0 if j == 1 else 0.5), scalar2=None,
                        op0=ALU.mult), reads=["MOD"], writes=["DER"])
        S.barrier()
        M.off = big0

        def normmod(l, s, j):
            for t in range(4):
                ts_ = slice(t * 512, (t + 1) * 512)
                for c in range(8):
                    q_ = sq[c % 2]
                    S.add("act", lambda e, q_=q_, c=c, ts_=ts_: e.activation(out=q_, in_=XT[:, c, ts_], func=AF.Square),
                          reads=[("XT", c, t)], writes=[("sq", c % 2)])
                    S.add("pe", lambda e, q_=q_, c=c: e.matmul(PS(6), onesb, q_, start=(c == 0), stop=(c == 7)),
                          reads=[("sq", c % 2), "onesb"], writes=[("ps", 6)])
                S.add("act", lambda e: e.activation(out=rs, in_=PS(6), func=AF.Sqrt, scale=1.0 / D, bias=EPS),
                      writes=[("ps", 6), "rs"])
                S.add("dve", lambda e: e.reciprocal(out=rstd, in_=rs), reads=["rs"], writes=["rstd"])
                for c in range(8):
                    tf = tmpf[c % 2]
                    S.add("dve", lambda e, tf=tf, c=c, ts_=ts_: e.scalar_tensor_tensor(
                        out=tf, in0=XT[:, c, ts_], scalar=DER[:, l, s, j, 0, c:c + 1], in1=rstd,
                        op0=ALU.mult, op1=ALU.mult), reads=[("XT", c, t), "rstd", "DER"], writes=[("tmpf", c % 2)])
                    S.add("act", lambda e, tf=tf, c=c, ts_=ts_: e.activation(
                        out=HT[:, c, ts_], in_=tf, func=AF.Identity, bias=DER[:, l, s, j, 1, c:c + 1], scale=1.0),
                        reads=[("tmpf", c % 2), "DER"], writes=[("HT", c, t)])

        def ffn(l, s, which):
            j = 0 if which == 1 else 2
            normmod(l, s, j)
            m0 = M.off
            act = M.alloc([11, SEQ], BF16)
            wgu = [M.alloc([8, 256], BF16) for _ in range(3)]
            wdn = M.alloc([11, D], BF16)

            def load_gu(f):
                S.add("pool", lambda e, f=f: e.dma_start(
                    out=wgu[f % 3], in_=wgu_d[which][l, f].rearrange("p (k n) -> p k n", k=8)),
                    writes=[("wgu", f % 3)], dma=True)

            for f in range(2):
                load_gu(f)
            pi = 0
            for grp in range(2):
                for fl in range(11):
                    f = grp * 11 + fl
                    if f + 2 < NF:
                        load_gu(f + 2)
                    S.add("pool", lambda e, f=f, fl=fl: e.dma_start(out=wdn[:, fl, :], in_=wdn_d[which][l, f]),
                          writes=[("wdn", fl)], dma=True)
                    w = wgu[f % 3]
                    for t in range(4):
                        ts_ = slice(t * 512, (t + 1) * 512)
                        bg, bu = 2 * (pi % 2), 2 * (pi % 2) + 1
                        pi += 1
                        for gu, bk in ((0, bg), (1, bu)):
                            for k in range(8):
                                S.add("pe", lambda e, w=w, gu=gu, bk=bk, k=k, ts_=ts_: e.matmul(
                                    PS(bk), w[:, k, gu * 128:(gu + 1) * 128], HT[:, k, ts_],
                                    start=(k == 0), stop=(k == 7)),
                                    reads=[("wgu", f % 3), ("HT", k, t)], writes=[("ps", bk)])
                        sg = sgf[pi % 2]
                        S.add("act", lambda e, sg=sg, bg=bg: e.activation(out=sg, in_=PS(bg), func=AF.Silu),
                              writes=[("ps", bg), ("sgf", pi % 2)])
                        S.add("dve", lambda e, sg=sg, bu=bu, fl=fl, ts_=ts_: e.tensor_tensor(
                            out=act[:, fl, ts_], in0=sg, in1=PS(bu), op=ALU.mult),
                            reads=[("sgf", pi % 2)], writes=[("ps", bu), ("act", fl, t)])
                di = 0
                for dc in range(8):
                    for t in range(4):
                        ts_ = slice(t * 512, (t + 1) * 512)
                        bk = 4 + di % 2
                        di += 1
                        for fl in range(11):
                            S.add("pe", lambda e, fl=fl, dc=dc, bk=bk, ts_=ts_: e.matmul(
                                PS(bk), wdn[:, fl, dc * 128:(dc + 1) * 128], act[:, fl, ts_],
                                start=(fl == 0), stop=(fl == 10)),
                                reads=[("wdn", fl), ("act", fl, t)], writes=[("ps", bk)])
                        S.add("dve", lambda e, dc=dc, bk=bk, ts_=ts_: e.scalar_tensor_tensor(
                            out=XT[:, dc, ts_], in0=PS(bk), scalar=DER[:, l, s, j, 2, dc:dc + 1], in1=XT[:, dc, ts_],
                            op0=ALU.mult, op1=ALU.add), reads=["DER"], writes=[("ps", bk), ("XT", dc, t)])
            S.barrier()
            M.off = m0

        def load_x(s):
            m0 = M.off
            stg = [M.alloc([D], F32) for _ in range(2)]
            for i in range(16):
                sg_ = stg[i % 2]
                S.add("sp", lambda e, sg_=sg_, i=i: e.dma_start(out=sg_, in_=x_d[s, i * 128:(i + 1) * 128, :]),
                      writes=[("stg", i % 2)], dma=True)
                for h in range(2):
                    bk = 6 + (2 * i + h) % 2
                    for cc in range(4):
                        c = 4 * h + cc
                        S.add("pe", lambda e, sg_=sg_, c=c, cc=cc, bk=bk: e.transpose(
                            psum[:, bk, cc * 128:(cc + 1) * 128], sg_[:, c * 128:(c + 1) * 128], identf),
                            reads=[("stg", i % 2), "identf"], writes=[("ps", bk)])
                    eng = "act" if h == 0 else "dve"
                    dst = XT[:, 4 * h:4 * h + 4, i * 128:(i + 1) * 128]
                    src = psum[:, bk, :].rearrange("p (a b) -> p a b", a=4)
                    if eng == "act":
                        S.add("act", lambda e, dst=dst, src=src: e.copy(out=dst, in_=src),
                              writes=[("ps", bk)] + [("XT", 4 * h + cc, i // 4) for cc in range(4)])
                    else:
                        S.add("dve", lambda e, dst=dst, src=src: e.tensor_copy(out=dst, in_=src),
                              writes=[("ps", bk)] + [("XT", 4 * h + cc, i // 4) for cc in range(4)])
            S.barrier()
            M.off = m0

        def store_x(s, final):
            m0 = M.off
            stg = [M.alloc([D], F32) for _ in range(2)]
            xn = [M.alloc([8, 128], F32) for _ in range(2)]
            for t in range(4):
                ts_ = slice(t * 512, (t + 1) * 512)
                if final:
                    for c in range(8):
                        q_ = sq[c % 2]
                        S.add("act", lambda e, q_=q_, c=c, ts_=ts_: e.activation(out=q_, in_=XT[:, c, ts_], func=AF.Square),
                              reads=[("XT", c, t)], writes=[("sq", c % 2)])
                        S.add("pe", lambda e, q_=q_, c=c: e.matmul(PS(5), onesb, q_, start=(c == 0), stop=(c == 7)),
                              reads=[("sq", c % 2), "onesb"], writes=[("ps", 5)])
                    S.add("act", lambda e: e.activation(out=rs, in_=PS(5), func=AF.Sqrt, scale=1.0 / D, bias=EPS),
                          writes=[("ps", 5), "rs"])
                    S.add("dve", lambda e: e.reciprocal(out=rstd, in_=rs), reads=["rs"], writes=["rstd"])
                for ii in range(4):
                    i = 4 * t + ii
                    isl = slice(i * 128, (i + 1) * 128)
                    xb_ = xn[i % 2]
                    if final:
                        for c in range(8):
                            S.add("dve", lambda e, xb_=xb_, c=c, isl=isl, ii=ii: e.scalar_tensor_tensor(
                                out=xb_[:, c, :], in0=XT[:, c, isl], scalar=par("fin")[:, c:c + 1],
                                in1=rstd[:, ii * 128:(ii + 1) * 128], op0=ALU.mult, op1=ALU.mult),
                                reads=[("XT", c, t), "rstd", "PAR"], writes=[("xn", i % 2)])
                    sg_ = stg[i % 2]
                    for h in range(2):
                        bk = 6 + (2 * i + h) % 2
                        for cc in range(4):
                            c = 4 * h + cc
                            src = xb_[:, c, :] if final else XT[:, c, isl]
                            S.add("pe", lambda e, src=src, cc=cc, bk=bk: e.transpose(
                                psum[:, bk, cc * 128:(cc + 1) * 128], src, identf),
                                reads=[("xn", i % 2), ("XT", c, t), "identf"], writes=[("ps", bk)])
                        dst = sg_[:, h * 512:(h + 1) * 512]
                        if h == 0:
                            S.add("act", lambda e, dst=dst, bk=bk: e.copy(out=dst, in_=PS(bk)),
                                  writes=[("ps", bk), ("stg", i % 2, h)])
                        else:
                            S.add("dve", lambda e, dst=dst, bk=bk: e.tensor_copy(out=dst, in_=PS(bk)),
                                  writes=[("ps", bk), ("stg", i % 2, h)])
                    S.add("sp", lambda e, sg_=sg_, isl=isl: e.dma_start(out=y_d[s, isl, :], in_=sg_),
                          reads=[("stg", i % 2, 0), ("stg", i % 2, 1)], writes=[("y", s, i)], dma=True)
            S.barrier()
            M.off = m0


        NIT = 16

        def gnorm_tile(src, skey, nch, gn, dst, dkey, bank):
            for c in range(nch):
                q_ = sq[c % 2]
                S.add("act", lambda e, q_=q_, c=c: e.activation(out=q_, in_=src[:, c, :], func=AF.Square),
                      reads=[skey], writes=[("sq", c % 2)])
                S.add("pe", lambda e, q_=q_, c=c: e.matmul(PS(bank), onesb, q_, start=(c == 0), stop=(c == nch - 1)),
                      reads=[("sq", c % 2), "onesb"], writes=[("ps", bank)])
            S.add("act", lambda e: e.activation(out=rs, in_=PS(bank), func=AF.Sqrt, scale=1.0 / (128 * nch), bias=EPS),
                  writes=[("ps", bank), "rs"])
            S.add("dve", lambda e: e.reciprocal(out=rstd, in_=rs), reads=["rs"], writes=["rstd"])
            for c in range(nch):
                S.add("dve", lambda e, c=c: e.scalar_tensor_tensor(
                    out=dst[:, c, :], in0=src[:, c, :], scalar=gn[:, c:c + 1], in1=rstd, op0=ALU.mult, op1=ALU.mult),
                    reads=[skey, "rstd", "PAR"], writes=[dkey])

        def wout_part(l, s, mc, mkeyf, nch, wo, wokey):
            di = 0
            for dc in range(8):
                for t in range(4):
                    ts_ = slice(t * 512, (t + 1) * 512)
                    bk = 4 + di % 2
                    di += 1
                    for kc in range(nch):
                        S.add("pe", lambda e, kc=kc, dc=dc, bk=bk, ts_=ts_: e.matmul(
                            PS(bk), wo[:, kc, dc * 128:(dc + 1) * 128], mc[:, kc, ts_],
                            start=(kc == 0), stop=(kc == nch - 1)), reads=[wokey, mkeyf(t)], writes=[("ps", bk)])
                    S.add("dve", lambda e, dc=dc, bk=bk, ts_=ts_: e.scalar_tensor_tensor(
                        out=XT[:, dc, ts_], in0=PS(bk), scalar=DER[:, l, s, 1, 2, dc:dc + 1], in1=XT[:, dc, ts_],
                        op0=ALU.mult, op1=ALU.add), reads=["DER"], writes=[("ps", bk), ("XT", dc, t)])

        def conv_part(l, s):
            m0 = M.off
            wab = M.alloc([8, 512], BF16)
            ucv = M.alloc([2, 30 + SEQ], F32)
            yacc = M.alloc([2, SEQ], F32)
            zs = M.alloc([2, SEQ], BF16)
            wpw = M.alloc([2, 256], BF16)
            wo = M.alloc([2, D], BF16)
            mc = M.alloc([2, SEQ], BF16)
            ycf = M.alloc([2, 512], F32)
            mt = M.alloc([512], F32)
            msq = M.alloc([512], F32)
            d1 = [M.alloc([512], F32) for _ in range(2)]
            ybf = [M.alloc([512], BF16) for _ in range(2)]
            cw = par(f"cw{l}").rearrange("p (a b) -> p a b", a=2)
            S.add("pool", lambda e: e.dma_start(out=wab, in_=win_d[l][:, :, C_A:C_A + 512]), writes=["wab"], dma=True)
            S.add("pool", lambda e: e.dma_start(out=wpw, in_=wpw_d[l]), writes=["wpw"], dma=True)
            S.add("pool", lambda e: e.dma_start(out=wo, in_=wout_d[l][:, 6:8, :]), writes=["wo"], dma=True)
            S.add("dve", lambda e: e.memset(ucv[:, :, 0:30], 0.0), writes=["ucvpad"])
            pi = 0
            for cc in range(2):
                for t in range(4):
                    ts_ = slice(t * 512, (t + 1) * 512)
                    ba, bg = 2 * (pi % 2), 2 * (pi % 2) + 1
                    pi += 1
                    for (c0, bk) in ((cc * 128, ba), (256 + cc * 128, bg)):
                        for k in range(8):
                            S.add("pe", lambda e, c0=c0, bk=bk, k=k, ts_=ts_: e.matmul(
                                PS(bk), wab[:, k, c0:c0 + 128], HT[:, k, ts_], start=(k == 0), stop=(k == 7)),
                                reads=["wab", ("HT", k, t)], writes=[("ps", bk)])
                    sg = sgf[pi % 2]
                    S.add("act", lambda e, sg=sg, bg=bg: e.activation(out=sg, in_=PS(bg), func=AF.Sigmoid),
                          writes=[("ps", bg), ("sgf", pi % 2)])
                    S.add("dve", lambda e, sg=sg, ba=ba, cc=cc, t=t: e.tensor_tensor(
                        out=ucv[:, cc, 30 + t * 512:30 + (t + 1) * 512], in0=sg, in1=PS(ba), op=ALU.mult),
                        reads=[("sgf", pi % 2)], writes=[("ps", ba), ("ucv", cc)])
            for cc in range(2):
                S.add("dve", lambda e, cc=cc: e.tensor_scalar(
                    out=yacc[:, cc, :], in0=ucv[:, cc, 0:SEQ], scalar1=cw[:, cc, 0:1], scalar2=par(f"cb{l}")[:, cc:cc + 1],
                    op0=ALU.mult, op1=ALU.add), reads=[("ucv", cc), "ucvpad", "PAR"], writes=[("yacc", cc)])
                for j in range(1, 31):
                    S.add("dve", lambda e, cc=cc, j=j: e.scalar_tensor_tensor(
                        out=yacc[:, cc, :], in0=ucv[:, cc, j:j + SEQ], scalar=cw[:, cc, j:j + 1], in1=yacc[:, cc, :],
                        op0=ALU.mult, op1=ALU.add), reads=[("ucv", cc), "ucvpad", "PAR", ("yacc", cc)], writes=[("yacc", cc)])
            for t in range(4):
                ts_ = slice(t * 512, (t + 1) * 512)
                for cc in range(2):
                    S.add("act", lambda e, cc=cc, ts_=ts_: e.copy(out=ybf[cc], in_=yacc[:, cc, ts_]),
                          reads=[("yacc", cc)], writes=[("ybf", cc)])
                    S.add("pe", lambda e, cc=cc: e.matmul(PS(0), onesb, ybf[cc], start=(cc == 0), stop=(cc == 1)),
                          reads=[("ybf", cc), "onesb"], writes=[("ps", 0)])
                for cc in range(2):
                    S.add("act", lambda e, cc=cc, ts_=ts_: e.activation(out=sq[cc], in_=yacc[:, cc, ts_], func=AF.Square),
                          reads=[("yacc", cc)], writes=[("sq", cc)])
                    S.add("pe", lambda e, cc=cc: e.matmul(PS(1), onesb, sq[cc], start=(cc == 0), stop=(cc == 1)),
                          reads=[("sq", cc), "onesb"], writes=[("ps", 1)])
                S.add("dve", lambda e: e.tensor_scalar(out=mt, in0=PS(0), scalar1=1.0 / 256, scalar2=None, op0=ALU.mult),
                      writes=[("ps", 0), "mt"])
                S.add("dve", lambda e: e.tensor_tensor(out=msq, in0=mt, in1=mt, op=ALU.mult), reads=["mt"], writes=["msq"])
                S.add("dve", lambda e: e.scalar_tensor_tensor(out=msq, in0=PS(1), scalar=1.0 / 256, in1=msq,
                                                              op0=ALU.mult, op1=ALU.subtract),
                      reads=["msq"], writes=[("ps", 1), "msq"])
                S.add("act", lambda e: e.activation(out=rs, in_=msq, func=AF.Sqrt, scale=1.0, bias=EPS),
                      reads=["msq"], writes=["rs"])
                S.add("dve", lambda e: e.reciprocal(out=rstd, in_=rs), reads=["rs"], writes=["rstd"])
                for cc in range(2):
                    S.add("dve", lambda e, cc=cc, ts_=ts_: e.tensor_tensor(out=d1[cc], in0=yacc[:, cc, ts_], in1=mt, op=ALU.subtract),
                          reads=[("yacc", cc), "mt"], writes=[("d1", cc)])
                    S.add("dve", lambda e, cc=cc: e.tensor_tensor(out=d1[cc], in0=d1[cc], in1=rstd, op=ALU.mult),
                          reads=[("d1", cc), "rstd"], writes=[("d1", cc)])
                    S.add("act", lambda e, cc=cc, ts_=ts_: e.activation(
                        out=zs[:, cc, ts_], in_=d1[cc], func=AF.Silu, scale=par(f"clg{l}")[:, cc:cc + 1],
                        bias=par(f"clb{l}")[:, cc:cc + 1]), reads=[("d1", cc), "PAR"], writes=[("zs", t)])
            for t in range(4):
                ts_ = slice(t * 512, (t + 1) * 512)
                for oc in range(2):
                    for kc in range(2):
                        S.add("pe", lambda e, oc=oc, kc=kc, ts_=ts_: e.matmul(
                            PS(2 + oc), wpw[:, kc, oc * 128:(oc + 1) * 128], zs[:, kc, ts_], start=(kc == 0), stop=(kc == 1)),
                            reads=["wpw", ("zs", t)], writes=[("ps", 2 + oc)])
                    S.add("act", lambda e, oc=oc: e.copy(out=ycf[:, oc, :], in_=PS(2 + oc)), writes=[("ps", 2 + oc), "ycf"])
                gnorm_tile(ycf, "ycf", 2, par(f"cgn{l}"), mc[:, :, ts_], ("mc", t), 3)
            wout_part(l, s, mc, lambda t: ("mc", t), 2, wo, "wo")
            S.barrier()
            M.off = m0

        def att_part(l, s):
            m0 = M.off
            qT = M.alloc([4, SEQ], BF16)
            kkT = M.alloc([SEQ], BF16)
            kiT = M.alloc([SEQ], BF16)
            qiT = M.alloc([2, SEQ], BF16)
            V1 = M.alloc([16, 66], BF16)
            WI = M.alloc([16, 8], F32)
            WA = M.alloc([16, 8], F32)
            WS = M.alloc([16, 8], F32)
            wo = M.alloc([4, D], BF16)
            P2 = M.alloc([NIT], F32)
            thr0 = M.alloc([1], F32)
            m1 = M.off
            wr = [M.alloc([8, 256], BF16) for _ in range(2)]
            S.add("pool", lambda e: e.dma_start(out=wo, in_=wout_d[l][:, 0:4, :]), writes=["wo"], dma=True)
            for i in range(NIT):
                S.add("pool", lambda e, i=i: e.memset(P2[:, i:i + 1], 2.0 ** -(i + 1)), writes=["P2"])
            S.add("pool", lambda e: e.memset(thr0, -1e29), writes=["thr0"])
            S.add("pool", lambda e: e.memset(V1[:, :, 64:65], 1.0), writes=["V1one"])
            groups = [(C_Q, 256, [("q", 0), ("q", 1)]), (C_Q + 256, 256, [("q", 2), ("q", 3)]),
                      (C_KK, 256, [("kk", 0), ("ki", 0)]), (C_QI, 256, [("qi", 0), ("qi", 1)]), (C_VW, 72, None)]
            pi = 0
            for gi, (c0, n, dests) in enumerate(groups):
                w = wr[gi % 2]
                S.add("pool", lambda e, w=w, c0=c0, n=n: e.dma_start(out=w[:, :, 0:n], in_=win_d[l][:, :, c0:c0 + n]),
                      writes=[("wr", gi % 2)], dma=True)
                if dests is not None:
                    for ci, (kind, idx) in enumerate(dests):
                        for t in range(4):
                            ts_ = slice(t * 512, (t + 1) * 512)
                            bk = pi % 4
                            pi += 1
                            for k in range(8):
                                S.add("pe", lambda e, w=w, ci=ci, bk=bk, k=k, ts_=ts_: e.matmul(
                                    PS(bk), w[:, k, ci * 128:(ci + 1) * 128], HT[:, k, ts_], start=(k == 0), stop=(k == 7)),
                                    reads=[("wr", gi % 2), ("HT", k, t)], writes=[("ps", bk)])
                            dst = {"q": lambda: qT[:, idx, ts_], "kk": lambda: kkT[:, ts_], "ki": lambda: kiT[:, ts_],
                                   "qi": lambda: qiT[:, idx, ts_]}[kind]()
                            sc_ = 0.125 if kind == "q" else 1.0
                            if pi % 2 == 0:
                                S.add("act", lambda e, dst=dst, bk=bk, sc_=sc_: e.activation(
                                    out=dst, in_=PS(bk), func=AF.Copy, scale=sc_), writes=[("ps", bk), (kind, idx, t)])
                            else:
                                S.add("dve", lambda e, dst=dst, bk=bk, sc_=sc_: e.tensor_scalar(
                                    out=dst, in0=PS(bk), scalar1=sc_, scalar2=None, op0=ALU.mult),
                                    writes=[("ps", bk), (kind, idx, t)])
                else:
                    for i in range(16):
                        bk = pi % 4
                        pi += 1
                        for k in range(8):
                            S.add("pe", lambda e, w=w, bk=bk, k=k, i=i: e.matmul(
                                psum[:, bk, 0:72], HT[:, k, i * 128:(i + 1) * 128], w[:, k, 0:72], start=(k == 0), stop=(k == 7)),
                                reads=[("wr", gi % 2), ("HT", k, i // 4)], writes=[("ps", bk)])
                        S.add("dve", lambda e, bk=bk, i=i: e.tensor_copy(out=V1[:, i, 0:64], in_=psum[:, bk, 0:64]),
                              writes=[("ps", bk), "V1"])
                        S.add("act", lambda e, bk=bk, i=i: e.copy(out=WI[:, i, :], in_=psum[:, bk, 64:72]),
                              writes=[("ps", bk), "WI"])
            S.add("act", lambda e: e.activation(out=WA, in_=WI, func=AF.Abs), reads=["WI"], writes=["WA"])
            S.add("dve", lambda e: e.tensor_scalar(out=WS, in0=WI, scalar1=0.0, scalar2=2.0, op0=ALU.is_ge, op1=ALU.mult),
                  reads=["WI"], writes=["WS"])
            S.add("dve", lambda e: e.tensor_scalar(out=WS, in0=WS, scalar1=-1.0, scalar2=None, op0=ALU.add),
                  reads=["WS"], writes=["WS"])
            S.barrier()
            M.off = m1
            sc = M.alloc([SEQ], F32)
            rt = [M.alloc([512], BF16) for _ in range(2)]
            mask = M.alloc([SEQ], BF16)
            maskT = M.alloc([16, 128], BF16)
            ex = [M.alloc([4, 128], BF16) for _ in range(4)]
            pT = [M.alloc([4, 128], BF16) for _ in range(4)]
            otok = M.alloc([8, 65], F32)
            on = M.alloc([8, 64], F32)
            onsq = M.alloc([512], F32)
            onb = M.alloc([512], BF16)
            attT = M.alloc([4, 128], BF16)
            sm = M.alloc([32], F32)
            lo, mid, cnt, ge, mx, mn, w0, ssq, rq = [sm[:, i:i + 1] for i in range(9)]
            Wd = sm[:, 12:12 + NIT]
            rden = M.alloc([8], F32)
            gatt = par(f"agn{l}")
            pb = [psum[:, bkk, :].bitcast(BF16) for bkk in range(8)]
            ri = 0
            for b in range(16):
                S_ = 128 * (b + 1)
                qs = slice(128 * b, 128 * b + 128)
                nseg = (S_ + 511) // 512
                tq = b // 4
                for h in range(8):
                    pr = slice(32 * (h % 4), 32 * (h % 4) + 32)
                    for sg_i in range(nseg):
                        c0, c1 = sg_i * 512, min(S_, sg_i * 512 + 512)
                        n = c1 - c0
                        bk = ri % 2
                        r_ = rt[ri % 2]
                        ri += 1
                        S.add("pe", lambda e, pr=pr, h=h, bk=bk, c0=c0, c1=c1, n=n, qs=qs: e.matmul(
                            psum[:, bk, 0:n], qiT[pr, h // 4, qs], kiT[pr, c0:c1], start=True, stop=True,
                            tile_position=(32 * (h % 4), 0)),
                            reads=[("qi", h // 4, tq), ("ki", 0, sg_i)], writes=[("ps", bk)])
                        S.add("act", lambda e, r_=r_, bk=bk, n=n, b=b, h=h: e.activation(
                            out=r_[:, 0:n], in_=psum[:, bk, 0:n], func=AF.Relu, scale=WA[:, b, h:h + 1]),
                            reads=["WA"], writes=[("ps", bk), ("rt", (ri - 1) % 2)])
                        if h == 0:
                            S.add("dve", lambda e, r_=r_, n=n, c0=c0, c1=c1, b=b: e.tensor_scalar(
                                out=sc[:, c0:c1], in0=r_[:, 0:n], scalar1=WS[:, b, 0:1], scalar2=None, op0=ALU.mult),
                                reads=[("rt", (ri - 1) % 2), "WS"], writes=[("sc", sg_i)])
                        else:
                            S.add("dve", lambda e, r_=r_, n=n, c0=c0, c1=c1, b=b, h=h: e.scalar_tensor_tensor(
                                out=sc[:, c0:c1], in0=r_[:, 0:n], scalar=WS[:, b, h:h + 1], in1=sc[:, c0:c1],
                                op0=ALU.mult, op1=ALU.add), reads=[("rt", (ri - 1) % 2), "WS", ("sc", sg_i)],
                                writes=[("sc", sg_i)])
                sck = [("sc", i) for i in range(nseg)]
                if b >= 2:
                    S.add("dve", lambda e, S_=S_: e.tensor_reduce(out=mx, in_=sc[:, 0:S_], axis=AX.X, op=ALU.max),
                          reads=sck, writes=["mx"])
                    S.add("dve", lambda e, S_=S_: e.tensor_reduce(out=mn, in_=sc[:, 0:S_], axis=AX.X, op=ALU.min),
                          reads=sck, writes=["mn"])
                S.add("dve", lambda e, S_=S_: e.memset(sc[0:64, S_ - 64:S_], -1e30), reads=sck, writes=sck)
                if b >= 2:
                    S.add("dve", lambda e: e.tensor_copy(out=lo, in_=mn), reads=["mn"], writes=["lo"])
                    S.add("dve", lambda e: e.tensor_tensor(out=w0, in0=mx, in1=mn, op=ALU.subtract),
                          reads=["mx", "mn"], writes=["w0"])
                    S.add("dve", lambda e: e.tensor_scalar(out=Wd, in0=P2, scalar1=w0, scalar2=None, op0=ALU.mult),
                          reads=["w0", "P2"], writes=["Wd"])
                    for i in range(NIT):
                        S.add("dve", lambda e, i=i: e.tensor_tensor(out=mid, in0=lo, in1=Wd[:, i:i + 1], op=ALU.add),
                              reads=["lo", "Wd"], writes=["mid"])
                        S.add("dve", lambda e, S_=S_: e.tensor_scalar(
                            out=mask[:, 0:S_], in0=sc[:, 0:S_], scalar1=mid, scalar2=None, op0=ALU.is_ge, op1=ALU.add,
                            accum_out=cnt), reads=sck + ["mid"], writes=["mask", "cnt"])
                        S.add("dve", lambda e: e.tensor_scalar(out=ge, in0=cnt, scalar1=255.5, scalar2=None, op0=ALU.is_ge),
                              reads=["cnt"], writes=["ge"])
                        S.add("dve", lambda e, i=i: e.scalar_tensor_tensor(
                            out=lo, in0=ge, scalar=Wd[:, i:i + 1], in1=lo, op0=ALU.mult, op1=ALU.add),
                            reads=["ge", "Wd", "lo"], writes=["lo"])
                    thr = lo
                else:
                    thr = thr0
                S.add("dve", lambda e, S_=S_, thr=thr: e.tensor_scalar(
                    out=mask[:, 0:S_], in0=sc[:, 0:S_], scalar1=thr, scalar2=None, op0=ALU.is_ge),
                    reads=sck + ["lo", "thr0"], writes=["mask"])
                for ch in range(b + 1):
                    bk = 4 + ch // 8
                    S.add("pe", lambda e, ch=ch, bk=bk: e.transpose(
                        pb[bk][:, (ch % 8) * 128:(ch % 8 + 1) * 128], mask[:, ch * 128:(ch + 1) * 128], identb),
                        reads=["mask", "identb"], writes=[("ps", bk)])
                n0 = min(b + 1, 8)
                S.add("act", lambda e, n0=n0: e.copy(out=maskT[:, 0:n0, :], in_=pb[4][:, 0:n0 * 128].rearrange("p (a b) -> p a b", a=n0)),
                      writes=[("ps", 4), "maskT"])
                if b + 1 > 8:
                    n1 = b + 1 - 8
                    S.add("dve", lambda e, n1=n1: e.tensor_copy(out=maskT[:, 8:8 + n1, :], in_=pb[5][:, 0:n1 * 128].rearrange("p (a b) -> p a b", a=n1)),
                          writes=[("ps", 5), "maskT"])
                for ch in range(b + 1):
                    st_ = ch % 2
                    ks = slice(ch * 128, (ch + 1) * 128)
                    for h in range(8):
                        bk = 2 + 2 * st_ + h % 2
                        hp = slice(64 * (h % 2), 64 * (h % 2) + 64)
                        S.add("pe", lambda e, h=h, bk=bk, hp=hp, ks=ks, qs=qs: e.matmul(
                            psum[:, bk, (h // 2) * 128:(h // 2 + 1) * 128], kkT[hp, ks], qT[hp, h // 2, qs], start=True, stop=True),
                            reads=[("kk", 0, ch // 4), ("q", h // 2, tq)], writes=[("ps", bk)])
                    for half in range(2):
                        bk = 2 + 2 * st_ + half
                        e_, p_ = ex[2 * st_ + half], pT[2 * st_ + half]
                        S.add("act", lambda e, e_=e_, bk=bk: e.activation(
                            out=e_, in_=psum[:, bk, :].rearrange("p (a b) -> p a b", a=4), func=AF.Exp),
                            writes=[("ps", bk), ("ex", 2 * st_ + half)])
                        eng = "dve" if half == 0 else "pool"
                        S.add(eng, lambda e, e_=e_, p_=p_, ch=ch: e.tensor_tensor(
                            out=p_, in0=e_, in1=maskT[:, ch, :].unsqueeze(1).broadcast_to([128, 4, 128]), op=ALU.mult),
                            reads=[("ex", 2 * st_ + half), "maskT"], writes=[("pT", 2 * st_ + half)])
                    for h in range(8):
                        ob = 6 + h // 4
                        S.add("pe", lambda e, h=h, ob=ob, ch=ch, st_=st_, b=b: e.matmul(
                            psum[:, ob, (h % 4) * 65:(h % 4) * 65 + 65], pT[2 * st_ + h % 2][:, h // 2, :], V1[:, ch, 0:65],
                            start=(ch == 0 and h % 4 == 0), stop=(ch == b), skip_group_check=True),
                            reads=[("pT", 2 * st_ + h % 2), "V1", "V1one"], writes=[("ps", ob)])
                S.add("act", lambda e: e.copy(out=otok[:, 0:4, :], in_=psum[:, 6, 0:260].rearrange("p (a b) -> p a b", a=4)),
                      writes=[("ps", 6), "otokA"])
                S.add("dve", lambda e: e.tensor_copy(out=otok[:, 4:8, :], in_=psum[:, 7, 0:260].rearrange("p (a b) -> p a b", a=4)),
                      writes=[("ps", 7), "otokB"])
                S.add("dve", lambda e: e.reciprocal(out=rden, in_=otok[:, :, 64]), reads=["otokA", "otokB"], writes=["rden"])
                S.add("dve", lambda e: e.tensor_tensor(out=on, in0=otok[:, :, 0:64], in1=rden.unsqueeze(2).broadcast_to([128, 8, 64]),
                                                       op=ALU.mult), reads=["otokA", "otokB", "rden"], writes=["on"])
                onf = on.rearrange("p a b -> p (a b)")
                S.add("dve", lambda e, onf=onf: e.tensor_tensor(out=onsq, in0=onf, in1=onf, op=ALU.mult), reads=["on"], writes=["onsq"])
                S.add("dve", lambda e: e.tensor_scalar(out=onsq, in0=onsq, scalar1=1.0, scalar2=None, op0=ALU.mult, op1=ALU.add,
                                                       accum_out=ssq), reads=["onsq"], writes=["onsq", "ssq"])
                S.add("act", lambda e: e.activation(out=rq, in_=ssq, func=AF.Sqrt, scale=1.0 / 512, bias=EPS), reads=["ssq"], writes=["rq"])
                S.add("dve", lambda e: e.reciprocal(out=rq, in_=rq), reads=["rq"], writes=["rq"])
                S.add("dve", lambda e, onf=onf: e.tensor_scalar(out=onb, in0=onf, scalar1=rq, scalar2=None, op0=ALU.mult),
                      reads=["on", "rq"], writes=["onb"])
                for c in range(4):
                    S.add("pe", lambda e, c=c: e.transpose(pb[2][:, c * 128:(c + 1) * 128], onb[:, c * 128:(c + 1) * 128], identb),
                          reads=["onb", "identb"], writes=[("ps", 2)])
                for c in range(4):
                    S.add("dve", lambda e, c=c: e.tensor_scalar(out=attT[:, c, :], in0=pb[2][:, c * 128:(c + 1) * 128],
                                                                scalar1=gatt[:, c:c + 1], scalar2=None, op0=ALU.mult),
                          reads=["PAR"], writes=[("ps", 2), "attT"])
                for dc in range(8):
                    bk = dc // 4
                    for kc in range(4):
                        S.add("pe", lambda e, dc=dc, kc=kc, bk=bk: e.matmul(
                            psum[:, bk, (dc % 4) * 128:(dc % 4 + 1) * 128], wo[:, kc, dc * 128:(dc + 1) * 128], attT[:, kc, :],
                            start=(kc == 0), stop=(kc == 3)), reads=["wo", "attT"], writes=[("ps", bk)])
                for dc in range(8):
                    bk = dc // 4
                    S.add("dve", lambda e, dc=dc, bk=bk, qs=qs: e.scalar_tensor_tensor(
                        out=XT[:, dc, qs], in0=psum[:, bk, (dc % 4) * 128:(dc % 4 + 1) * 128],
                        scalar=DER[:, l, s, 1, 2, dc:dc + 1], in1=XT[:, dc, qs], op0=ALU.mult, op1=ALU.add),
                        reads=["DER"], writes=[("ps", bk), ("XT", dc, tq)])
            S.barrier()
            M.off = m0


        def ssm_part(l, s):
            I32 = mybir.dt.int32
            m0 = M.off
            TWO_PI = float(2 * np.pi)
            PI = float(np.pi)
            wssm = M.alloc([8, 256], BF16)
            Abuf = M.alloc([2, 16, 8, 16], BF16)
            U = M.alloc([16, 256], BF16)
            Bq = M.alloc([16, 128], BF16)
            Rm = Bq
            E = M.alloc([16, 16, 16], BF16)
            Bs = M.alloc([16, 128], BF16)
            W = M.alloc([16, 128], BF16)
            wglu = M.alloc([2, 512], BF16)
            wo = M.alloc([2, D], BF16)
            zf = M.alloc([2, 512], F32)
            XR = M.alloc([16, 8], F32)
            YR = M.alloc([16, 8], F32)
            m1 = M.off
            S.add("pool", lambda e: e.dma_start(out=wssm, in_=win_d[l][:, :, C_SSM:C_SSM + 256]), writes=["wssm"], dma=True)
            S.add("pool", lambda e: e.dma_start(out=wglu, in_=wglu_d[l]), writes=["wglu"], dma=True)
            S.add("pool", lambda e: e.dma_start(out=wo, in_=wout_d[l][:, 4:6, :]), writes=["wo"], dma=True)
            NP = 32
            T = {n: M.alloc([16, NP], F32) for n in ("A", "KF", "FR", "LT", "FR2", "MAG")}
            ki_ = M.alloc([16, NP], F32).bitcast(I32)
            sm_ = {n: M.alloc([16], F32) for n in ("dt", "th", "lam", "nr", "den", "t1", "t2", "qr", "qi")}
            big = {n: M.alloc([16, 16], F32) for n in ("Q0", "t1", "t2", "BP", "BQ", "CP", "CQ")}
            tb = {n: M.alloc([8, 16], F32) for n in ("b1", "b2")}
            te = {n: M.alloc([16, 16], F32) for n in ("e1", "e2")}
            pr_ = lambda n: par(f"s{n}{l}")
            jall = par("sjall")
            sgnA, sgnC = par("ssgnA"), par("ssgnC")

            def V(eng, fn, r, w):
                S.add(eng, fn, reads=r, writes=w)

            def b3(ap2, n):
                return ap2.unsqueeze(2).broadcast_to([128, 16, n])

            def fl(ap3):
                return ap3.rearrange("p a b -> p (a b)")

            V("act", lambda e: e.activation(out=sm_["dt"], in_=pr_("ldt"), func=AF.Exp), ["PAR"], ["dt"])
            V("dve", lambda e: e.tensor_tensor(out=sm_["th"], in0=sm_["dt"], in1=pr_("aim"), op=ALU.mult), ["dt", "PAR"], ["th"])
            V("dve", lambda e: e.tensor_tensor(out=sm_["lam"], in0=sm_["dt"], in1=pr_("are"), op=ALU.mult), ["dt", "PAR"], ["lam"])
            jb = jall.unsqueeze(1).broadcast_to([128, 16, NP])
            V("dve", lambda e: e.tensor_tensor(out=T["A"], in0=b3(sm_["th"], NP), in1=jb, op=ALU.mult), ["th", "PAR"], ["A"])
            V("dve", lambda e: e.tensor_tensor(out=T["MAG"], in0=b3(sm_["lam"], NP), in1=jb, op=ALU.mult), ["lam", "PAR"], ["MAG"])
            V("act", lambda e: e.activation(out=T["MAG"], in_=T["MAG"], func=AF.Exp), ["MAG"], ["MAG"])
            V("dve", lambda e: e.tensor_scalar(out=T["A"], in0=T["A"], scalar1=1.0 / TWO_PI, scalar2=64.0, op0=ALU.mult, op1=ALU.add), ["A"], ["A"])
            V("dve", lambda e: e.tensor_copy(out=ki_, in_=T["A"]), ["A"], ["ki"])
            V("dve", lambda e: e.tensor_copy(out=T["KF"], in_=ki_), ["ki"], ["KF"])
            V("dve", lambda e: e.tensor_tensor(out=T["FR"], in0=T["A"], in1=T["KF"], op=ALU.subtract), ["A", "KF"], ["FR"])
            V("dve", lambda e: e.tensor_scalar(out=T["LT"], in0=T["FR"], scalar1=0.0, scalar2=None, op0=ALU.is_lt), ["FR"], ["LT"])
            V("dve", lambda e: e.tensor_tensor(out=T["FR"], in0=T["FR"], in1=T["LT"], op=ALU.add), ["FR", "LT"], ["FR"])
            V("dve", lambda e: e.tensor_scalar(out=T["FR2"], in0=T["FR"], scalar1=0.25, scalar2=None, op0=ALU.add), ["FR"], ["FR2"])
            V("dve", lambda e: e.tensor_scalar(out=T["LT"], in0=T["FR2"], scalar1=1.0, scalar2=None, op0=ALU.is_ge), ["FR2"], ["LT"])
            V("dve", lambda e: e.tensor_tensor(out=T["FR2"], in0=T["FR2"], in1=T["LT"], op=ALU.subtract), ["FR2", "LT"], ["FR2"])
            for nm in ("FR", "FR2"):
                V("dve", lambda e, nm=nm: e.tensor_scalar(out=T[nm], in0=T[nm], scalar1=TWO_PI, scalar2=-PI, op0=ALU.mult, op1=ALU.add), [nm], [nm])
                V("dve", lambda e, nm=nm: e.tensor_scalar(out=T[nm], in0=T[nm], scalar1=-3.1415925, scalar2=3.1415925, op0=ALU.max, op1=ALU.min), [nm], [nm])
                V("act", lambda e, nm=nm: e.activation(out=T[nm], in_=T[nm], func=AF.Sin), [nm], [nm])
                V("dve", lambda e, nm=nm: e.scalar_tensor_tensor(out=fl(T[nm]), in0=fl(T[nm]), scalar=-1.0, in1=fl(T["MAG"]),
                                                                 op0=ALU.mult, op1=ALU.mult), [nm, "MAG"], [nm])
            XA, YA = T["FR2"], T["FR"]
            XAk, YAk = "FR2", "FR"
            X1, Y1 = XA[:, :, 8], YA[:, :, 8]
            V("dve", lambda e: e.tensor_scalar(out=sm_["nr"], in0=X1, scalar1=-1.0, scalar2=None, op0=ALU.add), [XAk], ["nr"])
            V("dve", lambda e: e.tensor_tensor(out=sm_["den"], in0=pr_("are"), in1=pr_("are"), op=ALU.mult), ["PAR"], ["den"])
            V("dve", lambda e: e.tensor_tensor(out=sm_["t1"], in0=pr_("aim"), in1=pr_("aim"), op=ALU.mult), ["PAR"], ["st1"])
            V("dve", lambda e: e.tensor_tensor(out=sm_["den"], in0=sm_["den"], in1=sm_["t1"], op=ALU.add), ["den", "st1"], ["den"])
            V("dve", lambda e: e.reciprocal(out=sm_["den"], in_=sm_["den"]), ["den"], ["den"])
            V("dve", lambda e: e.tensor_tensor(out=sm_["t1"], in0=sm_["nr"], in1=pr_("are"), op=ALU.mult), ["nr", "PAR"], ["st1"])
            V("dve", lambda e: e.tensor_tensor(out=sm_["t2"], in0=Y1, in1=pr_("aim"), op=ALU.mult), [YAk, "PAR"], ["st2"])
            V("dve", lambda e: e.tensor_tensor(out=sm_["t1"], in0=sm_["t1"], in1=sm_["t2"], op=ALU.add), ["st1", "st2"], ["st1"])
            V("dve", lambda e: e.tensor_tensor(out=sm_["qr"], in0=sm_["t1"], in1=sm_["den"], op=ALU.mult), ["st1", "den"], ["qr"])
            V("dve", lambda e: e.tensor_tensor(out=sm_["t1"], in0=Y1, in1=pr_("are"), op=ALU.mult), [YAk, "PAR"], ["st1"])
            V("dve", lambda e: e.tensor_tensor(out=sm_["t2"], in0=sm_["nr"], in1=pr_("aim"), op=ALU.mult), ["nr", "PAR"], ["st2"])
            V("dve", lambda e: e.tensor_tensor(out=sm_["t1"], in0=sm_["t1"], in1=sm_["t2"], op=ALU.subtract), ["st1", "st2"], ["st1"])
            V("dve", lambda e: e.tensor_tensor(out=sm_["qi"], in0=sm_["t1"], in1=sm_["den"], op=ALU.mult), ["st1", "den"], ["qi"])
            bp3 = pr_("bp").rearrange("p (a b) -> p a b", a=16)
            bq3 = pr_("bq").rearrange("p (a b) -> p a b", a=16)
            V("dve", lambda e: e.tensor_scalar(out=big["Q0"], in0=bq3, scalar1=sgnA, scalar2=None, op0=ALU.mult), ["PAR"], ["Q0"])
            V("dve", lambda e: e.tensor_tensor(out=big["t1"], in0=bp3, in1=b3(sm_["qr"], 16), op=ALU.mult), ["PAR", "qr"], ["bt1"])
            V("dve", lambda e: e.tensor_tensor(out=big["t2"], in0=big["Q0"], in1=b3(sm_["qi"], 16), op=ALU.mult), ["Q0", "qi"], ["bt2"])
            V("dve", lambda e: e.tensor_tensor(out=big["BP"], in0=big["t1"], in1=big["t2"], op=ALU.add), ["bt1", "bt2"], ["BP"])
            V("dve", lambda e: e.tensor_tensor(out=big["t1"], in0=big["Q0"], in1=b3(sm_["qr"], 16), op=ALU.mult), ["Q0", "qr"], ["bt1"])
            V("dve", lambda e: e.tensor_tensor(out=big["t2"], in0=bp3, in1=b3(sm_["qi"], 16), op=ALU.mult), ["PAR", "qi"], ["bt2"])
            V("dve", lambda e: e.tensor_tensor(out=big["BQ"], in0=big["t1"], in1=big["t2"], op=ALU.subtract), ["bt1", "bt2"], ["BQ"])
            Bq4 = Bq.rearrange("p g (a b) -> p g a b", a=8)
            for g in range(16):
                xb_ = XA[:, g, 16:24].unsqueeze(2).broadcast_to([128, 8, 16])
                yb_ = YA[:, g, 16:24].unsqueeze(2).broadcast_to([128, 8, 16])
                bpg = big["BP"][:, g, :].unsqueeze(1).broadcast_to([128, 8, 16])
                bqg = big["BQ"][:, g, :].unsqueeze(1).broadcast_to([128, 8, 16])
                V("dve", lambda e, xb_=xb_, bpg=bpg: e.tensor_tensor(out=tb["b1"], in0=bpg, in1=xb_, op=ALU.mult), ["BP", XAk, "Bq"], ["b1"])
                V("dve", lambda e, yb_=yb_, bqg=bqg: e.tensor_tensor(out=tb["b2"], in0=bqg, in1=yb_, op=ALU.mult), ["BQ", YAk, "Bq"], ["b2"])
                V("dve", lambda e, g=g: e.tensor_tensor(out=Bq4[:, g], in0=tb["b1"], in1=tb["b2"], op=ALU.add), ["b1", "b2"], ["Bq"])
            cp3 = pr_("cp").rearrange("p (a b) -> p a b", a=16)
            cq3 = pr_("cq").rearrange("p (a b) -> p a b", a=16)
            V("dve", lambda e: e.tensor_scalar(out=big["CP"], in0=cp3, scalar1=sgnC, scalar2=None, op0=ALU.mult), ["PAR"], ["CP"])
            V("dve", lambda e: e.tensor_scalar(out=big["CQ"], in0=cq3, scalar1=-1.0, scalar2=None, op0=ALU.mult), ["PAR"], ["CQ"])
            for g in range(16):
                xe_ = XA[:, g, 0:16].unsqueeze(2).broadcast_to([128, 16, 16])
                ye_ = YA[:, g, 0:16].unsqueeze(2).broadcast_to([128, 16, 16])
                cpg = big["CP"][:, g, :].unsqueeze(1).broadcast_to([128, 16, 16])
                cqg = big["CQ"][:, g, :].unsqueeze(1).broadcast_to([128, 16, 16])
                V("dve", lambda e, xe_=xe_, cpg=cpg: e.tensor_tensor(out=te["e1"], in0=cpg, in1=xe_, op=ALU.mult), ["CP", XAk, "E"], ["e1"])
                V("dve", lambda e, ye_=ye_, cqg=cqg: e.tensor_tensor(out=te["e2"], in0=cqg, in1=ye_, op=ALU.mult), ["CQ", YAk, "E"], ["e2"])
                V("dve", lambda e, g=g: e.tensor_tensor(out=E[:, g], in0=te["e1"], in1=te["e2"], op=ALU.add), ["e1", "e2"], ["E"])
            V("dve", lambda e: e.tensor_copy(out=XR, in_=XA[:, :, 24:32]), [XAk], ["XR"])
            V("dve", lambda e: e.tensor_scalar(out=YR, in0=YA[:, :, 24:32], scalar1=sgnC, scalar2=None, op0=ALU.mult), [YAk, "PAR"], ["YR"])
            pb = [psum[:, bkk, :].bitcast(BF16) for bkk in range(8)]
            for g in range(16):
                bk = g // 8
                V("pe", lambda e, g=g, bk=bk: e.transpose(pb[bk][:, (g % 8) * 128:(g % 8 + 1) * 128], Bq[:, g, :], identb), ["Bq", "identb"], [("ps", bk)])
            for bk in range(2):
                V("act", lambda e, bk=bk: e.copy(out=Bs[:, 8 * bk:8 * bk + 8, :], in_=pb[bk].rearrange("p (a b) -> p a b", a=8)), [], [("ps", bk), "Bs"])
            tmk = par("stm")
            for g in range(16):
                bk = 2 + g // 4
                V("pe", lambda e, g=g, bk=bk: e.matmul(psum[:, bk, (g % 4) * 128:(g % 4 + 1) * 128], Bq[:, g, :],
                                                        E[:, g, 0:8, :].rearrange("p a b -> p (a b)"), start=True, stop=True), ["Bq", "E"], [("ps", bk)])
            for q4 in range(4):
                bk = 2 + q4
                V("dve", lambda e, q4=q4, bk=bk: e.tensor_tensor(
                    out=W[:, 4 * q4:4 * q4 + 4, :], in0=psum[:, bk, :].rearrange("p (a b) -> p a b", a=4),
                    in1=tmk.unsqueeze(1).broadcast_to([128, 4, 128]), op=ALU.mult), ["PAR"], [("ps", bk), "W"])
            for g in range(16):
                V("dve", lambda e, g=g: e.scalar_tensor_tensor(out=W[:, g, :], in0=identf, scalar=pr_("dcol")[:, g:g + 1], in1=W[:, g, :],
                                                              op0=ALU.mult, op1=ALU.add), ["W", "PAR", "identf"], ["W"])
            S.barrier()
            M.off = m1
            X32 = M.alloc([16, 257], F32)
            Xb = M.alloc([16, 257], BF16)
            for half in range(2):
                for sp_ in range(4):
                    bk = 4 * (half % 2) + sp_
                    for sg2 in range(2):
                        sig = 2 * sp_ + sg2
                        for k in range(8):
                            V("pe", lambda e, half=half, sig=sig, sg2=sg2, bk=bk, k=k: e.matmul(
                                psum[:, bk, sg2 * 256:(sg2 + 1) * 256], HT[:, k, 1024 * half + sig:1024 * (half + 1):8], wssm[:, k, :],
                                start=(k == 0), stop=(k == 7)), ["wssm", ("HT", k, 2 * half), ("HT", k, 2 * half + 1)], [("ps", bk)])
                    for sg2 in range(2):
                        sig = 2 * sp_ + sg2
                        src = psum[:, bk, sg2 * 256:(sg2 + 1) * 256].rearrange("p (a b) -> p a b", a=16)
                        dst = Abuf[:, half, :, sig, :]
                        if sg2 == 0:
                            V("act", lambda e, src=src, dst=dst: e.copy(out=dst, in_=src), [], [("ps", bk), ("Abuf", half)])
                        else:
                            V("dve", lambda e, src=src, dst=dst: e.tensor_copy(out=dst, in_=src), [], [("ps", bk), ("Abuf", half)])
            for half in range(2):
                for gh in range(2):
                    bk = 2 * half + gh
                    for gg in range(8):
                        g = 8 * gh + gg
                        V("pe", lambda e, half=half, g=g, gg=gg, bk=bk: e.transpose(
                            pb[bk][:, gg * 128:(gg + 1) * 128], Abuf[:, half, g].rearrange("p a b -> p (a b)"), identb),
                            [("Abuf", half), "identb"], [("ps", bk)])
                    dst = U[:, 8 * gh:8 * gh + 8, 128 * half:128 * (half + 1)]
                    src = pb[bk].rearrange("p (a b) -> p a b", a=8)
                    if gh == 0:
                        V("act", lambda e, src=src, dst=dst: e.copy(out=dst, in_=src), [], [("ps", bk), "U"])
                    else:
                        V("dve", lambda e, src=src, dst=dst: e.tensor_copy(out=dst, in_=src), [], [("ps", bk), "U"])
            V("dve", lambda e: e.memset(X32[:, :, 0:1], 0.0), [], ["X32z"])
            for g in range(16):
                bk = g // 2
                V("pe", lambda e, g=g, bk=bk: e.matmul(psum[:, bk, (g % 2) * 256:(g % 2 + 1) * 256], Bs[:, g, :], U[:, g, :], start=True, stop=True),
                  ["Bs", "U"], [("ps", bk)])
            for g2 in range(8):
                dst = X32[:, 2 * g2:2 * g2 + 2, 1:257]
                src = psum[:, g2, :].rearrange("p (a b) -> p a b", a=2)
                if g2 % 2 == 0:
                    V("act", lambda e, src=src, dst=dst: e.copy(out=dst, in_=src), [], [("ps", g2), "X32"])
                else:
                    V("dve", lambda e, src=src, dst=dst: e.tensor_copy(out=dst, in_=src), [], [("ps", g2), "X32"])
            V("act", lambda e: e.copy(out=Xb, in_=X32), ["X32", "X32z"], ["Xb"])
            swp = par("sswap")
            for lv in range(8):
                sh = 1 << lv
                n = 256 - sh
                for g in range(16):
                    V("dve", lambda e, g=g, lv=lv: e.tensor_scalar(out=Rm[:, g, :], in0=identf, scalar1=XR[:, g, lv:lv + 1], scalar2=None,
                                                                   op0=ALU.mult), ["XR", "identf"], ["Rm"])
                    V("dve", lambda e, g=g, lv=lv: e.scalar_tensor_tensor(out=Rm[:, g, :], in0=swp, scalar=YR[:, g, lv:lv + 1], in1=Rm[:, g, :],
                                                                          op0=ALU.mult, op1=ALU.add), ["YR", "PAR", "Rm"], ["Rm"])
                for g in range(16):
                    bk = g // 2
                    V("pe", lambda e, g=g, bk=bk, n=n: e.matmul(psum[:, bk, (g % 2) * 256:(g % 2) * 256 + n], Rm[:, g, :], Xb[:, g, 1:1 + n],
                                                                 start=True, stop=True), ["Rm", "Xb"], [("ps", bk)])
                for g2 in range(8):
                    V("dve", lambda e, g2=g2, n=n, sh=sh: e.tensor_tensor(
                        out=X32[:, 2 * g2:2 * g2 + 2, 1 + sh:257], in0=X32[:, 2 * g2:2 * g2 + 2, 1 + sh:257],
                        in1=psum[:, g2, :].rearrange("p (a b) -> p a b", a=2)[:, :, 0:n], op=ALU.add), ["X32"], [("ps", g2), "X32"])
                V("act", lambda e: e.copy(out=Xb, in_=X32), ["X32", "X32z"], ["Xb"])
            S.barrier()
            Ytok = Abuf.rearrange("p h g a b -> p h (g a b)").rearrange("p h (t c) -> p h t c", t=8)
            for half in range(2):
                for q4 in range(4):
                    bk = 4 * (half % 2) + q4
                    for gg in range(4):
                        g = 4 * q4 + gg
                        V("pe", lambda e, half=half, g=g, gg=gg, bk=bk: e.matmul(
                            psum[:, bk, gg * 128:(gg + 1) * 128], Xb[:, g, 128 * half:128 * half + 128],
                            E[:, g, 8:16, :].rearrange("p a b -> p (a b)"), start=True, stop=False), ["Xb", "E"], [("ps", bk)])
                        V("pe", lambda e, half=half, g=g, gg=gg, bk=bk: e.matmul(
                            psum[:, bk, gg * 128:(gg + 1) * 128], U[:, g, 128 * half:128 * half + 128], W[:, g, :],
                            start=False, stop=True), ["U", "W"], [("ps", bk)])
                    src = psum[:, bk, :].rearrange("p (g t c) -> p g t c", g=4, t=8)
                    dst = Ytok[:, half, :, 64 * q4:64 * q4 + 64].rearrange("p t (g c) -> p g t c", g=4)
                    V("act", lambda e, src=src, dst=dst: e.activation(out=dst, in_=src, func=AF.Gelu), [], [("ps", bk), ("Ytok", half)])
            S.barrier()
            M.off = m1
            YT = M.alloc([2, SEQ], BF16)
            for half in range(2):
                for cc in range(2):
                    bk = 2 * half + cc
                    for tau in range(8):
                        V("pe", lambda e, half=half, cc=cc, tau=tau, bk=bk: e.transpose(
                            pb[bk][:, tau * 128:(tau + 1) * 128], Ytok[:, half, tau, cc * 128:(cc + 1) * 128], identb),
                            [("Ytok", half), "identb"], [("ps", bk)])
                    dst = YT[:, cc, 1024 * half:1024 * (half + 1)].rearrange("p (k t) -> p t k", t=8)
                    src = pb[bk].rearrange("p (t k) -> p t k", t=8)
                    if cc == 0:
                        V("act", lambda e, src=src, dst=dst: e.copy(out=dst, in_=src), [], [("ps", bk), ("YT", half)])
                    else:
                        V("dve", lambda e, src=src, dst=dst: e.tensor_copy(out=dst, in_=src), [], [("ps", bk), ("YT", half)])
            ms = U.rearrange("p a b -> p (a b)").rearrange("p (c n) -> p c n", c=2)
            S.barrier()
            for t in range(4):
                ts_ = slice(t * 512, (t + 1) * 512)
                for oc in range(4):
                    for kc in range(2):
                        V("pe", lambda e, oc=oc, kc=kc, ts_=ts_: e.matmul(PS(oc), wglu[:, kc, oc * 128:(oc + 1) * 128], YT[:, kc, ts_],
                                                                         start=(kc == 0), stop=(kc == 1)), ["wglu", ("YT", t // 2)], [("ps", oc)])
                for cc in range(2):
                    sg = sgf[cc]
                    V("act", lambda e, sg=sg, cc=cc: e.activation(out=sg, in_=PS(2 + cc), func=AF.Sigmoid), [], [("ps", 2 + cc), ("sgf", cc)])
                    V("dve", lambda e, sg=sg, cc=cc: e.tensor_tensor(out=zf[:, cc, :], in0=sg, in1=PS(cc), op=ALU.mult),
                      [("sgf", cc)], [("ps", cc), "zf"])
                gnorm_tile(zf, "zf", 2, par(f"sgn{l}"), ms[:, :, ts_], ("ms", t), 6)
            wout_part(l, s, ms, lambda t: ("ms", t), 2, wo, "wo")
            S.barrier()
            M.off = m0

        def mixer(l, s):
            normmod(l, s, 1)
            if "conv" in MIXPARTS:
                conv_part(l, s)
            if "ssm" in MIXPARTS:
                ssm_part(l, s)
            if "att" in MIXPARTS:
                att_part(l, s)

        for s in range(2):
            load_x(s)
            for (l, kind) in phases:
                if kind == "ffn1":
                    ffn(l, s, 1)
                elif kind == "ffn2":
                    ffn(l, s, 2)
                else:
                    mixer(l, s)
            store_x(s, last)
        S.finish()
        nsem = S.emit(st)
        print(f"[build] ops={len(S.ops)} sems={nsem}")
    return nc


MIXPARTS = {"conv", "att", "ssm"}


ALL_PHASES = [(l, k) for l in range(2) for k in ("ffn1", "mix", "ffn2")]


def prep_weights(inp):
    w = {}
    for j, nm in ((1, "ffn1"), (2, "ffn2")):
        gu = inp[f"{nm}_w_gu"]
        g = gu[:, :, :DFF].reshape(2, 8, 128, NF, 128)
        u = gu[:, :, DFF:].reshape(2, 8, 128, NF, 128)
        cat = np.concatenate([g, u], axis=-1)
        w[f"wgu{j}"] = np.ascontiguousarray(cat.transpose(0, 3, 2, 1, 4)).reshape(2, NF, 128, 2048)
        w[f"wdn{j}"] = np.ascontiguousarray(inp[f"{nm}_w_down"]).reshape(2, NF, 128, D)
    w["mod_w"] = np.ascontiguousarray(inp["mod_w"])
    for l in range(2):
        wi = inp["w_in"][l]
        q, k, v, qi, ki, wv, ssm, ca, cg = (wi[:, 0:512], wi[:, 512:576], wi[:, 576:640], wi[:, 640:896], wi[:, 896:928],
                                            wi[:, 928:936], wi[:, 936:1192], wi[:, 1192:1448], wi[:, 1448:1704])
        cat = np.concatenate([q, k, k, ki, ki, ki, ki, qi, ca, cg, v, wv, ssm], axis=1)
        assert cat.shape[1] == NWIN
        w[f"win{l}"] = np.ascontiguousarray(cat.reshape(8, 128, NWIN).transpose(1, 0, 2))
        w[f"wout{l}"] = np.ascontiguousarray(inp["w_out"][l].reshape(8, 128, D).transpose(1, 0, 2))
        w[f"wpw{l}"] = np.ascontiguousarray(inp["conv_w_pw"][l].reshape(2, 128, 256).transpose(1, 0, 2))
        w[f"wglu{l}"] = np.ascontiguousarray(inp["ssm_w_glu"][l].reshape(2, 128, 512).transpose(1, 0, 2))
    return w


def run_phases(inp, xcur, phases, first, last, shared=None):
    shared = shared if shared is not None else prep_weights(inp)
    packs = [pack_small(inp, c) for c in range(8)]
    nc = build(phases, first, last, packs[0].off, packs[0].n)
    in_maps = []
    need = {"mod_w"}
    for l, k in phases:
        if k == "ffn1":
            need |= {"wgu1", "wdn1"}
        elif k == "ffn2":
            need |= {"wgu2", "wdn2"}
        else:
            need |= {f"win{l}", f"wout{l}", f"wpw{l}", f"wglu{l}"}
    shared = {k: v for k, v in shared.items() if k in need}
    for c in range(8):
        m = dict(shared)
        m["x"] = np.ascontiguousarray(xcur[2 * c:2 * c + 2])
        m["par"] = packs[c].array()
        in_maps.append(m)
    res = run_bass_kernel_spmd(nc, in_maps, core_ids=list(range(8)))
    return np.concatenate([r["y"] for r in res.results], axis=0)


def kernel(**inputs):
    inp = {k: np.asarray(v) for k, v in inputs.items()}
    x = np.ascontiguousarray(inp["x"], dtype=np.float32)
    return run_phases(inp, x, ALL_PHASES, True, True)
```

```python
import numpy as np
from contextlib import ExitStack
import concourse.bass as bass
import concourse.mybir as mybir
from concourse.bass_utils import run_bass_kernel_spmd

F32 = mybir.dt.float32
BF16 = mybir.dt.bfloat16
ALU = mybir.AluOpType
AF = mybir.ActivationFunctionType
AX = mybir.AxisListType

ROT = 2048
DMA_SLOTS = 12
DMA_ROT = 120

D = 1024
SEQ = 2048
DFF = 2816
NF = 22
NWIN = 1864
C_Q, C_KK, C_KI, C_QI, C_A, C_G, C_VW, C_SSM = 0, 512, 640, 768, 1024, 1280, 1536, 1608
EPS = 1e-6


class Op:
    __slots__ = ("eng", "fn", "reads", "writes", "dma", "deps", "inc", "cnt",
                 "dslot", "dgen", "dval", "waits", "clock", "idx")


class Sched:
    def __init__(self, nc):
        self.nc = nc
        self.ops = []
        self.last_w = {}
        self.readers = {}
        self.dma_n = 0
        self.dma_prev = {}

    def add(self, eng, fn, reads=(), writes=(), dma=False):
        op = Op()
        op.eng, op.fn, op.dma = eng, fn, dma
        op.reads, op.writes = tuple(reads), tuple(writes)
        op.inc = False
        op.idx = len(self.ops)
        deps = {}
        for k in op.reads:
            w = self.last_w.get(k)
            if w is not None:
                deps[w.idx] = (w, True)
        for k in op.writes:
            w = self.last_w.get(k)
            if w is not None and w.idx not in deps:
                deps[w.idx] = (w, False)
            for r in self.readers.get(k, ()):
                if r.idx not in deps:
                    deps[r.idx] = (r, False)
        if dma:
            slot = self.dma_n % DMA_SLOTS
            self.dma_n += 1
            prev = self.dma_prev.get(slot)
            if prev is not None and prev.idx not in deps:
                deps[prev.idx] = (prev, True)
            self.dma_prev[slot] = op
            op.dslot = slot
            op.inc = True
        need = []
        for d, raw in deps.values():
            if d is op:
                continue
            if d.dma or op.dma or d.eng != eng:
                need.append(d)
            elif raw and eng != "pe":
                need.append(d)
        for d in need:
            d.inc = True
        op.deps = need
        for k in op.reads:
            self.readers.setdefault(k, []).append(op)
        for k in op.writes:
            self.last_w[k] = op
            self.readers[k] = []
        self.ops.append(op)
        return op

    def barrier(self):
        keys = set(self.last_w.keys()) | set(self.readers.keys())
        keys.discard("BAR")
        self.add("dve", lambda e: e.nop(), reads=list(keys), writes=["BAR"])
        for en in ("pe", "act", "pool", "sp"):
            self.add(en, lambda e: e.nop(), reads=["BAR"], writes=[("BARL", en)])
        bar = self.last_w["BAR"]
        self.last_w = {"BAR": bar}
        self.readers = {}
        self._bar = bar

    def finish(self, eng="sp"):
        keys = [k for k, w in self.last_w.items() if w.dma]
        self.add(eng, lambda e: e.nop(), reads=keys)

    def emit(self, stack):
        nc = self.nc
        engs = ["pe", "act", "dve", "pool", "sp"]
        cnt = {e: 0 for e in engs}
        dcount = {}
        for op in self.ops:
            if op.dma:
                g = dcount.get(op.dslot, 0)
                op.dgen, op.dval = g // DMA_ROT, 16 * (g % DMA_ROT + 1)
                dcount[op.dslot] = g + 1
            elif op.inc:
                cnt[op.eng] += 1
                op.cnt = cnt[op.eng]
        sems = {}

        def sem(name):
            if name not in sems:
                sems[name] = stack.enter_context(nc.semaphore(name))
            return sems[name]

        known = {e: {} for e in engs}
        for op in self.ops:
            kn = known[op.eng]
            waits = []
            for d in sorted(op.deps, key=lambda o: o.idx):
                if d.dma:
                    key = ("d", d.dslot, d.dgen)
                    val = d.dval
                else:
                    key = d.eng
                    val = d.cnt
                if kn.get(key, 0) >= val:
                    continue
                waits.append(d)
                for k2, v2 in d.clock.items():
                    if kn.get(k2, 0) < v2:
                        kn[k2] = v2
                kn[key] = val
            op.waits = waits
            if op.inc:
                op.clock = dict(kn)
                if op.dma:
                    op.clock[("d", op.dslot, op.dgen)] = op.dval
                else:
                    op.clock[op.eng] = op.cnt
            else:
                op.clock = None

        for op in self.ops:
            if op.inc:
                if op.dma:
                    sem(f"d{op.dslot}_{op.dgen}")
                else:
                    sem(f"{op.eng}_{(op.cnt - 1) // ROT}")

        def emit_engine(ename, e):
            for op in self.ops:
                if op.eng != ename:
                    continue
                for d in op.waits:
                    if d.dma:
                        e.wait_ge(sem(f"d{d.dslot}_{d.dgen}"), d.dval)
                    else:
                        c = d.cnt - 1
                        e.wait_ge(sem(f"{d.eng}_{c // ROT}"), c % ROT + 1)
                ins = op.fn(e)
                if op.inc:
                    if op.dma:
                        ins.then_inc(sem(f"d{op.dslot}_{op.dgen}"), 16)
                    else:
                        c = op.cnt - 1
                        ins.then_inc(sem(f"{op.eng}_{c // ROT}"), 1)

        block = stack.enter_context(nc.Block())

        @block.tensor
        def _(e):
            emit_engine("pe", e)

        @block.scalar
        def _(e):
            emit_engine("act", e)

        @block.vector
        def _(e):
            emit_engine("dve", e)

        @block.gpsimd
        def _(e):
            emit_engine("pool", e)

        @block.sync
        def _(e):
            emit_engine("sp", e)
        return len(sems)


class Mem:
    def __init__(self, pool, nbytes):
        self.pool, self.nbytes, self.off = pool, nbytes, 0

    def view(self, off, shape, dtype):
        o = self.off
        self.off = off
        v = self.alloc(shape, dtype)
        self.off = o
        return v

    def alloc(self, shape, dtype):
        isz = 4 if dtype == F32 else 2
        n = int(np.prod(shape))
        nb = (n * isz + 63) // 64 * 64
        assert self.off + nb <= self.nbytes, (self.off, nb, self.nbytes)
        v = self.pool[:, self.off // 2:(self.off + n * isz) // 2]
        self.off += nb
        if dtype == F32:
            v = v.bitcast(F32)
        if len(shape) > 1:
            names = [f"a{i}" for i in range(len(shape))]
            kw = {n: int(d) for n, d in zip(names[:-1], shape[:-1])}
            v = v.rearrange(f"p ({' '.join(names)}) -> p {' '.join(names)}", **kw)
        return v


def _fm(v, nch):
    return np.ascontiguousarray(v.reshape(nch, 128).T)


class Pack:
    def __init__(self):
        self.cols, self.off, self.n = [], {}, 0

    def add(self, name, arr):
        arr = np.asarray(arr, np.float32).reshape(128, -1)
        self.off[name] = (self.n, arr.shape[1])
        self.cols.append(arr)
        self.n += arr.shape[1]

    def array(self):
        return np.ascontiguousarray(np.concatenate(self.cols, axis=1))


def pack_small(inp, core):
    P = Pack()
    b0 = 2 * core
    P.add("cT", np.stack([_fm(inp["c"][b0 + s], 8) for s in range(2)], axis=-1))
    for l in range(2):
        P.add(f"modb{l}", _fm(inp["mod_b"][l], 72))
        for j, nm in enumerate(("ffn1_norm", "mix_norm", "ffn2_norm")):
            P.add(f"ng{l}{j}", _fm(inp[nm][l], 8))
    P.add("fin", _fm(inp["final_norm"], 8))
    for l in range(2):
        P.add(f"cw{l}", inp["conv_w_dw"][l].T.reshape(2, 128, 31).transpose(1, 0, 2))
        P.add(f"cb{l}", _fm(inp["conv_b_dw"][l], 2))
        P.add(f"clg{l}", _fm(inp["conv_ln_g"][l], 2))
        P.add(f"clb{l}", _fm(inp["conv_ln_b"][l], 2))
        P.add(f"cgn{l}", _fm(inp["conv_out_norm"][l], 2))
        P.add(f"sgn{l}", _fm(inp["ssm_out_norm"][l], 2))
        P.add(f"agn{l}", _fm(inp["att_out_norm"][l], 4))
        dup = lambda a: np.concatenate([a, a], axis=0)
        P.add(f"sare{l}", dup(inp["ssm_a_re"][l].T))
        P.add(f"saim{l}", dup(inp["ssm_a_im"][l].T))
        P.add(f"sldt{l}", np.broadcast_to(inp["ssm_log_dt"][l][None, :], (128, 16)))
        bre = inp["ssm_b_re"][l].transpose(1, 0, 2).reshape(64, 256)
        bim = inp["ssm_b_im"][l].transpose(1, 0, 2).reshape(64, 256)
        P.add(f"sbp{l}", np.concatenate([bre, bim], axis=0))
        P.add(f"sbq{l}", np.concatenate([bim, bre], axis=0))
        cre = inp["ssm_c_re"][l].transpose(2, 0, 1).reshape(64, 256)
        cim = inp["ssm_c_im"][l].transpose(2, 0, 1).reshape(64, 256)
        P.add(f"scp{l}", np.concatenate([cre, cim], axis=0))
        P.add(f"scq{l}", np.concatenate([cim, cre], axis=0))
        P.add(f"sdcol{l}", np.tile(inp["ssm_d"][l].T, (8, 1)))
    jall = np.concatenate([np.arange(-7, 9), np.arange(7, -1, -1), 8 * 2 ** np.arange(8)]).astype(np.float32)
    P.add("sjall", np.broadcast_to(jall[None, :], (128, 32)))
    sg = np.ones((128, 1), np.float32); sg[:64] = -1
    P.add("ssgnA", sg)
    P.add("ssgnC", -sg)
    sig = np.arange(128) // 16
    P.add("stm", (sig[None, :] >= sig[:, None]).astype(np.float32))
    sw = np.zeros((128, 128), np.float32); sw[np.arange(128), (np.arange(128) + 64) % 128] = 1
    P.add("sswap", sw)
    return P


def build(phases, first, last, poff, npar):
    nc = bass.Bass("TRN2", target_bir_lowering=False)
    x_d = nc.dram_tensor("x", [2, SEQ, D], F32, kind="ExternalInput").ap()
    par_d = nc.dram_tensor("par", [128, npar], F32, kind="ExternalInput").ap()
    modw_d = nc.dram_tensor("mod_w", [2, D, 9 * D], F32, kind="ExternalInput").ap()
    kinds = {k for _, k in phases}
    wgu_d = {j: nc.dram_tensor(f"wgu{j}", [2, NF, 128, 2048], F32, kind="ExternalInput").ap()
             for j in (1, 2) if f"ffn{j}" in kinds}
    wdn_d = {j: nc.dram_tensor(f"wdn{j}", [2, NF, 128, D], F32, kind="ExternalInput").ap()
             for j in (1, 2) if f"ffn{j}" in kinds}
    y_d = nc.dram_tensor("y", [2, SEQ, D], F32, kind="ExternalOutput").ap()
    mixl = sorted({l for l, k in phases if k == "mix"})
    win_d = {l: nc.dram_tensor(f"win{l}", [128, 8, NWIN], F32, kind="ExternalInput").ap() for l in mixl}
    wout_d = {l: nc.dram_tensor(f"wout{l}", [128, 8, D], F32, kind="ExternalInput").ap() for l in mixl}
    wpw_d = {l: nc.dram_tensor(f"wpw{l}", [128, 2, 256], F32, kind="ExternalInput").ap() for l in mixl}
    wglu_d = {l: nc.dram_tensor(f"wglu{l}", [128, 2, 512], F32, kind="ExternalInput").ap() for l in mixl}

    st = ExitStack()
    with st:
        S = Sched(nc)
        POOLB = 206 * 1024
        pool = st.enter_context(nc.sbuf_tensor("pool", [128, POOLB // 2], BF16))
        psum = st.enter_context(nc.psum_tensor("psum", [128, 8, 512], F32))
        M = Mem(pool, POOLB)

        def PS(b):
            return psum[:, b, :]

        XT = M.alloc([8, SEQ], F32)
        HT = M.alloc([8, SEQ], BF16)
        identf = M.alloc([128], F32)
        identb = M.alloc([128], BF16)
        onesb = M.alloc([128], BF16)
        PAR = M.alloc([npar], F32)
        MOD = M.alloc([2, 72, 2], F32)
        DER = M.alloc([2, 2, 3, 3, 8], F32)
        condb = M.alloc([8, 2], BF16)
        sq = [M.alloc([512], BF16) for _ in range(2)]
        rs = M.alloc([512], F32)
        rstd = M.alloc([512], F32)
        tmpf = [M.alloc([512], F32) for _ in range(2)]
        sgf = [M.alloc([512], F32) for _ in range(2)]
        big0 = M.off

        def par(name):
            o, n = poff[name]
            return PAR[:, o:o + n]

        S.add("sp", lambda e: e.dma_start(out=PAR, in_=par_d), writes=["PAR"], dma=True)
        S.add("pool", lambda e: e.memset(identf, 0.0), writes=["identf"])
        S.add("pool", lambda e: e.affine_select(out=identf, in_=identf, pattern=[[-1, 128]],
                                                compare_op=ALU.not_equal, fill=1.0, base=0,
                                                channel_multiplier=1), reads=["identf"], writes=["identf"])
        S.add("dve", lambda e: e.tensor_copy(out=identb, in_=identf), reads=["identf"], writes=["identb"])
        S.add("dve", lambda e: e.memset(onesb, 1.0), writes=["onesb"])

        S.add("act", lambda e: e.activation(out=condb, in_=par("cT").rearrange("p (a b) -> p a b", a=8),
                                            func=AF.Silu), reads=["PAR"], writes=["condb"])
        layers = sorted({l for l, _ in phases})
        mslab = [M.alloc([8, 1024], BF16) for _ in range(2)]
        for l in layers:
            for sl in range(9):
                buf = mslab[sl % 2]
                S.add("pool", lambda e, buf=buf, l=l, sl=sl: e.dma_start(
                    out=buf, in_=modw_d[l, :, sl * 1024:(sl + 1) * 1024].rearrange("(k p) n -> p k n", p=128)),
                    writes=[("mslab", sl % 2)], dma=True)
                for fc in range(8):
                    ch = sl * 8 + fc
                    for k in range(8):
                        S.add("pe", lambda e, buf=buf, fc=fc, k=k, ch=ch: e.matmul(
                            psum[:, 0, 2 * ch:2 * ch + 2], buf[:, k, fc * 128:(fc + 1) * 128], condb[:, k, :],
                            start=(k == 0), stop=(k == 7)),
                            reads=[("mslab", sl % 2), "condb"], writes=[("ps", 0)])
            for s in range(2):
                S.add("dve", lambda e, l=l, s=s: e.tensor_tensor(
                    out=MOD[:, l, :, s], in0=psum[:, 0, s:144:2], in1=par(f"modb{l}"), op=ALU.add),
                    reads=["PAR"], writes=[("ps", 0), "MOD"])
            for s in range(2):
                for j in range(3):
                    a_, sh_, g_ = DER[:, l, s, j, 0, :], DER[:, l, s, j, 1, :], DER[:, l, s, j, 2, :]
                    c0 = 3 * j * 8
                    S.add("dve", lambda e, a_=a_, l=l, s=s, j=j, c0=c0: e.scalar_tensor_tensor(
                        out=a_, in0=MOD[:, l, c0 + 8:c0 + 16, s], scalar=1.0, in1=par(f"ng{l}{j}"),
                        op0=ALU.add, op1=ALU.mult), reads=["MOD", "PAR"], writes=["DER"])
                    S.add("dve", lambda e, sh_=sh_, l=l, s=s, c0=c0: e.tensor_copy(
                        out=sh_, in_=MOD[:, l, c0:c0 + 8, s]), reads=["MOD"], writes=["DER"])
                    S.add("dve", lambda e, g_=g_, l=l, s=s, j=j, c0=c0: e.tensor_scalar(
                        out=g_, in0=MOD[:, l, c0 + 16:c0 + 24, s], scalar1=(1.0 if j == 1 else 0.5), scalar2=None,
                        op0=ALU.mult), reads=["MOD"], writes=["DER"])
        S.barrier()
        M.off = big0

        def normmod(l, s, j):
            for t in range(4):
                ts_ = slice(t * 512, (t + 1) * 512)
                for c in range(8):
                    q_ = sq[c % 2]
                    S.add("act", lambda e, q_=q_, c=c, ts_=ts_: e.activation(out=q_, in_=XT[:, c, ts_], func=AF.Square),
                          reads=[("XT", c, t)], writes=[("sq", c % 2)])
                    S.add("pe", lambda e, q_=q_, c=c: e.matmul(PS(6), onesb, q_, start=(c == 0), stop=(c == 7)),
                          reads=[("sq", c % 2), "onesb"], writes=[("ps", 6)])
                S.add("act", lambda e: e.activation(out=rs, in_=PS(6), func=AF.Sqrt, scale=1.0 / D, bias=EPS),
                      writes=[("ps", 6), "rs"])
                S.add("dve", lambda e: e.reciprocal(out=rstd, in_=rs), reads=["rs"], writes=["rstd"])
                for c in range(8):
                    tf = tmpf[c % 2]
                    S.add("dve", lambda e, tf=tf, c=c, ts_=ts_: e.scalar_tensor_tensor(
                        out=tf, in0=XT[:, c, ts_], scalar=DER[:, l, s, j, 0, c:c + 1], in1=rstd,
                        op0=ALU.mult, op1=ALU.mult), reads=[("XT", c, t), "rstd", "DER"], writes=[("tmpf", c % 2)])
                    S.add("act", lambda e, tf=tf, c=c, ts_=ts_: e.activation(
                        out=HT[:, c, ts_], in_=tf, func=AF.Identity, bias=DER[:, l, s, j, 1, c:c + 1], scale=1.0),
                        reads=[("tmpf", c % 2), "DER"], writes=[("HT", c, t)])

        def ffn(l, s, which):
            j = 0 if which == 1 else 2
            normmod(l, s, j)
            m0 = M.off
            act = M.alloc([11, SEQ], BF16)
            wgu = [M.alloc([8, 256], BF16) for _ in range(3)]
            wdn = M.alloc([11, D], BF16)

            def load_gu(f):
                S.add("pool", lambda e, f=f: e.dma_start(
                    out=wgu[f % 3], in_=wgu_d[which][l, f].rearrange("p (k n) -> p k n", k=8)),
                    writes=[("wgu", f % 3)], dma=True)

            for f in range(2):
                load_gu(f)
            pi = 0
            for grp in range(2):
                for fl in range(11):
                    f = grp * 11 + fl
                    if f + 2 < NF:
                        load_gu(f + 2)
                    S.add("pool", lambda e, f=f, fl=fl: e.dma_start(out=wdn[:, fl, :], in_=wdn_d[which][l, f]),
                          writes=[("wdn", fl)], dma=True)
                    w = wgu[f % 3]
                    for t in range(4):
                        ts_ = slice(t * 512, (t + 1) * 512)
                        bg, bu = 2 * (pi % 2), 2 * (pi % 2) + 1
                        pi += 1
                        for gu, bk in ((0, bg), (1, bu)):
                            for k in range(8):
                                S.add("pe", lambda e, w=w, gu=gu, bk=bk, k=k, ts_=ts_: e.matmul(
                                    PS(bk), w[:, k, gu * 128:(gu + 1) * 128], HT[:, k, ts_],
                                    start=(k == 0), stop=(k == 7)),
                                    reads=[("wgu", f % 3), ("HT", k, t)], writes=[("ps", bk)])
                        sg = sgf[pi % 2]
                        S.add("act", lambda e, sg=sg, bg=bg: e.activation(out=sg, in_=PS(bg), func=AF.Silu),
                              writes=[("ps", bg), ("sgf", pi % 2)])
                        S.add("dve", lambda e, sg=sg, bu=bu, fl=fl, ts_=ts_: e.tensor_tensor(
                            out=act[:, fl, ts_], in0=sg, in1=PS(bu), op=ALU.mult),
                            reads=[("sgf", pi % 2)], writes=[("ps", bu), ("act", fl, t)])
                di = 0
                for dc in range(8):
                    for t in range(4):
                        ts_ = slice(t * 512, (t + 1) * 512)
                        bk = 4 + di % 2
                        di += 1
                        for fl in range(11):
                            S.add("pe", lambda e, fl=fl, dc=dc, bk=bk, ts_=ts_: e.matmul(
                                PS(bk), wdn[:, fl, dc * 128:(dc + 1) * 128], act[:, fl, ts_],
                                start=(fl == 0), stop=(fl == 10)),
                                reads=[("wdn", fl), ("act", fl, t)], writes=[("ps", bk)])
                        S.add("dve", lambda e, dc=dc, bk=bk, ts_=ts_: e.scalar_tensor_tensor(
                            out=XT[:, dc, ts_], in0=PS(bk), scalar=DER[:, l, s, j, 2, dc:dc + 1], in1=XT[:, dc, ts_],
                            op0=ALU.mult, op1=ALU.add), reads=["DER"], writes=[("ps", bk), ("XT", dc, t)])
            S.barrier()
            M.off = m0

        def load_x(s):
            m0 = M.off
            stg = [M.alloc([D], F32) for _ in range(2)]
            for i in range(16):
                sg_ = stg[i % 2]
                S.add("sp", lambda e, sg_=sg_, i=i: e.dma_start(out=sg_, in_=x_d[s, i * 128:(i + 1) * 128, :]),
                      writes=[("stg", i % 2)], dma=True)
                for h in range(2):
                    bk = 6 + (2 * i + h) % 2
                    for cc in range(4):
                        c = 4 * h + cc
                        S.add("pe", lambda e, sg_=sg_, c=c, cc=cc, bk=bk: e.transpose(
                            psum[:, bk, cc * 128:(cc + 1) * 128], sg_[:, c * 128:(c + 1) * 128], identf),
                            reads=[("stg", i % 2), "identf"], writes=[("ps", bk)])
                    eng = "act" if h == 0 else "dve"
                    dst = XT[:, 4 * h:4 * h + 4, i * 128:(i + 1) * 128]
                    src = psum[:, bk, :].rearrange("p (a b) -> p a b", a=4)
                    if eng == "act":
                        S.add("act", lambda e, dst=dst, src=src: e.copy(out=dst, in_=src),
                              writes=[("ps", bk)] + [("XT", 4 * h + cc, i // 4) for cc in range(4)])
                    else:
                        S.add("dve", lambda e, dst=dst, src=src: e.tensor_copy(out=dst, in_=src),
                              writes=[("ps", bk)] + [("XT", 4 * h + cc, i // 4) for cc in range(4)])
            S.barrier()
            M.off = m0

        def store_x(s, final):
            m0 = M.off
            stg = [M.alloc([D], F32) for _ in range(2)]
            xn = [M.alloc([8, 128], F32) for _ in range(2)]
            for t in range(4):
                ts_ = slice(t * 512, (t + 1) * 512)
                if final:
                    for c in range(8):
                        q_ = sq[c % 2]
                        S.add("act", lambda e, q_=q_, c=c, ts_=ts_: e.activation(out=q_, in_=XT[:, c, ts_], func=AF.Square),
                              reads=[("XT", c, t)], writes=[("sq", c % 2)])
                        S.add("pe", lambda e, q_=q_, c=c: e.matmul(PS(5), onesb, q_, start=(c == 0), stop=(c == 7)),
                              reads=[("sq", c % 2), "onesb"], writes=[("ps", 5)])
                    S.add("act", lambda e: e.activation(out=rs, in_=PS(5), func=AF.Sqrt, scale=1.0 / D, bias=EPS),
                          writes=[("ps", 5), "rs"])
                    S.add("dve", lambda e: e.reciprocal(out=rstd, in_=rs), reads=["rs"], writes=["rstd"])
                for ii in range(4):
                    i = 4 * t + ii
                    isl = slice(i * 128, (i + 1) * 128)
                    xb_ = xn[i % 2]
                    if final:
                        for c in range(8):
                            S.add("dve", lambda e, xb_=xb_, c=c, isl=isl, ii=ii: e.scalar_tensor_tensor(
                                out=xb_[:, c, :], in0=XT[:, c, isl], scalar=par("fin")[:, c:c + 1],
                                in1=rstd[:, ii * 128:(ii + 1) * 128], op0=ALU.mult, op1=ALU.mult),
                                reads=[("XT", c, t), "rstd", "PAR"], writes=[("xn", i % 2)])
                    sg_ = stg[i % 2]
                    for h in range(2):
                        bk = 6 + (2 * i + h) % 2
                        for cc in range(4):
                            c = 4 * h + cc
                            src = xb_[:, c, :] if final else XT[:, c, isl]
                            S.add("pe", lambda e, src=src, cc=cc, bk=bk: e.transpose(
                                psum[:, bk, cc * 128:(cc + 1) * 128], src, identf),
                                reads=[("xn", i % 2), ("XT", c, t), "identf"], writes=[("ps", bk)])
                        dst = sg_[:, h * 512:(h + 1) * 512]
                        if h == 0:
                            S.add("act", lambda e, dst=dst, bk=bk: e.copy(out=dst, in_=PS(bk)),
                                  writes=[("ps", bk), ("stg", i % 2, h)])
                        else:
                            S.add("dve", lambda e, dst=dst, bk=bk: e.tensor_copy(out=dst, in_=PS(bk)),
                                  writes=[("ps", bk), ("stg", i % 2, h)])
                    S.add("sp", lambda e, sg_=sg_, isl=isl: e.dma_start(out=y_d[s, isl, :], in_=sg_),
                          reads=[("stg", i % 2, 0), ("stg", i % 2, 1)], writes=[("y", s, i)], dma=True)
            S.barrier()
            M.off = m0


        NIT = 10

        def gnorm_tile(src, skey, nch, gn, dst, dkey, bank):
            for c in range(nch):
                q_ = sq[c % 2]
                S.add("act", lambda e, q_=q_, c=c: e.activation(out=q_, in_=src[:, c, :], func=AF.Square),
                      reads=[skey], writes=[("sq", c % 2)])
                S.add("pe", lambda e, q_=q_, c=c: e.matmul(PS(bank), onesb, q_, start=(c == 0), stop=(c == nch - 1)),
                      reads=[("sq", c % 2), "onesb"], writes=[("ps", bank)])
            S.add("act", lambda e: e.activation(out=rs, in_=PS(bank), func=AF.Sqrt, scale=1.0 / (128 * nch), bias=EPS),
                  writes=[("ps", bank), "rs"])
            S.add("dve", lambda e: e.reciprocal(out=rstd, in_=rs), reads=["rs"], writes=["rstd"])
            for c in range(nch):
                S.add("dve", lambda e, c=c: e.scalar_tensor_tensor(
                    out=dst[:, c, :], in0=src[:, c, :], scalar=gn[:, c:c + 1], in1=rstd, op0=ALU.mult, op1=ALU.mult),
                    reads=[skey, "rstd", "PAR"], writes=[dkey])

        def wout_part(l, s, mc, mkeyf, nch, wo, wokey):
            di = 0
            for dc in range(8):
                for t in range(4):
                    ts_ = slice(t * 512, (t + 1) * 512)
                    bk = 4 + di % 2
                    di += 1
                    for kc in range(nch):
                        S.add("pe", lambda e, kc=kc, dc=dc, bk=bk, ts_=ts_: e.matmul(
                            PS(bk), wo[:, kc, dc * 128:(dc + 1) * 128], mc[:, kc, ts_],
                            start=(kc == 0), stop=(kc == nch - 1)), reads=[wokey, mkeyf(t)], writes=[("ps", bk)])
                    S.add("dve", lambda e, dc=dc, bk=bk, ts_=ts_: e.scalar_tensor_tensor(
                        out=XT[:, dc, ts_], in0=PS(bk), scalar=DER[:, l, s, 1, 2, dc:dc + 1], in1=XT[:, dc, ts_],
                        op0=ALU.mult, op1=ALU.add), reads=["DER"], writes=[("ps", bk), ("XT", dc, t)])

        def conv_part(l, s):
            m0 = M.off
            wab = M.alloc([8, 512], BF16)
            ucv = M.alloc([2, 30 + SEQ], BF16)
            DWc = M.alloc([2, 31, 128], BF16)
            yacc = M.alloc([2, SEQ], F32)
            zs = M.alloc([2, SEQ], BF16)
            wpw = M.alloc([2, 256], BF16)
            wo = M.alloc([2, D], BF16)
            mc = DWc.rearrange("p a b c -> p (a b c)")[:, 0:2 * SEQ].rearrange("p (a b) -> p a b", a=2)
            ycf = M.alloc([2, 512], F32)
            mt = M.alloc([512], F32)
            msq = M.alloc([512], F32)
            d1 = [M.alloc([512], F32) for _ in range(2)]
            ybf = [M.alloc([512], BF16) for _ in range(2)]
            cw = par(f"cw{l}").rearrange("p (a b) -> p a b", a=2)
            S.add("pool", lambda e: e.dma_start(out=wab, in_=win_d[l][:, :, C_A:C_A + 512]), writes=["wab"], dma=True)
            S.add("pool", lambda e: e.dma_start(out=wpw, in_=wpw_d[l]), writes=["wpw"], dma=True)
            S.add("pool", lambda e: e.dma_start(out=wo, in_=wout_d[l][:, 6:8, :]), writes=["wo"], dma=True)
            S.add("dve", lambda e: e.memset(ucv[:, :, 0:30], 0.0), writes=["ucvpad"])
            pi = 0
            for cc in range(2):
                for t in range(4):
                    ts_ = slice(t * 512, (t + 1) * 512)
                    ba, bg = 2 * (pi % 2), 2 * (pi % 2) + 1
                    pi += 1
                    for (c0, bk) in ((cc * 128, ba), (256 + cc * 128, bg)):
                        for k in range(8):
                            S.add("pe", lambda e, c0=c0, bk=bk, k=k, ts_=ts_: e.matmul(
                                PS(bk), wab[:, k, c0:c0 + 128], HT[:, k, ts_], start=(k == 0), stop=(k == 7)),
                                reads=["wab", ("HT", k, t)], writes=[("ps", bk)])
                    sg = sgf[pi % 2]
                    S.add("act", lambda e, sg=sg, bg=bg: e.activation(out=sg, in_=PS(bg), func=AF.Sigmoid),
                          writes=[("ps", bg), ("sgf", pi % 2)])
                    S.add("dve", lambda e, sg=sg, ba=ba, cc=cc, t=t: e.tensor_tensor(
                        out=ucv[:, cc, 30 + t * 512:30 + (t + 1) * 512], in0=sg, in1=PS(ba), op=ALU.mult),
                        reads=[("sgf", pi % 2)], writes=[("ps", ba), ("ucv", cc)])
            for cc in range(2):
                for j in range(31):
                    S.add("pool", lambda e, cc=cc, j=j: e.tensor_scalar(out=DWc[:, cc, j, :], in0=identb, scalar1=cw[:, cc, j:j + 1],
                                                                        scalar2=None, op0=ALU.mult),
                          reads=["PAR", "identb"], writes=[("DWc", cc)])
            ci = 0
            for cc in range(2):
                for t in range(4):
                    bk = ci % 4
                    ci += 1
                    for j in range(31):
                        S.add("pe", lambda e, cc=cc, t=t, j=j, bk=bk: e.matmul(
                            PS(bk), DWc[:, cc, j, :], ucv[:, cc, j + t * 512:j + t * 512 + 512], start=(j == 0), stop=(j == 30)),
                            reads=[("DWc", cc), ("ucv", cc), "ucvpad"], writes=[("ps", bk)])
                    S.add("act", lambda e, cc=cc, t=t, bk=bk: e.activation(
                        out=yacc[:, cc, t * 512:(t + 1) * 512], in_=PS(bk), func=AF.Identity, bias=par(f"cb{l}")[:, cc:cc + 1], scale=1.0),
                        reads=["PAR"], writes=[("ps", bk), ("yacc", cc)])
            for t in range(4):
                ts_ = slice(t * 512, (t + 1) * 512)
                for cc in range(2):
                    S.add("act", lambda e, cc=cc, ts_=ts_: e.copy(out=ybf[cc], in_=yacc[:, cc, ts_]),
                          reads=[("yacc", cc)], writes=[("ybf", cc)])
                    S.add("pe", lambda e, cc=cc: e.matmul(PS(0), onesb, ybf[cc], start=(cc == 0), stop=(cc == 1)),
                          reads=[("ybf", cc), "onesb"], writes=[("ps", 0)])
                for cc in range(2):
                    S.add("act", lambda e, cc=cc, ts_=ts_: e.activation(out=sq[cc], in_=yacc[:, cc, ts_], func=AF.Square),
                          reads=[("yacc", cc)], writes=[("sq", cc)])
                    S.add("pe", lambda e, cc=cc: e.matmul(PS(1), onesb, sq[cc], start=(cc == 0), stop=(cc == 1)),
                          reads=[("sq", cc), "onesb"], writes=[("ps", 1)])
                S.add("dve", lambda e: e.tensor_scalar(out=mt, in0=PS(0), scalar1=1.0 / 256, scalar2=None, op0=ALU.mult),
                      writes=[("ps", 0), "mt"])
                S.add("dve", lambda e: e.tensor_tensor(out=msq, in0=mt, in1=mt, op=ALU.mult), reads=["mt"], writes=["msq"])
                S.add("dve", lambda e: e.scalar_tensor_tensor(out=msq, in0=PS(1), scalar=1.0 / 256, in1=msq,
                                                              op0=ALU.mult, op1=ALU.subtract),
                      reads=["msq"], writes=[("ps", 1), "msq"])
                S.add("act", lambda e: e.activation(out=rs, in_=msq, func=AF.Sqrt, scale=1.0, bias=EPS),
                      reads=["msq"], writes=["rs"])
                S.add("dve", lambda e: e.reciprocal(out=rstd, in_=rs), reads=["rs"], writes=["rstd"])
                for cc in range(2):
                    S.add("dve", lambda e, cc=cc, ts_=ts_: e.tensor_tensor(out=d1[cc], in0=yacc[:, cc, ts_], in1=mt, op=ALU.subtract),
                          reads=[("yacc", cc), "mt"], writes=[("d1", cc)])
                    S.add("dve", lambda e, cc=cc: e.tensor_tensor(out=d1[cc], in0=d1[cc], in1=rstd, op=ALU.mult),
                          reads=[("d1", cc), "rstd"], writes=[("d1", cc)])
                    S.add("act", lambda e, cc=cc, ts_=ts_: e.activation(
                        out=zs[:, cc, ts_], in_=d1[cc], func=AF.Silu, scale=par(f"clg{l}")[:, cc:cc + 1],
                        bias=par(f"clb{l}")[:, cc:cc + 1]), reads=[("d1", cc), "PAR"], writes=[("zs", t)])
            S.barrier()
            for t in range(4):
                ts_ = slice(t * 512, (t + 1) * 512)
                for oc in range(2):
                    for kc in range(2):
                        S.add("pe", lambda e, oc=oc, kc=kc, ts_=ts_: e.matmul(
                            PS(2 + oc), wpw[:, kc, oc * 128:(oc + 1) * 128], zs[:, kc, ts_], start=(kc == 0), stop=(kc == 1)),
                            reads=["wpw", ("zs", t)], writes=[("ps", 2 + oc)])
                    S.add("act", lambda e, oc=oc: e.copy(out=ycf[:, oc, :], in_=PS(2 + oc)), writes=[("ps", 2 + oc), "ycf"])
                gnorm_tile(ycf, "ycf", 2, par(f"cgn{l}"), mc[:, :, ts_], ("mc", t), 3)
            wout_part(l, s, mc, lambda t: ("mc", t), 2, wo, "wo")
            S.barrier()
            M.off = m0

        def att_part(l, s):
            m0 = M.off
            qT = M.alloc([4, SEQ], BF16)
            kkT = M.alloc([SEQ], BF16)
            kiT = M.alloc([SEQ], BF16)
            qiT = M.alloc([2, SEQ], BF16)
            V1 = M.alloc([16, 66], BF16)
            WI = M.alloc([16, 8], F32)
            wo = M.alloc([4, D], BF16)
            P2 = M.alloc([NIT + 1], F32)
            thr0 = M.alloc([1], F32)
            m1 = M.off
            wr = [M.alloc([8, 256], BF16) for _ in range(2)]
            S.add("pool", lambda e: e.dma_start(out=wo, in_=wout_d[l][:, 0:4, :]), writes=["wo"], dma=True)
            for i in range(NIT + 1):
                S.add("pool", lambda e, i=i: e.memset(P2[:, i:i + 1], 2.0 ** -(i + 1)), writes=["P2"])
            S.add("pool", lambda e: e.memset(thr0, -1e29), writes=["thr0"])
            S.add("pool", lambda e: e.memset(V1[:, :, 64:65], 1.0), writes=["V1one"])
            groups = [(C_Q, 256, [("q", 0), ("q", 1)]), (C_Q + 256, 256, [("q", 2), ("q", 3)]),
                      (C_KK, 256, [("kk", 0), ("ki", 0)]), (C_QI, 256, [("qi", 0), ("qi", 1)]), (C_VW, 72, None)]
            pi = 0
            for gi, (c0, n, dests) in enumerate(groups):
                w = wr[gi % 2]
                S.add("pool", lambda e, w=w, c0=c0, n=n: e.dma_start(out=w[:, :, 0:n], in_=win_d[l][:, :, c0:c0 + n]),
                      writes=[("wr", gi % 2)], dma=True)
                if dests is not None:
                    for ci, (kind, idx) in enumerate(dests):
                        for t in range(4):
                            ts_ = slice(t * 512, (t + 1) * 512)
                            bk = pi % 4
                            pi += 1
                            for k in range(8):
                                S.add("pe", lambda e, w=w, ci=ci, bk=bk, k=k, ts_=ts_: e.matmul(
                                    PS(bk), w[:, k, ci * 128:(ci + 1) * 128], HT[:, k, ts_], start=(k == 0), stop=(k == 7)),
                                    reads=[("wr", gi % 2), ("HT", k, t)], writes=[("ps", bk)])
                            dst = {"q": lambda: qT[:, idx, ts_], "kk": lambda: kkT[:, ts_], "ki": lambda: kiT[:, ts_],
                                   "qi": lambda: qiT[:, idx, ts_]}[kind]()
                            sc_ = 0.125 if kind == "q" else 1.0
                            if pi % 2 == 0:
                                S.add("act", lambda e, dst=dst, bk=bk, sc_=sc_: e.activation(
                                    out=dst, in_=PS(bk), func=AF.Copy, scale=sc_), writes=[("ps", bk), (kind, idx, t)])
                            else:
                                S.add("dve", lambda e, dst=dst, bk=bk, sc_=sc_: e.tensor_scalar(
                                    out=dst, in0=PS(bk), scalar1=sc_, scalar2=None, op0=ALU.mult),
                                    writes=[("ps", bk), (kind, idx, t)])
                else:
                    for i in range(16):
                        bk = pi % 4
                        pi += 1
                        for k in range(8):
                            S.add("pe", lambda e, w=w, bk=bk, k=k, i=i: e.matmul(
                                psum[:, bk, 0:72], HT[:, k, i * 128:(i + 1) * 128], w[:, k, 0:72], start=(k == 0), stop=(k == 7)),
                                reads=[("wr", gi % 2), ("HT", k, i // 4)], writes=[("ps", bk)])
                        S.add("dve", lambda e, bk=bk, i=i: e.tensor_copy(out=V1[:, i, 0:64], in_=psum[:, bk, 0:64]),
                              writes=[("ps", bk), "V1"])
                        S.add("act", lambda e, bk=bk, i=i: e.copy(out=WI[:, i, :], in_=psum[:, bk, 64:72]),
                              writes=[("ps", bk), "WI"])
            S.barrier()
            M.off = m1
            scs = [M.alloc([SEQ], F32) for _ in range(2)]
            rt = [M.alloc([512], BF16) for _ in range(3)]
            DW = [M.alloc([8, 128], BF16) for _ in range(2)]
            negm = [M.alloc([SEQ], BF16) for _ in range(2)]
            ident4 = M.alloc([4, 128], BF16)
            ex = [M.alloc([4, 128], BF16) for _ in range(4)]
            otok = M.alloc([8, 65], F32)
            on = sgf[0].rearrange("p (a b) -> p a b", a=8)
            onsq = tmpf[0]
            onb = rs.bitcast(BF16)[:, 0:512]
            attT = rstd.bitcast(BF16)[:, 0:512].rearrange("p (a b) -> p a b", a=4)
            sm = M.alloc([48], F32)
            mid, cnt, ge, mx, mn, w0, ssq, rq = [sm[:, i:i + 1] for i in range(8)]
            los = [sm[:, 8:9], sm[:, 9:10]]
            Wd = sm[:, 12:12 + NIT + 1]
            rden = M.alloc([8], F32)
            gatt = par(f"agn{l}")
            pb = [psum[:, bkk, :].bitcast(BF16) for bkk in range(8)]
            for i4 in range(4):
                S.add("pool", lambda e, i4=i4: e.tensor_copy(out=ident4[:, i4, :], in_=identb), reads=["identb"], writes=["ident4"])
            cntr = {"ri": 0}

            def stageA(b):
                S_ = 128 * (b + 1)
                qs = slice(128 * b, 128 * b + 128)
                nseg = (S_ + 511) // 512
                tq = b // 4
                nm = negm[b % 2]
                nmk = ("nm", b % 2)
                dw = DW[b % 2]
                sc = scs[b % 2]
                for h in range(8):
                    S.add("pool", lambda e, dw=dw, h=h: e.tensor_scalar(out=dw[:, h, :], in0=identb, scalar1=WI[:, b, h:h + 1],
                                                                       scalar2=None, op0=ALU.mult),
                          reads=["WI", "identb"], writes=[("DW", b % 2)])
                for sg_i in range(nseg):
                    c0, c1 = sg_i * 512, min(S_, sg_i * 512 + 512)
                    n = c1 - c0
                    accb = 2
                    prev = None
                    for h in range(9):
                        if h < 8:
                            ri = cntr["ri"]
                            cntr["ri"] += 1
                            pr = slice(32 * (h % 4), 32 * (h % 4) + 32)
                            bk = ri % 2
                            r_ = rt[ri % 3]
                            rk = ("rt", ri % 3)
                            S.add("pe", lambda e, pr=pr, h=h, bk=bk, c0=c0, c1=c1, n=n: e.matmul(
                                psum[:, bk, 0:n], qiT[pr, h // 4, qs], kiT[pr, c0:c1], start=True, stop=True,
                                tile_position=(32 * (h % 4), 0)),
                                reads=[("qi", h // 4, tq), ("ki", 0, sg_i)], writes=[("ps", bk)])
                            S.add("act", lambda e, r_=r_, bk=bk, n=n: e.activation(out=r_[:, 0:n], in_=psum[:, bk, 0:n], func=AF.Relu),
                                  writes=[("ps", bk), rk])
                        if prev is not None:
                            ph, pr_t, prk = prev
                            S.add("pe", lambda e, ph=ph, pr_t=pr_t, n=n: e.matmul(
                                psum[:, accb, 0:n], dw[:, ph, :], pr_t[:, 0:n], start=(ph == 0), stop=(ph == 7)),
                                reads=[("DW", b % 2), prk], writes=[("ps", accb)])
                        prev = (h, r_, rk) if h < 8 else None
                    S.add("act", lambda e, n=n, c0=c0, c1=c1: e.copy(out=sc[:, c0:c1], in_=psum[:, accb, 0:n]),
                          writes=[("ps", accb), ("sc", b % 2, sg_i)])
                sck = [("sc", b % 2, i) for i in range(nseg)]
                lo = los[b % 2]
                lok = ("lo", b % 2)
                if b >= 2:
                    S.add("dve", lambda e: e.tensor_reduce(out=mx, in_=sc[:, 0:S_], axis=AX.X, op=ALU.max), reads=sck, writes=["mx"])
                    S.add("dve", lambda e: e.tensor_reduce(out=mn, in_=sc[:, 0:S_], axis=AX.X, op=ALU.min), reads=sck, writes=["mn"])
                S.add("dve", lambda e: e.memset(sc[0:64, S_ - 64:S_], -1e30), reads=sck, writes=sck)
                if b >= 2:
                    S.add("dve", lambda e: e.tensor_tensor(out=w0, in0=mx, in1=mn, op=ALU.subtract), reads=["mx", "mn"], writes=["w0"])
                    S.add("dve", lambda e: e.tensor_scalar(out=Wd, in0=P2, scalar1=w0, scalar2=None, op0=ALU.mult),
                          reads=["w0", "P2"], writes=["Wd"])
                    S.add("dve", lambda e: e.tensor_tensor(out=mid, in0=mn, in1=Wd[:, 0:1], op=ALU.add), reads=["mn", "Wd"], writes=["mid"])
                    for i in range(NIT):
                        S.add("dve", lambda e: e.tensor_scalar(
                            out=nm[:, 0:S_], in0=sc[:, 0:S_], scalar1=mid, scalar2=None, op0=ALU.is_ge, op1=ALU.add,
                            accum_out=cnt), reads=sck + ["mid"], writes=[nmk, "cnt"])
                        S.add("dve", lambda e, i=i: e.tensor_scalar(out=ge, in0=cnt, scalar1=255.5, scalar2=Wd[:, i:i + 1],
                                                                    op0=ALU.is_ge, op1=ALU.mult), reads=["cnt", "Wd"], writes=["ge"])
                        S.add("dve", lambda e, i=i: e.scalar_tensor_tensor(
                            out=mid, in0=mid, scalar=Wd[:, i + 1:i + 2], in1=ge, op0=ALU.subtract, op1=ALU.add),
                            reads=["ge", "Wd", "mid"], writes=["mid"])
                    S.add("dve", lambda e: e.tensor_tensor(out=lo, in0=mid, in1=Wd[:, NIT:NIT + 1], op=ALU.subtract),
                          reads=["mid", "Wd"], writes=[lok])
                    thr = lo
                else:
                    thr = thr0
                S.add("dve", lambda e: e.tensor_scalar(
                    out=nm[:, 0:S_], in0=sc[:, 0:S_], scalar1=thr, scalar2=-30000.0, op0=ALU.is_lt, op1=ALU.mult),
                    reads=sck + [lok, "thr0"], writes=[nmk])

            def stageB(b):
                qs = slice(128 * b, 128 * b + 128)
                tq = b // 4
                nm = negm[b % 2]
                nmk = ("nm", b % 2)
                i4f = ident4.rearrange("p a b -> p (a b)")
                for ch in range(b + 1):
                    ks = slice(ch * 128, (ch + 1) * 128)
                    for par_ in range(2):
                        S.add("pe", lambda e, par_=par_, ks=ks: e.matmul(psum[:, 3 + par_, :], nm[:, ks], i4f, start=True, stop=False,
                                                                         skip_group_check=True),
                              reads=[nmk, "ident4"], writes=[("ps", 3 + par_)])
                    for hh in range(4):
                        for par_ in range(2):
                            hp = slice(64 * par_, 64 * par_ + 64)
                            S.add("pe", lambda e, hh=hh, par_=par_, hp=hp, ks=ks: e.matmul(
                                psum[:, 3 + par_, hh * 128:(hh + 1) * 128], kkT[hp, ks], qT[hp, hh, qs], start=False, stop=(hh == 3),
                                skip_group_check=True),
                                reads=[("kk", 0, ch // 4), ("q", hh, tq)], writes=[("ps", 3 + par_)])
                    for par_ in range(2):
                        e_ = ex[2 * (ch % 2) + par_]
                        S.add("act", lambda e, e_=e_, par_=par_: e.activation(
                            out=e_, in_=psum[:, 3 + par_, :].rearrange("p (a b) -> p a b", a=4), func=AF.Exp),
                            writes=[("ps", 3 + par_), ("ex", 2 * (ch % 2) + par_)])
                    for h in range(8):
                        ob = 5 + h // 4
                        S.add("pe", lambda e, h=h, ob=ob, ch=ch: e.matmul(
                            psum[:, ob, (h % 4) * 65:(h % 4) * 65 + 65], ex[2 * (ch % 2) + h % 2][:, h // 2, :], V1[:, ch, 0:65],
                            start=(ch == 0 and h % 4 == 0), stop=(ch == b), skip_group_check=True),
                            reads=[("ex", 2 * (ch % 2) + h % 2), "V1", "V1one"], writes=[("ps", ob)])
                S.add("act", lambda e: e.copy(out=otok[:, 0:4, :], in_=psum[:, 5, 0:260].rearrange("p (a b) -> p a b", a=4)),
                      writes=[("ps", 5), "otokA"])
                S.add("act", lambda e: e.copy(out=otok[:, 4:8, :], in_=psum[:, 6, 0:260].rearrange("p (a b) -> p a b", a=4)),
                      writes=[("ps", 6), "otokB"])
                S.add("dve", lambda e: e.reciprocal(out=rden, in_=otok[:, :, 64]), reads=["otokA", "otokB"], writes=["rden"])
                S.add("dve", lambda e: e.tensor_tensor(out=on, in0=otok[:, :, 0:64], in1=rden.unsqueeze(2).broadcast_to([128, 8, 64]),
                                                       op=ALU.mult), reads=["otokA", "otokB", "rden"], writes=["on"])
                onf = on.rearrange("p a b -> p (a b)")
                S.add("dve", lambda e: e.tensor_tensor(out=onsq, in0=onf, in1=onf, op=ALU.mult), reads=["on"], writes=["onsq"])
                S.add("dve", lambda e: e.tensor_scalar(out=onsq, in0=onsq, scalar1=1.0, scalar2=None, op0=ALU.mult, op1=ALU.add,
                                                       accum_out=ssq), reads=["onsq"], writes=["onsq", "ssq"])
                S.add("act", lambda e: e.activation(out=rq, in_=ssq, func=AF.Sqrt, scale=1.0 / 512, bias=EPS), reads=["ssq"], writes=["rq"])
                S.add("dve", lambda e: e.reciprocal(out=rq, in_=rq), reads=["rq"], writes=["rq"])
                S.add("dve", lambda e: e.tensor_scalar(out=onb, in0=onf, scalar1=rq, scalar2=None, op0=ALU.mult),
                      reads=["on", "rq"], writes=["onb"])
                for c in range(4):
                    S.add("pe", lambda e, c=c: e.transpose(pb[7][:, c * 128:(c + 1) * 128], onb[:, c * 128:(c + 1) * 128], identb),
                          reads=["onb", "identb"], writes=[("ps", 7)])
                for c in range(4):
                    S.add("act", lambda e, c=c: e.activation(out=attT[:, c, :], in_=pb[7][:, c * 128:(c + 1) * 128], func=AF.Copy,
                                                             scale=gatt[:, c:c + 1]),
                          reads=["PAR"], writes=[("ps", 7), "attT"])
                for dc in range(8):
                    bk = 3 + dc // 4
                    for kc in range(4):
                        S.add("pe", lambda e, dc=dc, kc=kc, bk=bk: e.matmul(
                            psum[:, bk, (dc % 4) * 128:(dc % 4 + 1) * 128], wo[:, kc, dc * 128:(dc + 1) * 128], attT[:, kc, :],
                            start=(kc == 0), stop=(kc == 3)), reads=["wo", "attT"], writes=[("ps", bk)])
                for dc in range(8):
                    bk = 3 + dc // 4
                    S.add("dve", lambda e, dc=dc, bk=bk: e.scalar_tensor_tensor(
                        out=XT[:, dc, qs], in0=psum[:, bk, (dc % 4) * 128:(dc % 4 + 1) * 128],
                        scalar=DER[:, l, s, 1, 2, dc:dc + 1], in1=XT[:, dc, qs], op0=ALU.mult, op1=ALU.add),
                        reads=["DER"], writes=[("ps", bk), ("XT", dc, tq)])

            stageA(0)
            for b in range(16):
                if b + 1 < 16:
                    stageA(b + 1)
                stageB(b)
            S.barrier()
            M.off = m0


        def ssm_part(l, s):
            I32 = mybir.dt.int32
            m0 = M.off
            TWO_PI = float(2 * np.pi)
            PI = float(np.pi)
            wssm = M.alloc([8, 256], BF16)
            Abuf = M.alloc([2, 16, 8, 16], BF16)
            U = M.alloc([16, 256], BF16)
            Bq = M.alloc([16, 128], BF16)
            Rm = Bq
            E = M.alloc([16, 16, 16], BF16)
            Bs = M.alloc([16, 128], BF16)
            W = M.alloc([16, 128], BF16)
            wglu = M.alloc([2, 512], BF16)
            wo = M.alloc([2, D], BF16)
            zf = M.alloc([2, 512], F32)
            XR = M.alloc([16, 8], F32)
            YR = M.alloc([16, 8], F32)
            m1 = M.off
            S.add("pool", lambda e: e.dma_start(out=wssm, in_=win_d[l][:, :, C_SSM:C_SSM + 256]), writes=["wssm"], dma=True)
            S.add("pool", lambda e: e.dma_start(out=wglu, in_=wglu_d[l]), writes=["wglu"], dma=True)
            S.add("pool", lambda e: e.dma_start(out=wo, in_=wout_d[l][:, 4:6, :]), writes=["wo"], dma=True)
            NP = 32
            tbase = M.off
            T = {n: M.alloc([16, NP], F32) for n in ("A", "KF", "LT", "MAG", "FR", "FR2")}
            ki_ = M.alloc([16, NP], F32).bitcast(I32)
            sm_ = {n: M.alloc([16], F32) for n in ("dt", "th", "lam", "nr", "den", "t1", "t2", "qr", "qi")}
            big = {n: M.alloc([16, 16], F32) for n in ("Q0", "t1", "t2", "BP", "BQ", "CP", "CQ")}
            tb = {"b1": M.view(tbase, [8, 8, 16], BF16), "b2": M.view(tbase + 4096, [8, 8, 16], BF16)}
            te = {"e1": M.view(tbase, [8, 16, 16], BF16), "e2": M.view(tbase + 4096, [8, 16, 16], BF16)}
            pr_ = lambda n: par(f"s{n}{l}")
            jall = par("sjall")
            sgnA, sgnC = par("ssgnA"), par("ssgnC")

            def V(eng, fn, r, w):
                S.add(eng, fn, reads=r, writes=w)

            def b3(ap2, n):
                return ap2.unsqueeze(2).broadcast_to([128, 16, n])

            def fl(ap3):
                return ap3.rearrange("p a b -> p (a b)")

            V("act", lambda e: e.activation(out=sm_["dt"], in_=pr_("ldt"), func=AF.Exp), ["PAR"], ["dt"])
            V("dve", lambda e: e.tensor_tensor(out=sm_["th"], in0=sm_["dt"], in1=pr_("aim"), op=ALU.mult), ["dt", "PAR"], ["th"])
            V("dve", lambda e: e.tensor_tensor(out=sm_["lam"], in0=sm_["dt"], in1=pr_("are"), op=ALU.mult), ["dt", "PAR"], ["lam"])
            jb = jall.unsqueeze(1).broadcast_to([128, 16, NP])
            V("dve", lambda e: e.tensor_tensor(out=T["A"], in0=b3(sm_["th"], NP), in1=jb, op=ALU.mult), ["th", "PAR"], ["A"])
            V("dve", lambda e: e.tensor_tensor(out=T["MAG"], in0=b3(sm_["lam"], NP), in1=jb, op=ALU.mult), ["lam", "PAR"], ["MAG"])
            V("act", lambda e: e.activation(out=T["MAG"], in_=T["MAG"], func=AF.Exp), ["MAG"], ["MAG"])
            V("dve", lambda e: e.tensor_scalar(out=T["A"], in0=T["A"], scalar1=1.0 / TWO_PI, scalar2=64.0, op0=ALU.mult, op1=ALU.add), ["A"], ["A"])
            V("dve", lambda e: e.tensor_copy(out=ki_, in_=T["A"]), ["A"], ["ki"])
            V("dve", lambda e: e.tensor_copy(out=T["KF"], in_=ki_), ["ki"], ["KF"])
            V("dve", lambda e: e.tensor_tensor(out=T["FR"], in0=T["A"], in1=T["KF"], op=ALU.subtract), ["A", "KF"], ["FR"])
            V("dve", lambda e: e.tensor_scalar(out=T["LT"], in0=T["FR"], scalar1=0.0, scalar2=None, op0=ALU.is_lt), ["FR"], ["LT"])
            V("dve", lambda e: e.tensor_tensor(out=T["FR"], in0=T["FR"], in1=T["LT"], op=ALU.add), ["FR", "LT"], ["FR"])
            V("dve", lambda e: e.tensor_scalar(out=T["FR2"], in0=T["FR"], scalar1=0.25, scalar2=None, op0=ALU.add), ["FR"], ["FR2"])
            V("dve", lambda e: e.tensor_scalar(out=T["LT"], in0=T["FR2"], scalar1=1.0, scalar2=None, op0=ALU.is_ge), ["FR2"], ["LT"])
            V("dve", lambda e: e.tensor_tensor(out=T["FR2"], in0=T["FR2"], in1=T["LT"], op=ALU.subtract), ["FR2", "LT"], ["FR2"])
            for nm in ("FR", "FR2"):
                V("dve", lambda e, nm=nm: e.tensor_scalar(out=T[nm], in0=T[nm], scalar1=TWO_PI, scalar2=-PI, op0=ALU.mult, op1=ALU.add), [nm], [nm])
                V("dve", lambda e, nm=nm: e.tensor_scalar(out=T[nm], in0=T[nm], scalar1=-3.1415925, scalar2=3.1415925, op0=ALU.max, op1=ALU.min), [nm], [nm])
                V("act", lambda e, nm=nm: e.activation(out=T[nm], in_=T[nm], func=AF.Sin), [nm], [nm])
                V("dve", lambda e, nm=nm: e.scalar_tensor_tensor(out=fl(T[nm]), in0=fl(T[nm]), scalar=-1.0, in1=fl(T["MAG"]),
                                                                 op0=ALU.mult, op1=ALU.mult), [nm, "MAG"], [nm])
            XA, YA = T["FR2"], T["FR"]
            XAk, YAk = "FR2", "FR"
            X1, Y1 = XA[:, :, 8], YA[:, :, 8]
            V("dve", lambda e: e.tensor_scalar(out=sm_["nr"], in0=X1, scalar1=-1.0, scalar2=None, op0=ALU.add), [XAk], ["nr"])
            V("dve", lambda e: e.tensor_tensor(out=sm_["den"], in0=pr_("are"), in1=pr_("are"), op=ALU.mult), ["PAR"], ["den"])
            V("dve", lambda e: e.tensor_tensor(out=sm_["t1"], in0=pr_("aim"), in1=pr_("aim"), op=ALU.mult), ["PAR"], ["st1"])
            V("dve", lambda e: e.tensor_tensor(out=sm_["den"], in0=sm_["den"], in1=sm_["t1"], op=ALU.add), ["den", "st1"], ["den"])
            V("dve", lambda e: e.reciprocal(out=sm_["den"], in_=sm_["den"]), ["den"], ["den"])
            V("dve", lambda e: e.tensor_tensor(out=sm_["t1"], in0=sm_["nr"], in1=pr_("are"), op=ALU.mult), ["nr", "PAR"], ["st1"])
            V("dve", lambda e: e.tensor_tensor(out=sm_["t2"], in0=Y1, in1=pr_("aim"), op=ALU.mult), [YAk, "PAR"], ["st2"])
            V("dve", lambda e: e.tensor_tensor(out=sm_["t1"], in0=sm_["t1"], in1=sm_["t2"], op=ALU.add), ["st1", "st2"], ["st1"])
            V("dve", lambda e: e.tensor_tensor(out=sm_["qr"], in0=sm_["t1"], in1=sm_["den"], op=ALU.mult), ["st1", "den"], ["qr"])
            V("dve", lambda e: e.tensor_tensor(out=sm_["t1"], in0=Y1, in1=pr_("are"), op=ALU.mult), [YAk, "PAR"], ["st1"])
            V("dve", lambda e: e.tensor_tensor(out=sm_["t2"], in0=sm_["nr"], in1=pr_("aim"), op=ALU.mult), ["nr", "PAR"], ["st2"])
            V("dve", lambda e: e.tensor_tensor(out=sm_["t1"], in0=sm_["t1"], in1=sm_["t2"], op=ALU.subtract), ["st1", "st2"], ["st1"])
            V("dve", lambda e: e.tensor_tensor(out=sm_["qi"], in0=sm_["t1"], in1=sm_["den"], op=ALU.mult), ["st1", "den"], ["qi"])
            bp3 = pr_("bp").rearrange("p (a b) -> p a b", a=16)
            bq3 = pr_("bq").rearrange("p (a b) -> p a b", a=16)
            V("dve", lambda e: e.tensor_scalar(out=big["Q0"], in0=bq3, scalar1=sgnA, scalar2=None, op0=ALU.mult), ["PAR"], ["Q0"])
            V("dve", lambda e: e.tensor_tensor(out=big["t1"], in0=bp3, in1=b3(sm_["qr"], 16), op=ALU.mult), ["PAR", "qr"], ["bt1"])
            V("dve", lambda e: e.tensor_tensor(out=big["t2"], in0=big["Q0"], in1=b3(sm_["qi"], 16), op=ALU.mult), ["Q0", "qi"], ["bt2"])
            V("dve", lambda e: e.tensor_tensor(out=big["BP"], in0=big["t1"], in1=big["t2"], op=ALU.add), ["bt1", "bt2"], ["BP"])
            V("dve", lambda e: e.tensor_tensor(out=big["t1"], in0=big["Q0"], in1=b3(sm_["qr"], 16), op=ALU.mult), ["Q0", "qr"], ["bt1"])
            V("dve", lambda e: e.tensor_tensor(out=big["t2"], in0=bp3, in1=b3(sm_["qi"], 16), op=ALU.mult), ["PAR", "qi"], ["bt2"])
            V("dve", lambda e: e.tensor_tensor(out=big["BQ"], in0=big["t1"], in1=big["t2"], op=ALU.subtract), ["bt1", "bt2"], ["BQ"])
            Bq4 = Bq.rearrange("p g (a b) -> p g a b", a=8)
            for gh in range(2):
                gs = slice(8 * gh, 8 * gh + 8)
                xb_ = XA[:, gs, 16:24].unsqueeze(3).broadcast_to([128, 8, 8, 16])
                yb_ = YA[:, gs, 16:24].unsqueeze(3).broadcast_to([128, 8, 8, 16])
                bpg = big["BP"][:, gs, :].unsqueeze(2).broadcast_to([128, 8, 8, 16])
                bqg = big["BQ"][:, gs, :].unsqueeze(2).broadcast_to([128, 8, 8, 16])
                V("dve", lambda e, xb_=xb_, bpg=bpg: e.tensor_tensor(out=tb["b1"], in0=bpg, in1=xb_, op=ALU.mult), ["BP", XAk, "Bq", "MAG", "A", "KF", "LT"], ["b1"])
                V("dve", lambda e, yb_=yb_, bqg=bqg: e.tensor_tensor(out=tb["b2"], in0=bqg, in1=yb_, op=ALU.mult), ["BQ", YAk, "Bq", "MAG", "A", "KF", "LT"], ["b2"])
                V("dve", lambda e, gs=gs: e.tensor_tensor(out=Bq4[:, gs], in0=tb["b1"], in1=tb["b2"], op=ALU.add), ["b1", "b2"], ["Bq"])
            cp3 = pr_("cp").rearrange("p (a b) -> p a b", a=16)
            cq3 = pr_("cq").rearrange("p (a b) -> p a b", a=16)
            V("dve", lambda e: e.tensor_scalar(out=big["CP"], in0=cp3, scalar1=sgnC, scalar2=None, op0=ALU.mult), ["PAR"], ["CP"])
            V("dve", lambda e: e.tensor_scalar(out=big["CQ"], in0=cq3, scalar1=-1.0, scalar2=None, op0=ALU.mult), ["PAR"], ["CQ"])
            for gh in range(2):
                gs = slice(8 * gh, 8 * gh + 8)
                xe_ = XA[:, gs, 0:16].unsqueeze(3).broadcast_to([128, 8, 16, 16])
                ye_ = YA[:, gs, 0:16].unsqueeze(3).broadcast_to([128, 8, 16, 16])
                cpg = big["CP"][:, gs, :].unsqueeze(2).broadcast_to([128, 8, 16, 16])
                cqg = big["CQ"][:, gs, :].unsqueeze(2).broadcast_to([128, 8, 16, 16])
                V("dve", lambda e, xe_=xe_, cpg=cpg: e.tensor_tensor(out=te["e1"], in0=cpg, in1=xe_, op=ALU.mult), ["CP", XAk, "E", "b1", "b2", "Bq"], ["e1"])
                V("dve", lambda e, ye_=ye_, cqg=cqg: e.tensor_tensor(out=te["e2"], in0=cqg, in1=ye_, op=ALU.mult), ["CQ", YAk, "E", "b1", "b2", "Bq"], ["e2"])
                V("dve", lambda e, gs=gs: e.tensor_tensor(out=E[:, gs], in0=te["e1"], in1=te["e2"], op=ALU.add), ["e1", "e2"], ["E"])
            V("dve", lambda e: e.tensor_copy(out=XR, in_=XA[:, :, 24:32]), [XAk], ["XR"])
            V("dve", lambda e: e.tensor_scalar(out=YR, in0=YA[:, :, 24:32], scalar1=sgnC, scalar2=None, op0=ALU.mult), [YAk, "PAR"], ["YR"])
            pb = [psum[:, bkk, :].bitcast(BF16) for bkk in range(8)]
            for g in range(16):
                bk = g // 8
                V("pe", lambda e, g=g, bk=bk: e.transpose(pb[bk][:, (g % 8) * 128:(g % 8 + 1) * 128], Bq[:, g, :], identb), ["Bq", "identb"], [("ps", bk)])
            for bk in range(2):
                V("act", lambda e, bk=bk: e.copy(out=Bs[:, 8 * bk:8 * bk + 8, :], in_=pb[bk].rearrange("p (a b) -> p a b", a=8)), [], [("ps", bk), "Bs"])
            tmk = par("stm")
            for g in range(16):
                bk = 2 + g // 4
                V("pe", lambda e, g=g, bk=bk: e.matmul(psum[:, bk, (g % 4) * 128:(g % 4 + 1) * 128], Bq[:, g, :],
                                                        E[:, g, 0:8, :].rearrange("p a b -> p (a b)"), start=True, stop=True), ["Bq", "E"], [("ps", bk)])
            for q4 in range(4):
                bk = 2 + q4
                V("dve", lambda e, q4=q4, bk=bk: e.tensor_tensor(
                    out=W[:, 4 * q4:4 * q4 + 4, :], in0=psum[:, bk, :].rearrange("p (a b) -> p a b", a=4),
                    in1=tmk.unsqueeze(1).broadcast_to([128, 4, 128]), op=ALU.mult), ["PAR"], [("ps", bk), "W"])
            for g in range(16):
                V("dve", lambda e, g=g: e.scalar_tensor_tensor(out=W[:, g, :], in0=identf, scalar=pr_("dcol")[:, g:g + 1], in1=W[:, g, :],
                                                              op0=ALU.mult, op1=ALU.add), ["W", "PAR", "identf"], ["W"])
            S.barrier()
            M.off = m1
            X32 = M.alloc([16, 257], F32)
            Xb = M.alloc([16, 257], BF16)
            for half in range(2):
                for sp_ in range(4):
                    bk = 4 * (half % 2) + sp_
                    for sg2 in range(2):
                        sig = 2 * sp_ + sg2
                        for k in range(8):
                            V("pe", lambda e, half=half, sig=sig, sg2=sg2, bk=bk, k=k: e.matmul(
                                psum[:, bk, sg2 * 256:(sg2 + 1) * 256], HT[:, k, 1024 * half + sig:1024 * (half + 1):8], wssm[:, k, :],
                                start=(k == 0), stop=(k == 7)), ["wssm", ("HT", k, 2 * half), ("HT", k, 2 * half + 1)], [("ps", bk)])
                    for sg2 in range(2):
                        sig = 2 * sp_ + sg2
                        src = psum[:, bk, sg2 * 256:(sg2 + 1) * 256].rearrange("p (a b) -> p a b", a=16)
                        dst = Abuf[:, half, :, sig, :]
                        if sg2 == 0:
                            V("act", lambda e, src=src, dst=dst: e.copy(out=dst, in_=src), [], [("ps", bk), ("Abuf", half)])
                        else:
                            V("dve", lambda e, src=src, dst=dst: e.tensor_copy(out=dst, in_=src), [], [("ps", bk), ("Abuf", half)])
            for half in range(2):
                for gh in range(2):
                    bk = 2 * half + gh
                    for gg in range(8):
                        g = 8 * gh + gg
                        V("pe", lambda e, half=half, g=g, gg=gg, bk=bk: e.transpose(
                            pb[bk][:, gg * 128:(gg + 1) * 128], Abuf[:, half, g].rearrange("p a b -> p (a b)"), identb),
                            [("Abuf", half), "identb"], [("ps", bk)])
                    dst = U[:, 8 * gh:8 * gh + 8, 128 * half:128 * (half + 1)]
                    src = pb[bk].rearrange("p (a b) -> p a b", a=8)
                    if gh == 0:
                        V("act", lambda e, src=src, dst=dst: e.copy(out=dst, in_=src), [], [("ps", bk), "U"])
                    else:
                        V("dve", lambda e, src=src, dst=dst: e.tensor_copy(out=dst, in_=src), [], [("ps", bk), "U"])
            V("dve", lambda e: e.memset(X32[:, :, 0:1], 0.0), [], ["X32z"])
            for g in range(16):
                bk = g // 2
                V("pe", lambda e, g=g, bk=bk: e.matmul(psum[:, bk, (g % 2) * 256:(g % 2 + 1) * 256], Bs[:, g, :], U[:, g, :], start=True, stop=True),
                  ["Bs", "U"], [("ps", bk)])
            for g2 in range(8):
                dst = X32[:, 2 * g2:2 * g2 + 2, 1:257]
                src = psum[:, g2, :].rearrange("p (a b) -> p a b", a=2)
                if g2 % 2 == 0:
                    V("act", lambda e, src=src, dst=dst: e.copy(out=dst, in_=src), [], [("ps", g2), "X32"])
                else:
                    V("dve", lambda e, src=src, dst=dst: e.tensor_copy(out=dst, in_=src), [], [("ps", g2), "X32"])
            V("act", lambda e: e.copy(out=Xb, in_=X32), ["X32", "X32z"], ["Xb"])
            swp = par("sswap")
            for lv in range(8):
                sh = 1 << lv
                n = 256 - sh
                for ph in range(2):
                    for chh in range(2):
                        pp = slice(64 * ph, 64 * ph + 64)
                        cs = slice(64 * chh, 64 * chh + 64)
                        src = identf if ph == chh else swp
                        coef = XR if ph == chh else YR
                        V("dve", lambda e, pp=pp, cs=cs, src=src, coef=coef, lv=lv: e.tensor_tensor(
                            out=Rm[pp, :, cs], in0=src[pp, cs].unsqueeze(1).broadcast_to([64, 16, 64]),
                            in1=coef[pp, :, lv:lv + 1].broadcast_to([64, 16, 64]), op=ALU.mult),
                            ["XR", "YR", "identf", "PAR"], ["Rm"])
                for g in range(16):
                    bk = g // 2
                    V("pe", lambda e, g=g, bk=bk, n=n: e.matmul(psum[:, bk, (g % 2) * 256:(g % 2) * 256 + n], Rm[:, g, :], Xb[:, g, 1:1 + n],
                                                                 start=True, stop=True), ["Rm", "Xb"], [("ps", bk)])
                for g2 in range(8):
                    V("dve", lambda e, g2=g2, n=n, sh=sh: e.tensor_tensor(
                        out=X32[:, 2 * g2:2 * g2 + 2, 1 + sh:257], in0=X32[:, 2 * g2:2 * g2 + 2, 1 + sh:257],
                        in1=psum[:, g2, :].rearrange("p (a b) -> p a b", a=2)[:, :, 0:n], op=ALU.add), ["X32"], [("ps", g2), "X32"])
                V("act", lambda e: e.copy(out=Xb, in_=X32), ["X32", "X32z"], ["Xb"])
            S.barrier()
            Ytok = Abuf.rearrange("p h g a b -> p h (g a b)").rearrange("p h (t c) -> p h t c", t=8)
            for half in range(2):
                for q4 in range(4):
                    bk = 4 * (half % 2) + q4
                    for gg in range(4):
                        g = 4 * q4 + gg
                        V("pe", lambda e, half=half, g=g, gg=gg, bk=bk: e.matmul(
                            psum[:, bk, gg * 128:(gg + 1) * 128], Xb[:, g, 128 * half:128 * half + 128],
                            E[:, g, 8:16, :].rearrange("p a b -> p (a b)"), start=True, stop=False), ["Xb", "E"], [("ps", bk)])
                        V("pe", lambda e, half=half, g=g, gg=gg, bk=bk: e.matmul(
                            psum[:, bk, gg * 128:(gg + 1) * 128], U[:, g, 128 * half:128 * half + 128], W[:, g, :],
                            start=False, stop=True), ["U", "W"], [("ps", bk)])
                    src = psum[:, bk, :].rearrange("p (g t c) -> p g t c", g=4, t=8)
                    dst = Ytok[:, half, :, 64 * q4:64 * q4 + 64].rearrange("p t (g c) -> p g t c", g=4)
                    V("act", lambda e, src=src, dst=dst: e.activation(out=dst, in_=src, func=AF.Gelu), [], [("ps", bk), ("Ytok", half)])
            S.barrier()
            M.off = m1
            YT = M.alloc([2, SEQ], BF16)
            for half in range(2):
                for cc in range(2):
                    bk = 2 * half + cc
                    for tau in range(8):
                        V("pe", lambda e, half=half, cc=cc, tau=tau, bk=bk: e.transpose(
                            pb[bk][:, tau * 128:(tau + 1) * 128], Ytok[:, half, tau, cc * 128:(cc + 1) * 128], identb),
                            [("Ytok", half), "identb"], [("ps", bk)])
                    dst = YT[:, cc, 1024 * half:1024 * (half + 1)].rearrange("p (k t) -> p t k", t=8)
                    src = pb[bk].rearrange("p (t k) -> p t k", t=8)
                    if cc == 0:
                        V("act", lambda e, src=src, dst=dst: e.copy(out=dst, in_=src), [], [("ps", bk), ("YT", half)])
                    else:
                        V("dve", lambda e, src=src, dst=dst: e.tensor_copy(out=dst, in_=src), [], [("ps", bk), ("YT", half)])
            ms = U.rearrange("p a b -> p (a b)").rearrange("p (c n) -> p c n", c=2)
            S.barrier()
            for t in range(4):
                ts_ = slice(t * 512, (t + 1) * 512)
                for oc in range(4):
                    for kc in range(2):
                        V("pe", lambda e, oc=oc, kc=kc, ts_=ts_: e.matmul(PS(oc), wglu[:, kc, oc * 128:(oc + 1) * 128], YT[:, kc, ts_],
                                                                         start=(kc == 0), stop=(kc == 1)), ["wglu", ("YT", t // 2)], [("ps", oc)])
                for cc in range(2):
                    sg = sgf[cc]
                    V("act", lambda e, sg=sg, cc=cc: e.activation(out=sg, in_=PS(2 + cc), func=AF.Sigmoid), [], [("ps", 2 + cc), ("sgf", cc)])
                    V("dve", lambda e, sg=sg, cc=cc: e.tensor_tensor(out=zf[:, cc, :], in0=sg, in1=PS(cc), op=ALU.mult),
                      [("sgf", cc)], [("ps", cc), "zf"])
                gnorm_tile(zf, "zf", 2, par(f"sgn{l}"), ms[:, :, ts_], ("ms", t), 6)
            wout_part(l, s, ms, lambda t: ("ms", t), 2, wo, "wo")
            S.barrier()
            M.off = m0

        def mixer(l, s):
            normmod(l, s, 1)
            if "conv" in MIXPARTS:
                conv_part(l, s)
            if "ssm" in MIXPARTS:
                ssm_part(l, s)
            if "att" in MIXPARTS:
                att_part(l, s)

        for s in range(2):
            load_x(s)
            for (l, kind) in phases:
                if kind == "ffn1":
                    ffn(l, s, 1)
                elif kind == "ffn2":
                    ffn(l, s, 2)
                else:
                    mixer(l, s)
            store_x(s, last)
        S.finish()
        nsem = S.emit(st)
        print(f"[build] ops={len(S.ops)} sems={nsem}")
    return nc


MIXPARTS = {"conv", "att", "ssm"}


ALL_PHASES = [(l, k) for l in range(2) for k in ("ffn1", "mix", "ffn2")]


def prep_weights(inp):
    w = {}
    for j, nm in ((1, "ffn1"), (2, "ffn2")):
        gu = inp[f"{nm}_w_gu"]
        g = gu[:, :, :DFF].reshape(2, 8, 128, NF, 128)
        u = gu[:, :, DFF:].reshape(2, 8, 128, NF, 128)
        cat = np.concatenate([g, u], axis=-1)
        w[f"wgu{j}"] = np.ascontiguousarray(cat.transpose(0, 3, 2, 1, 4)).reshape(2, NF, 128, 2048)
        w[f"wdn{j}"] = np.ascontiguousarray(inp[f"{nm}_w_down"]).reshape(2, NF, 128, D)
    w["mod_w"] = np.ascontiguousarray(inp["mod_w"])
    for l in range(2):
        wi = inp["w_in"][l]
        q, k, v, qi, ki, wv, ssm, ca, cg = (wi[:, 0:512], wi[:, 512:576], wi[:, 576:640], wi[:, 640:896], wi[:, 896:928],
                                            wi[:, 928:936], wi[:, 936:1192], wi[:, 1192:1448], wi[:, 1448:1704])
        cat = np.concatenate([q, k, k, ki, ki, ki, ki, qi, ca, cg, v, wv, ssm], axis=1)
        assert cat.shape[1] == NWIN
        w[f"win{l}"] = np.ascontiguousarray(cat.reshape(8, 128, NWIN).transpose(1, 0, 2))
        w[f"wout{l}"] = np.ascontiguousarray(inp["w_out"][l].reshape(8, 128, D).transpose(1, 0, 2))
        w[f"wpw{l}"] = np.ascontiguousarray(inp["conv_w_pw"][l].reshape(2, 128, 256).transpose(1, 0, 2))
        w[f"wglu{l}"] = np.ascontiguousarray(inp["ssm_w_glu"][l].reshape(2, 128, 512).transpose(1, 0, 2))
    return w


def run_phases(inp, xcur, phases, first, last, shared=None):
    shared = shared if shared is not None else prep_weights(inp)
    packs = [pack_small(inp, c) for c in range(8)]
    nc = build(phases, first, last, packs[0].off, packs[0].n)
    in_maps = []
    need = {"mod_w"}
    for l, k in phases:
        if k == "ffn1":
            need |= {"wgu1", "wdn1"}
        elif k == "ffn2":
            need |= {"wgu2", "wdn2"}
        else:
            need |= {f"win{l}", f"wout{l}", f"wpw{l}", f"wglu{l}"}
    shared = {k: v for k, v in shared.items() if k in need}
    for c in range(8):
        m = dict(shared)
        m["x"] = np.ascontiguousarray(xcur[2 * c:2 * c + 2])
        m["par"] = packs[c].array()
        in_maps.append(m)
    res = run_bass_kernel_spmd(nc, in_maps, core_ids=list(range(8)))
    return np.concatenate([r["y"] for r in res.results], axis=0)


def kernel(**inputs):
    inp = {k: np.asarray(v) for k, v in inputs.items()}
    x = np.ascontiguousarray(inp["x"], dtype=np.float32)
    return run_phases(inp, x, ALL_PHASES, True, True)
```

```python
import numpy as np
from contextlib import ExitStack
import concourse.bass as bass
import concourse.mybir as mybir
from concourse.bass_utils import run_bass_kernel_spmd

F32 = mybir.dt.float32
BF16 = mybir.dt.bfloat16
ALU = mybir.AluOpType
AF = mybir.ActivationFunctionType
AX = mybir.AxisListType

ROT = 2048
DMA_SLOTS = 12
DMA_ROT = 120

D = 1024
SEQ = 2048
DFF = 2816
NF = 22
NWIN = 1864
C_Q, C_KK, C_KI, C_QI, C_A, C_G, C_VW, C_SSM = 0, 512, 640, 768, 1024, 1280, 1536, 1608
EPS = 1e-6


class Op:
    __slots__ = ("eng", "fn", "reads", "writes", "dma", "deps", "inc", "cnt",
                 "dslot", "dgen", "dval", "waits", "clock", "idx")


class Sched:
    def __init__(self, nc):
        self.nc = nc
        self.ops = []
        self.last_w = {}
        self.readers = {}
        self.dma_n = 0
        self.dma_prev = {}

    def add(self, eng, fn, reads=(), writes=(), dma=False):
        op = Op()
        op.eng, op.fn, op.dma = eng, fn, dma
        op.reads, op.writes = tuple(reads), tuple(writes)
        op.inc = False
        op.idx = len(self.ops)
        deps = {}
        for k in op.reads:
            w = self.last_w.get(k)
            if w is not None:
                deps[w.idx] = (w, True)
        for k in op.writes:
            w = self.last_w.get(k)
            if w is not None and w.idx not in deps:
                deps[w.idx] = (w, False)
            for r in self.readers.get(k, ()):
                if r.idx not in deps:
                    deps[r.idx] = (r, False)
        if dma:
            slot = self.dma_n % DMA_SLOTS
            self.dma_n += 1
            prev = self.dma_prev.get(slot)
            if prev is not None and prev.idx not in deps:
                deps[prev.idx] = (prev, True)
            self.dma_prev[slot] = op
            op.dslot = slot
            op.inc = True
        need = []
        for d, raw in deps.values():
            if d is op:
                continue
            if d.dma or op.dma or d.eng != eng:
                need.append(d)
            elif raw and eng != "pe":
                need.append(d)
        for d in need:
            d.inc = True
        op.deps = need
        for k in op.reads:
            self.readers.setdefault(k, []).append(op)
        for k in op.writes:
            self.last_w[k] = op
            self.readers[k] = []
        self.ops.append(op)
        return op

    def barrier(self):
        keys = set(self.last_w.keys()) | set(self.readers.keys())
        keys.discard("BAR")
        self.add("dve", lambda e: e.nop(), reads=list(keys), writes=["BAR"])
        for en in ("pe", "act", "pool", "sp"):
            self.add(en, lambda e: e.nop(), reads=["BAR"], writes=[("BARL", en)])
        bar = self.last_w["BAR"]
        self.last_w = {"BAR": bar}
        self.readers = {}
        self._bar = bar

    def finish(self, eng="sp"):
        keys = [k for k, w in self.last_w.items() if w.dma]
        self.add(eng, lambda e: e.nop(), reads=keys)

    def emit(self, stack):
        nc = self.nc
        engs = ["pe", "act", "dve", "pool", "sp"]
        cnt = {e: 0 for e in engs}
        dcount = {}
        for op in self.ops:
            if op.dma:
                g = dcount.get(op.dslot, 0)
                op.dgen, op.dval = g // DMA_ROT, 16 * (g % DMA_ROT + 1)
                dcount[op.dslot] = g + 1
            elif op.inc:
                cnt[op.eng] += 1
                op.cnt = cnt[op.eng]
        sems = {}

        def sem(name):
            if name not in sems:
                sems[name] = stack.enter_context(nc.semaphore(name))
            return sems[name]

        known = {e: {} for e in engs}
        for op in self.ops:
            kn = known[op.eng]
            waits = []
            for d in sorted(op.deps, key=lambda o: o.idx):
                if d.dma:
                    key = ("d", d.dslot, d.dgen)
                    val = d.dval
                else:
                    key = d.eng
                    val = d.cnt
                if kn.get(key, 0) >= val:
                    continue
                waits.append(d)
                for k2, v2 in d.clock.items():
                    if kn.get(k2, 0) < v2:
                        kn[k2] = v2
                kn[key] = val
            op.waits = waits
            if op.inc:
                op.clock = dict(kn)
                if op.dma:
                    op.clock[("d", op.dslot, op.dgen)] = op.dval
                else:
                    op.clock[op.eng] = op.cnt
            else:
                op.clock = None

        for op in self.ops:
            if op.inc:
                if op.dma:
                    sem(f"d{op.dslot}_{op.dgen}")
                else:
                    sem(f"{op.eng}_{(op.cnt - 1) // ROT}")

        def emit_engine(ename, e):
            for op in self.ops:
                if op.eng != ename:
                    continue
                for d in op.waits:
                    if d.dma:
                        e.wait_ge(sem(f"d{d.dslot}_{d.dgen}"), d.dval)
                    else:
                        c = d.cnt - 1
                        e.wait_ge(sem(f"{d.eng}_{c // ROT}"), c % ROT + 1)
                ins = op.fn(e)
                if op.inc:
                    if op.dma:
                        ins.then_inc(sem(f"d{op.dslot}_{op.dgen}"), 16)
                    else:
                        c = op.cnt - 1
                        ins.then_inc(sem(f"{op.eng}_{c // ROT}"), 1)

        block = stack.enter_context(nc.Block())

        @block.tensor
        def _(e):
            emit_engine("pe", e)

        @block.scalar
        def _(e):
            emit_engine("act", e)

        @block.vector
        def _(e):
            emit_engine("dve", e)

        @block.gpsimd
        def _(e):
            emit_engine("pool", e)

        @block.sync
        def _(e):
            emit_engine("sp", e)
        return len(sems)


class Mem:
    def __init__(self, pool, nbytes):
        self.pool, self.nbytes, self.off = pool, nbytes, 0

    def view(self, off, shape, dtype):
        o = self.off
        self.off = off
        v = self.alloc(shape, dtype)
        self.off = o
        return v

    def alloc(self, shape, dtype):
        isz = 4 if dtype == F32 else 2
        n = int(np.prod(shape))
        nb = (n * isz + 63) // 64 * 64
        assert self.off + nb <= self.nbytes, (self.off, nb, self.nbytes)
        v = self.pool[:, self.off // 2:(self.off + n * isz) // 2]
        self.off += nb
        if dtype == F32:
            v = v.bitcast(F32)
        if len(shape) > 1:
            names = [f"a{i}" for i in range(len(shape))]
            kw = {n: int(d) for n, d in zip(names[:-1], shape[:-1])}
            v = v.rearrange(f"p ({' '.join(names)}) -> p {' '.join(names)}", **kw)
        return v


def _fm(v, nch):
    return np.ascontiguousarray(v.reshape(nch, 128).T)


class Pack:
    def __init__(self):
        self.cols, self.off, self.n = [], {}, 0

    def add(self, name, arr):
        arr = np.asarray(arr, np.float32).reshape(128, -1)
        self.off[name] = (self.n, arr.shape[1])
        self.cols.append(arr)
        self.n += arr.shape[1]

    def array(self):
        return np.ascontiguousarray(np.concatenate(self.cols, axis=1))


def pack_small(inp, core):
    P = Pack()
    b0 = 2 * core
    P.add("cT", np.stack([_fm(inp["c"][b0 + s], 8) for s in range(2)], axis=-1))
    for l in range(2):
        P.add(f"modb{l}", _fm(inp["mod_b"][l], 72))
        for j, nm in enumerate(("ffn1_norm", "mix_norm", "ffn2_norm")):
            P.add(f"ng{l}{j}", _fm(inp[nm][l], 8))
    P.add("fin", _fm(inp["final_norm"], 8))
    for l in range(2):
        P.add(f"cw{l}", inp["conv_w_dw"][l].T.reshape(2, 128, 31).transpose(1, 0, 2))
        P.add(f"cb{l}", _fm(inp["conv_b_dw"][l], 2))
        P.add(f"clg{l}", _fm(inp["conv_ln_g"][l], 2))
        P.add(f"clb{l}", _fm(inp["conv_ln_b"][l], 2))
        P.add(f"cgn{l}", _fm(inp["conv_out_norm"][l], 2))
        P.add(f"sgn{l}", _fm(inp["ssm_out_norm"][l], 2))
        P.add(f"agn{l}", _fm(inp["att_out_norm"][l], 4))
        dup = lambda a: np.concatenate([a, a], axis=0)
        P.add(f"sare{l}", dup(inp["ssm_a_re"][l].T))
        P.add(f"saim{l}", dup(inp["ssm_a_im"][l].T))
        P.add(f"sldt{l}", np.broadcast_to(inp["ssm_log_dt"][l][None, :], (128, 16)))
        bre = inp["ssm_b_re"][l].transpose(1, 0, 2).reshape(64, 256)
        bim = inp["ssm_b_im"][l].transpose(1, 0, 2).reshape(64, 256)
        P.add(f"sbp{l}", np.concatenate([bre, bim], axis=0))
        P.add(f"sbq{l}", np.concatenate([bim, bre], axis=0))
        cre = inp["ssm_c_re"][l].transpose(2, 0, 1).reshape(64, 256)
        cim = inp["ssm_c_im"][l].transpose(2, 0, 1).reshape(64, 256)
        P.add(f"scp{l}", np.concatenate([cre, cim], axis=0))
        P.add(f"scq{l}", np.concatenate([cim, cre], axis=0))
        P.add(f"sdcol{l}", np.tile(inp["ssm_d"][l].T, (8, 1)))
    jall = np.concatenate([np.arange(-7, 9), np.arange(7, -1, -1), 8 * 2 ** np.arange(8)]).astype(np.float32)
    P.add("sjall", np.broadcast_to(jall[None, :], (128, 32)))
    sg = np.ones((128, 1), np.float32); sg[:64] = -1
    P.add("ssgnA", sg)
    P.add("ssgnC", -sg)
    sig = np.arange(128) // 16
    P.add("stm", (sig[None, :] >= sig[:, None]).astype(np.float32))
    sw = np.zeros((128, 128), np.float32); sw[np.arange(128), (np.arange(128) + 64) % 128] = 1
    P.add("sswap", sw)
    return P


def build(phases, first, last, poff, npar):
    nc = bass.Bass("TRN2", target_bir_lowering=False)
    x_d = nc.dram_tensor("x", [2, SEQ, D], F32, kind="ExternalInput").ap()
    par_d = nc.dram_tensor("par", [128, npar], F32, kind="ExternalInput").ap()
    modw_d = nc.dram_tensor("mod_w", [2, D, 9 * D], F32, kind="ExternalInput").ap()
    kinds = {k for _, k in phases}
    wgu_d = {j: nc.dram_tensor(f"wgu{j}", [2, NF, 128, 2048], F32, kind="ExternalInput").ap()
             for j in (1, 2) if f"ffn{j}" in kinds}
    wdn_d = {j: nc.dram_tensor(f"wdn{j}", [2, NF, 128, D], F32, kind="ExternalInput").ap()
             for j in (1, 2) if f"ffn{j}" in kinds}
    y_d = nc.dram_tensor("y", [2, SEQ, D], F32, kind="ExternalOutput").ap()
    mixl = sorted({l for l, k in phases if k == "mix"})
    win_d = {l: nc.dram_tensor(f"win{l}", [128, 8, NWIN], F32, kind="ExternalInput").ap() for l in mixl}
    wout_d = {l: nc.dram_tensor(f"wout{l}", [128, 8, D], F32, kind="ExternalInput").ap() for l in mixl}
    wpw_d = {l: nc.dram_tensor(f"wpw{l}", [128, 2, 256], F32, kind="ExternalInput").ap() for l in mixl}
    wglu_d = {l: nc.dram_tensor(f"wglu{l}", [128, 2, 512], F32, kind="ExternalInput").ap() for l in mixl}

    st = ExitStack()
    with st:
        S = Sched(nc)
        POOLB = 206 * 1024
        pool = st.enter_context(nc.sbuf_tensor("pool", [128, POOLB // 2], BF16))
        psum = st.enter_context(nc.psum_tensor("psum", [128, 8, 512], F32))
        M = Mem(pool, POOLB)

        def PS(b):
            return psum[:, b, :]

        XT = M.alloc([8, SEQ], F32)
        HT = M.alloc([8, SEQ], BF16)
        identf = M.alloc([128], F32)
        identb = M.alloc([128], BF16)
        onesb = M.alloc([128], BF16)
        PAR = M.alloc([npar], F32)
        MOD = M.alloc([2, 72, 2], F32)
        DER = M.alloc([2, 2, 3, 3, 8], F32)
        condb = M.alloc([8, 2], BF16)
        sq = [M.alloc([512], BF16) for _ in range(2)]
        rs = M.alloc([512], F32)
        rstd = M.alloc([512], F32)
        tmpf = [M.alloc([512], F32) for _ in range(2)]
        sgf = [M.alloc([512], F32) for _ in range(2)]
        big0 = M.off

        def par(name):
            o, n = poff[name]
            return PAR[:, o:o + n]

        S.add("sp", lambda e: e.dma_start(out=PAR, in_=par_d), writes=["PAR"], dma=True)
        S.add("pool", lambda e: e.memset(identf, 0.0), writes=["identf"])
        S.add("pool", lambda e: e.affine_select(out=identf, in_=identf, pattern=[[-1, 128]],
                                                compare_op=ALU.not_equal, fill=1.0, base=0,
                                                channel_multiplier=1), reads=["identf"], writes=["identf"])
        S.add("dve", lambda e: e.tensor_copy(out=identb, in_=identf), reads=["identf"], writes=["identb"])
        S.add("dve", lambda e: e.memset(onesb, 1.0), writes=["onesb"])

        S.add("act", lambda e: e.activation(out=condb, in_=par("cT").rearrange("p (a b) -> p a b", a=8),
                                            func=AF.Silu), reads=["PAR"], writes=["condb"])
        layers = sorted({l for l, _ in phases})
        mslab = [M.alloc([8, 1024], BF16) for _ in range(2)]
        for l in layers:
            for sl in range(9):
                buf = mslab[sl % 2]
                S.add("pool", lambda e, buf=buf, l=l, sl=sl: e.dma_start(
                    out=buf, in_=modw_d[l, :, sl * 1024:(sl + 1) * 1024].rearrange("(k p) n -> p k n", p=128)),
                    writes=[("mslab", sl % 2)], dma=True)
                for fc in range(8):
                    ch = sl * 8 + fc
                    for k in range(8):
                        S.add("pe", lambda e, buf=buf, fc=fc, k=k, ch=ch: e.matmul(
                            psum[:, 0, 2 * ch:2 * ch + 2], buf[:, k, fc * 128:(fc + 1) * 128], condb[:, k, :],
                            start=(k == 0), stop=(k == 7)),
                            reads=[("mslab", sl % 2), "condb"], writes=[("ps", 0)])
            for s in range(2):
                S.add("dve", lambda e, l=l, s=s: e.tensor_tensor(
                    out=MOD[:, l, :, s], in0=psum[:, 0, s:144:2], in1=par(f"modb{l}"), op=ALU.add),
                    reads=["PAR"], writes=[("ps", 0), "MOD"])
            for s in range(2):
                for j in range(3):
                    a_, sh_, g_ = DER[:, l, s, j, 0, :], DER[:, l, s, j, 1, :], DER[:, l, s, j, 2, :]
                    c0 = 3 * j * 8
                    S.add("dve", lambda e, a_=a_, l=l, s=s, j=j, c0=c0: e.scalar_tensor_tensor(
                        out=a_, in0=MOD[:, l, c0 + 8:c0 + 16, s], scalar=1.0, in1=par(f"ng{l}{j}"),
                        op0=ALU.add, op1=ALU.mult), reads=["MOD", "PAR"], writes=["DER"])
                    S.add("dve", lambda e, sh_=sh_, l=l, s=s, c0=c0: e.tensor_copy(
                        out=sh_, in_=MOD[:, l, c0:c0 + 8, s]), reads=["MOD"], writes=["DER"])
                    S.add("dve", lambda e, g_=g_, l=l, s=s, j=j, c0=c0: e.tensor_scalar(
                        out=g_, in0=MOD[:, l, c0 + 16:c0 + 24, s], scalar1=(1.0 if j == 1 else 0.5), scalar2=None,
                        op0=ALU.mult), reads=["MOD"], writes=["DER"])
        S.barrier()
        M.off = big0

        def normmod(l, s, j):
            rss = [rs, rstd]

            def stats(t):
                ts_ = slice(t * 512, (t + 1) * 512)
                bank = 6 + t % 2
                for c in range(8):
                    q_ = sq[c % 2]
                    S.add("act", lambda e, q_=q_, c=c, ts_=ts_: e.activation(out=q_, in_=XT[:, c, ts_], func=AF.Square),
                          reads=[("XT", c, t)], writes=[("sq", c % 2)])
                    S.add("pe", lambda e, q_=q_, c=c, bank=bank: e.matmul(PS(bank), onesb, q_, start=(c == 0), stop=(c == 7)),
                          reads=[("sq", c % 2), "onesb"], writes=[("ps", bank)])
                r_ = rss[t % 2]
                S.add("act", lambda e, r_=r_, bank=bank: e.activation(out=r_, in_=PS(bank), func=AF.Sqrt, scale=1.0 / D, bias=EPS),
                      writes=[("ps", bank), ("rs" if t % 2 == 0 else "rstd")])
                S.add("dve", lambda e, r_=r_: e.reciprocal(out=r_, in_=r_), reads=[("rs" if t % 2 == 0 else "rstd")], writes=[("rs" if t % 2 == 0 else "rstd")])

            def modulate(t):
                ts_ = slice(t * 512, (t + 1) * 512)
                r_ = rss[t % 2]
                for c in range(8):
                    tf = tmpf[c % 2]
                    S.add("dve", lambda e, tf=tf, c=c, ts_=ts_, r_=r_: e.scalar_tensor_tensor(
                        out=tf, in0=XT[:, c, ts_], scalar=DER[:, l, s, j, 0, c:c + 1], in1=r_,
                        op0=ALU.mult, op1=ALU.mult), reads=[("XT", c, t), ("rs" if t % 2 == 0 else "rstd"), "DER"], writes=[("tmpf", c % 2)])
                    S.add("act", lambda e, tf=tf, c=c, ts_=ts_: e.activation(
                        out=HT[:, c, ts_], in_=tf, func=AF.Identity, bias=DER[:, l, s, j, 1, c:c + 1], scale=1.0),
                        reads=[("tmpf", c % 2), "DER"], writes=[("HT", c, t)])

            stats(0)
            for t in range(4):
                if t + 1 < 4:
                    stats(t + 1)
                modulate(t)

        def ffn(l, s, which):
            j = 0 if which == 1 else 2
            normmod(l, s, j)
            m0 = M.off
            act = M.alloc([11, SEQ], BF16)
            wgu = [M.alloc([8, 256], BF16) for _ in range(3)]
            wdn = M.alloc([11, D], BF16)

            def load_gu(f):
                S.add("pool", lambda e, f=f: e.dma_start(
                    out=wgu[f % 3], in_=wgu_d[which][l, f].rearrange("p (k n) -> p k n", k=8)),
                    writes=[("wgu", f % 3)], dma=True)

            for f in range(2):
                load_gu(f)
            pi = 0
            for grp in range(2):
                for fl in range(11):
                    f = grp * 11 + fl
                    if f + 2 < NF:
                        load_gu(f + 2)
                    S.add("pool", lambda e, f=f, fl=fl: e.dma_start(out=wdn[:, fl, :], in_=wdn_d[which][l, f]),
                          writes=[("wdn", fl)], dma=True)
                    w = wgu[f % 3]
                    for t in range(4):
                        ts_ = slice(t * 512, (t + 1) * 512)
                        bg, bu = 2 * (pi % 2), 2 * (pi % 2) + 1
                        pi += 1
                        for gu, bk in ((0, bg), (1, bu)):
                            for k in range(8):
                                S.add("pe", lambda e, w=w, gu=gu, bk=bk, k=k, ts_=ts_: e.matmul(
                                    PS(bk), w[:, k, gu * 128:(gu + 1) * 128], HT[:, k, ts_],
                                    start=(k == 0), stop=(k == 7)),
                                    reads=[("wgu", f % 3), ("HT", k, t)], writes=[("ps", bk)])
                        sg = sgf[pi % 2]
                        S.add("act", lambda e, sg=sg, bg=bg: e.activation(out=sg, in_=PS(bg), func=AF.Silu),
                              writes=[("ps", bg), ("sgf", pi % 2)])
                        S.add("dve", lambda e, sg=sg, bu=bu, fl=fl, ts_=ts_: e.tensor_tensor(
                            out=act[:, fl, ts_], in0=sg, in1=PS(bu), op=ALU.mult),
                            reads=[("sgf", pi % 2)], writes=[("ps", bu), ("act", fl, t)])
                di = 0
                for dc in range(8):
                    for t in range(4):
                        ts_ = slice(t * 512, (t + 1) * 512)
                        bk = 4 + di % 2
                        di += 1
                        for fl in range(11):
                            S.add("pe", lambda e, fl=fl, dc=dc, bk=bk, ts_=ts_: e.matmul(
                                PS(bk), wdn[:, fl, dc * 128:(dc + 1) * 128], act[:, fl, ts_],
                                start=(fl == 0), stop=(fl == 10)),
                                reads=[("wdn", fl), ("act", fl, t)], writes=[("ps", bk)])
                        S.add("dve", lambda e, dc=dc, bk=bk, ts_=ts_: e.scalar_tensor_tensor(
                            out=XT[:, dc, ts_], in0=PS(bk), scalar=DER[:, l, s, j, 2, dc:dc + 1], in1=XT[:, dc, ts_],
                            op0=ALU.mult, op1=ALU.add), reads=["DER"], writes=[("ps", bk), ("XT", dc, t)])
            S.barrier()
            M.off = m0

        def load_x(s):
            m0 = M.off
            stg = [M.alloc([D], F32) for _ in range(2)]
            for i in range(16):
                sg_ = stg[i % 2]
                S.add("sp", lambda e, sg_=sg_, i=i: e.dma_start(out=sg_, in_=x_d[s, i * 128:(i + 1) * 128, :]),
                      writes=[("stg", i % 2)], dma=True)
                for h in range(2):
                    bk = 6 + (2 * i + h) % 2
                    for cc in range(4):
                        c = 4 * h + cc
                        S.add("pe", lambda e, sg_=sg_, c=c, cc=cc, bk=bk: e.transpose(
                            psum[:, bk, cc * 128:(cc + 1) * 128], sg_[:, c * 128:(c + 1) * 128], identf),
                            reads=[("stg", i % 2), "identf"], writes=[("ps", bk)])
                    eng = "act" if h == 0 else "dve"
                    dst = XT[:, 4 * h:4 * h + 4, i * 128:(i + 1) * 128]
                    src = psum[:, bk, :].rearrange("p (a b) -> p a b", a=4)
                    if eng == "act":
                        S.add("act", lambda e, dst=dst, src=src: e.copy(out=dst, in_=src),
                              writes=[("ps", bk)] + [("XT", 4 * h + cc, i // 4) for cc in range(4)])
                    else:
                        S.add("dve", lambda e, dst=dst, src=src: e.tensor_copy(out=dst, in_=src),
                              writes=[("ps", bk)] + [("XT", 4 * h + cc, i // 4) for cc in range(4)])
            S.barrier()
            M.off = m0

        def store_x(s, final):
            m0 = M.off
            stg = [M.alloc([D], F32) for _ in range(2)]
            xn = [M.alloc([8, 128], F32) for _ in range(2)]
            for t in range(4):
                ts_ = slice(t * 512, (t + 1) * 512)
                if final:
                    for c in range(8):
                        q_ = sq[c % 2]
                        S.add("act", lambda e, q_=q_, c=c, ts_=ts_: e.activation(out=q_, in_=XT[:, c, ts_], func=AF.Square),
                              reads=[("XT", c, t)], writes=[("sq", c % 2)])
                        S.add("pe", lambda e, q_=q_, c=c: e.matmul(PS(5), onesb, q_, start=(c == 0), stop=(c == 7)),
                              reads=[("sq", c % 2), "onesb"], writes=[("ps", 5)])
                    S.add("act", lambda e: e.activation(out=rs, in_=PS(5), func=AF.Sqrt, scale=1.0 / D, bias=EPS),
                          writes=[("ps", 5), "rs"])
                    S.add("dve", lambda e: e.reciprocal(out=rstd, in_=rs), reads=["rs"], writes=["rstd"])
                for ii in range(4):
                    i = 4 * t + ii
                    isl = slice(i * 128, (i + 1) * 128)
                    xb_ = xn[i % 2]
                    if final:
                        for c in range(8):
                            S.add("dve", lambda e, xb_=xb_, c=c, isl=isl, ii=ii: e.scalar_tensor_tensor(
                                out=xb_[:, c, :], in0=XT[:, c, isl], scalar=par("fin")[:, c:c + 1],
                                in1=rstd[:, ii * 128:(ii + 1) * 128], op0=ALU.mult, op1=ALU.mult),
                                reads=[("XT", c, t), "rstd", "PAR"], writes=[("xn", i % 2)])
                    sg_ = stg[i % 2]
                    for h in range(2):
                        bk = 6 + (2 * i + h) % 2
                        for cc in range(4):
                            c = 4 * h + cc
                            src = xb_[:, c, :] if final else XT[:, c, isl]
                            S.add("pe", lambda e, src=src, cc=cc, bk=bk: e.transpose(
                                psum[:, bk, cc * 128:(cc + 1) * 128], src, identf),
                                reads=[("xn", i % 2), ("XT", c, t), "identf"], writes=[("ps", bk)])
                        dst = sg_[:, h * 512:(h + 1) * 512]
                        if h == 0:
                            S.add("act", lambda e, dst=dst, bk=bk: e.copy(out=dst, in_=PS(bk)),
                                  writes=[("ps", bk), ("stg", i % 2, h)])
                        else:
                            S.add("dve", lambda e, dst=dst, bk=bk: e.tensor_copy(out=dst, in_=PS(bk)),
                                  writes=[("ps", bk), ("stg", i % 2, h)])
                    S.add("sp", lambda e, sg_=sg_, isl=isl: e.dma_start(out=y_d[s, isl, :], in_=sg_),
                          reads=[("stg", i % 2, 0), ("stg", i % 2, 1)], writes=[("y", s, i)], dma=True)
            S.barrier()
            M.off = m0


        NIT = 10

        def gnorm_tile(src, skey, nch, gn, dst, dkey, bank):
            for c in range(nch):
                q_ = sq[c % 2]
                S.add("act", lambda e, q_=q_, c=c: e.activation(out=q_, in_=src[:, c, :], func=AF.Square),
                      reads=[skey], writes=[("sq", c % 2)])
                S.add("pe", lambda e, q_=q_, c=c: e.matmul(PS(bank), onesb, q_, start=(c == 0), stop=(c == nch - 1)),
                      reads=[("sq", c % 2), "onesb"], writes=[("ps", bank)])
            S.add("act", lambda e: e.activation(out=rs, in_=PS(bank), func=AF.Sqrt, scale=1.0 / (128 * nch), bias=EPS),
                  writes=[("ps", bank), "rs"])
            S.add("dve", lambda e: e.reciprocal(out=rstd, in_=rs), reads=["rs"], writes=["rstd"])
            for c in range(nch):
                S.add("dve", lambda e, c=c: e.scalar_tensor_tensor(
                    out=dst[:, c, :], in0=src[:, c, :], scalar=gn[:, c:c + 1], in1=rstd, op0=ALU.mult, op1=ALU.mult),
                    reads=[skey, "rstd", "PAR"], writes=[dkey])

        def wout_part(l, s, mc, mkeyf, nch, wo, wokey):
            di = 0
            for dc in range(8):
                for t in range(4):
                    ts_ = slice(t * 512, (t + 1) * 512)
                    bk = 4 + di % 2
                    di += 1
                    for kc in range(nch):
                        S.add("pe", lambda e, kc=kc, dc=dc, bk=bk, ts_=ts_: e.matmul(
                            PS(bk), wo[:, kc, dc * 128:(dc + 1) * 128], mc[:, kc, ts_],
                            start=(kc == 0), stop=(kc == nch - 1)), reads=[wokey, mkeyf(t)], writes=[("ps", bk)])
                    S.add("dve", lambda e, dc=dc, bk=bk, ts_=ts_: e.scalar_tensor_tensor(
                        out=XT[:, dc, ts_], in0=PS(bk), scalar=DER[:, l, s, 1, 2, dc:dc + 1], in1=XT[:, dc, ts_],
                        op0=ALU.mult, op1=ALU.add), reads=["DER"], writes=[("ps", bk), ("XT", dc, t)])

        def conv_part(l, s):
            m0 = M.off
            wab = M.alloc([8, 512], BF16)
            ucv = M.alloc([2, 30 + SEQ], BF16)
            DWc = M.alloc([2, 31, 128], BF16)
            yacc = M.alloc([2, SEQ], F32)
            zs = M.alloc([2, SEQ], BF16)
            wpw = M.alloc([2, 256], BF16)
            wo = M.alloc([2, D], BF16)
            mc = DWc.rearrange("p a b c -> p (a b c)")[:, 0:2 * SEQ].rearrange("p (a b) -> p a b", a=2)
            ycf = M.alloc([2, 512], F32)
            mt = M.alloc([512], F32)
            msq = M.alloc([512], F32)
            d1 = [M.alloc([512], F32) for _ in range(2)]
            ybf = [M.alloc([512], BF16) for _ in range(2)]
            cw = par(f"cw{l}").rearrange("p (a b) -> p a b", a=2)
            S.add("pool", lambda e: e.dma_start(out=wab, in_=win_d[l][:, :, C_A:C_A + 512]), writes=["wab"], dma=True)
            S.add("pool", lambda e: e.dma_start(out=wpw, in_=wpw_d[l]), writes=["wpw"], dma=True)
            S.add("pool", lambda e: e.dma_start(out=wo, in_=wout_d[l][:, 6:8, :]), writes=["wo"], dma=True)
            S.add("dve", lambda e: e.memset(ucv[:, :, 0:30], 0.0), writes=["ucvpad"])
            pi = 0
            for cc in range(2):
                for t in range(4):
                    ts_ = slice(t * 512, (t + 1) * 512)
                    ba, bg = 2 * (pi % 2), 2 * (pi % 2) + 1
                    pi += 1
                    for (c0, bk) in ((cc * 128, ba), (256 + cc * 128, bg)):
                        for k in range(8):
                            S.add("pe", lambda e, c0=c0, bk=bk, k=k, ts_=ts_: e.matmul(
                                PS(bk), wab[:, k, c0:c0 + 128], HT[:, k, ts_], start=(k == 0), stop=(k == 7)),
                                reads=["wab", ("HT", k, t)], writes=[("ps", bk)])
                    sg = sgf[pi % 2]
                    S.add("act", lambda e, sg=sg, bg=bg: e.activation(out=sg, in_=PS(bg), func=AF.Sigmoid),
                          writes=[("ps", bg), ("sgf", pi % 2)])
                    S.add("dve", lambda e, sg=sg, ba=ba, cc=cc, t=t: e.tensor_tensor(
                        out=ucv[:, cc, 30 + t * 512:30 + (t + 1) * 512], in0=sg, in1=PS(ba), op=ALU.mult),
                        reads=[("sgf", pi % 2)], writes=[("ps", ba), ("ucv", cc)])
            for cc in range(2):
                for j in range(31):
                    S.add("pool", lambda e, cc=cc, j=j: e.tensor_scalar(out=DWc[:, cc, j, :], in0=identb, scalar1=cw[:, cc, j:j + 1],
                                                                        scalar2=None, op0=ALU.mult),
                          reads=["PAR", "identb"], writes=[("DWc", cc)])
            ci = 0
            for cc in range(2):
                for t in range(4):
                    bk = ci % 4
                    ci += 1
                    for j in range(31):
                        S.add("pe", lambda e, cc=cc, t=t, j=j, bk=bk: e.matmul(
                            PS(bk), DWc[:, cc, j, :], ucv[:, cc, j + t * 512:j + t * 512 + 512], start=(j == 0), stop=(j == 30)),
                            reads=[("DWc", cc), ("ucv", cc), "ucvpad"], writes=[("ps", bk)])
                    S.add("act", lambda e, cc=cc, t=t, bk=bk: e.activation(
                        out=yacc[:, cc, t * 512:(t + 1) * 512], in_=PS(bk), func=AF.Identity, bias=par(f"cb{l}")[:, cc:cc + 1], scale=1.0),
                        reads=["PAR"], writes=[("ps", bk), ("yacc", cc)])
            for t in range(4):
                ts_ = slice(t * 512, (t + 1) * 512)
                for cc in range(2):
                    S.add("act", lambda e, cc=cc, ts_=ts_: e.copy(out=ybf[cc], in_=yacc[:, cc, ts_]),
                          reads=[("yacc", cc)], writes=[("ybf", cc)])
                    S.add("pe", lambda e, cc=cc: e.matmul(PS(0), onesb, ybf[cc], start=(cc == 0), stop=(cc == 1)),
                          reads=[("ybf", cc), "onesb"], writes=[("ps", 0)])
                for cc in range(2):
                    S.add("act", lambda e, cc=cc, ts_=ts_: e.activation(out=sq[cc], in_=yacc[:, cc, ts_], func=AF.Square),
                          reads=[("yacc", cc)], writes=[("sq", cc)])
                    S.add("pe", lambda e, cc=cc: e.matmul(PS(1), onesb, sq[cc], start=(cc == 0), stop=(cc == 1)),
                          reads=[("sq", cc), "onesb"], writes=[("ps", 1)])
                S.add("dve", lambda e: e.tensor_scalar(out=mt, in0=PS(0), scalar1=1.0 / 256, scalar2=None, op0=ALU.mult),
                      writes=[("ps", 0), "mt"])
                S.add("dve", lambda e: e.tensor_tensor(out=msq, in0=mt, in1=mt, op=ALU.mult), reads=["mt"], writes=["msq"])
                S.add("dve", lambda e: e.scalar_tensor_tensor(out=msq, in0=PS(1), scalar=1.0 / 256, in1=msq,
                                                              op0=ALU.mult, op1=ALU.subtract),
                      reads=["msq"], writes=[("ps", 1), "msq"])
                S.add("act", lambda e: e.activation(out=rs, in_=msq, func=AF.Sqrt, scale=1.0, bias=EPS),
                      reads=["msq"], writes=["rs"])
                S.add("dve", lambda e: e.reciprocal(out=rstd, in_=rs), reads=["rs"], writes=["rstd"])
                for cc in range(2):
                    S.add("dve", lambda e, cc=cc, ts_=ts_: e.tensor_tensor(out=d1[cc], in0=yacc[:, cc, ts_], in1=mt, op=ALU.subtract),
                          reads=[("yacc", cc), "mt"], writes=[("d1", cc)])
                    S.add("dve", lambda e, cc=cc: e.tensor_tensor(out=d1[cc], in0=d1[cc], in1=rstd, op=ALU.mult),
                          reads=[("d1", cc), "rstd"], writes=[("d1", cc)])
                    S.add("act", lambda e, cc=cc, ts_=ts_: e.activation(
                        out=zs[:, cc, ts_], in_=d1[cc], func=AF.Silu, scale=par(f"clg{l}")[:, cc:cc + 1],
                        bias=par(f"clb{l}")[:, cc:cc + 1]), reads=[("d1", cc), "PAR"], writes=[("zs", t)])
            S.barrier()
            for t in range(4):
                ts_ = slice(t * 512, (t + 1) * 512)
                for oc in range(2):
                    for kc in range(2):
                        S.add("pe", lambda e, oc=oc, kc=kc, ts_=ts_: e.matmul(
                            PS(2 + oc), wpw[:, kc, oc * 128:(oc + 1) * 128], zs[:, kc, ts_], start=(kc == 0), stop=(kc == 1)),
                            reads=["wpw", ("zs", t)], writes=[("ps", 2 + oc)])
                    S.add("act", lambda e, oc=oc: e.copy(out=ycf[:, oc, :], in_=PS(2 + oc)), writes=[("ps", 2 + oc), "ycf"])
                gnorm_tile(ycf, "ycf", 2, par(f"cgn{l}"), mc[:, :, ts_], ("mc", t), 3)
            wout_part(l, s, mc, lambda t: ("mc", t), 2, wo, "wo")
            S.barrier()
            M.off = m0

        def att_part(l, s):
            m0 = M.off
            qT = M.alloc([4, SEQ], BF16)
            kkT = M.alloc([SEQ], BF16)
            kiT = M.alloc([SEQ], BF16)
            qiT = M.alloc([2, SEQ], BF16)
            V1 = M.alloc([16, 66], BF16)
            WI = M.alloc([16, 8], F32)
            wo = M.alloc([4, D], BF16)
            P2 = M.alloc([NIT + 1], F32)
            thr0 = M.alloc([1], F32)
            m1 = M.off
            wr = [M.alloc([8, 256], BF16) for _ in range(2)]
            S.add("pool", lambda e: e.dma_start(out=wo, in_=wout_d[l][:, 0:4, :]), writes=["wo"], dma=True)
            for i in range(NIT + 1):
                S.add("pool", lambda e, i=i: e.memset(P2[:, i:i + 1], 2.0 ** -(i + 1)), writes=["P2"])
            S.add("pool", lambda e: e.memset(thr0, -1e29), writes=["thr0"])
            S.add("pool", lambda e: e.memset(V1[:, :, 64:65], 1.0), writes=["V1one"])
            groups = [(C_Q, 256, [("q", 0), ("q", 1)]), (C_Q + 256, 256, [("q", 2), ("q", 3)]),
                      (C_KK, 256, [("kk", 0), ("ki", 0)]), (C_QI, 256, [("qi", 0), ("qi", 1)]), (C_VW, 72, None)]
            pi = 0
            for gi, (c0, n, dests) in enumerate(groups):
                w = wr[gi % 2]
                S.add("pool", lambda e, w=w, c0=c0, n=n: e.dma_start(out=w[:, :, 0:n], in_=win_d[l][:, :, c0:c0 + n]),
                      writes=[("wr", gi % 2)], dma=True)
                if dests is not None:
                    for ci, (kind, idx) in enumerate(dests):
                        for t in range(4):
                            ts_ = slice(t * 512, (t + 1) * 512)
                            bk = pi % 4
                            pi += 1
                            for k in range(8):
                                S.add("pe", lambda e, w=w, ci=ci, bk=bk, k=k, ts_=ts_: e.matmul(
                                    PS(bk), w[:, k, ci * 128:(ci + 1) * 128], HT[:, k, ts_], start=(k == 0), stop=(k == 7)),
                                    reads=[("wr", gi % 2), ("HT", k, t)], writes=[("ps", bk)])
                            dst = {"q": lambda: qT[:, idx, ts_], "kk": lambda: kkT[:, ts_], "ki": lambda: kiT[:, ts_],
                                   "qi": lambda: qiT[:, idx, ts_]}[kind]()
                            sc_ = 0.125 if kind == "q" else 1.0
                            if pi % 2 == 0:
                                S.add("act", lambda e, dst=dst, bk=bk, sc_=sc_: e.activation(
                                    out=dst, in_=PS(bk), func=AF.Copy, scale=sc_), writes=[("ps", bk), (kind, idx, t)])
                            else:
                                S.add("dve", lambda e, dst=dst, bk=bk, sc_=sc_: e.tensor_scalar(
                                    out=dst, in0=PS(bk), scalar1=sc_, scalar2=None, op0=ALU.mult),
                                    writes=[("ps", bk), (kind, idx, t)])
                else:
                    for i in range(16):
                        bk = pi % 4
                        pi += 1
                        for k in range(8):
                            S.add("pe", lambda e, w=w, bk=bk, k=k, i=i: e.matmul(
                                psum[:, bk, 0:72], HT[:, k, i * 128:(i + 1) * 128], w[:, k, 0:72], start=(k == 0), stop=(k == 7)),
                                reads=[("wr", gi % 2), ("HT", k, i // 4)], writes=[("ps", bk)])
                        S.add("dve", lambda e, bk=bk, i=i: e.tensor_copy(out=V1[:, i, 0:64], in_=psum[:, bk, 0:64]),
                              writes=[("ps", bk), "V1"])
                        S.add("act", lambda e, bk=bk, i=i: e.copy(out=WI[:, i, :], in_=psum[:, bk, 64:72]),
                              writes=[("ps", bk), "WI"])
            S.barrier()
            M.off = m1
            scs = [M.alloc([SEQ], F32) for _ in range(2)]
            rt = [M.alloc([512], BF16) for _ in range(3)]
            DW = [M.alloc([8, 128], BF16) for _ in range(2)]
            negm = [M.alloc([SEQ], BF16) for _ in range(2)]
            ident4 = M.alloc([4, 128], BF16)
            ex = [M.alloc([4, 128], BF16) for _ in range(4)]
            otok = M.alloc([8, 65], F32)
            on = sgf[0].rearrange("p (a b) -> p a b", a=8)
            onsq = tmpf[0]
            onb = rs.bitcast(BF16)[:, 0:512]
            attT = rstd.bitcast(BF16)[:, 0:512].rearrange("p (a b) -> p a b", a=4)
            sm = M.alloc([48], F32)
            mid, cnt, ge, mx, mn, w0, ssq, rq = [sm[:, i:i + 1] for i in range(8)]
            los = [sm[:, 8:9], sm[:, 9:10]]
            Wd = sm[:, 12:12 + NIT + 1]
            rden = M.alloc([8], F32)
            gatt = par(f"agn{l}")
            pb = [psum[:, bkk, :].bitcast(BF16) for bkk in range(8)]
            for i4 in range(4):
                S.add("pool", lambda e, i4=i4: e.tensor_copy(out=ident4[:, i4, :], in_=identb), reads=["identb"], writes=["ident4"])
            cntr = {"ri": 0}

            def stageA(b):
                S_ = 128 * (b + 1)
                qs = slice(128 * b, 128 * b + 128)
                nseg = (S_ + 511) // 512
                tq = b // 4
                nm = negm[b % 2]
                nmk = ("nm", b % 2)
                dw = DW[b % 2]
                sc = scs[b % 2]
                for h in range(8):
                    S.add("pool", lambda e, dw=dw, h=h: e.tensor_scalar(out=dw[:, h, :], in0=identb, scalar1=WI[:, b, h:h + 1],
                                                                       scalar2=None, op0=ALU.mult),
                          reads=["WI", "identb"], writes=[("DW", b % 2)])
                for sg_i in range(nseg):
                    c0, c1 = sg_i * 512, min(S_, sg_i * 512 + 512)
                    n = c1 - c0
                    accb = 2
                    prev = None
                    for h in range(9):
                        if h < 8:
                            ri = cntr["ri"]
                            cntr["ri"] += 1
                            pr = slice(32 * (h % 4), 32 * (h % 4) + 32)
                            bk = ri % 2
                            r_ = rt[ri % 3]
                            rk = ("rt", ri % 3)
                            S.add("pe", lambda e, pr=pr, h=h, bk=bk, c0=c0, c1=c1, n=n: e.matmul(
                                psum[:, bk, 0:n], qiT[pr, h // 4, qs], kiT[pr, c0:c1], start=True, stop=True,
                                tile_position=(32 * (h % 4), 0)),
                                reads=[("qi", h // 4, tq), ("ki", 0, sg_i)], writes=[("ps", bk)])
                            S.add("act", lambda e, r_=r_, bk=bk, n=n: e.activation(out=r_[:, 0:n], in_=psum[:, bk, 0:n], func=AF.Relu),
                                  writes=[("ps", bk), rk])
                        if prev is not None:
                            ph, pr_t, prk = prev
                            S.add("pe", lambda e, ph=ph, pr_t=pr_t, n=n: e.matmul(
                                psum[:, accb, 0:n], dw[:, ph, :], pr_t[:, 0:n], start=(ph == 0), stop=(ph == 7)),
                                reads=[("DW", b % 2), prk], writes=[("ps", accb)])
                        prev = (h, r_, rk) if h < 8 else None
                    S.add("act", lambda e, n=n, c0=c0, c1=c1: e.copy(out=sc[:, c0:c1], in_=psum[:, accb, 0:n]),
                          writes=[("ps", accb), ("sc", b % 2, sg_i)])

            def stageAd(b):
                S_ = 128 * (b + 1)
                nseg = (S_ + 511) // 512
                nm = negm[b % 2]
                nmk = ("nm", b % 2)
                sc = scs[b % 2]
                sck = [("sc", b % 2, i) for i in range(nseg)]
                lo = los[b % 2]
                lok = ("lo", b % 2)
                if b >= 2:
                    S.add("dve", lambda e: e.tensor_reduce(out=mx, in_=sc[:, 0:S_], axis=AX.X, op=ALU.max), reads=sck, writes=["mx"])
                    S.add("dve", lambda e: e.tensor_reduce(out=mn, in_=sc[:, 0:S_], axis=AX.X, op=ALU.min), reads=sck, writes=["mn"])
                S.add("dve", lambda e: e.memset(sc[0:64, S_ - 64:S_], -1e30), reads=sck, writes=sck)
                if b >= 2:
                    S.add("dve", lambda e: e.tensor_tensor(out=w0, in0=mx, in1=mn, op=ALU.subtract), reads=["mx", "mn"], writes=["w0"])
                    S.add("dve", lambda e: e.tensor_scalar(out=Wd, in0=P2, scalar1=w0, scalar2=None, op0=ALU.mult),
                          reads=["w0", "P2"], writes=["Wd"])
                    S.add("dve", lambda e: e.tensor_tensor(out=mid, in0=mn, in1=Wd[:, 0:1], op=ALU.add), reads=["mn", "Wd"], writes=["mid"])
                    for i in range(NIT):
                        S.add("dve", lambda e: e.tensor_scalar(
                            out=nm[:, 0:S_], in0=sc[:, 0:S_], scalar1=mid, scalar2=None, op0=ALU.is_ge, op1=ALU.add,
                            accum_out=cnt), reads=sck + ["mid"], writes=[nmk, "cnt"])
                        S.add("dve", lambda e, i=i: e.tensor_scalar(out=ge, in0=cnt, scalar1=255.5, scalar2=Wd[:, i:i + 1],
                                                                    op0=ALU.is_ge, op1=ALU.mult), reads=["cnt", "Wd"], writes=["ge"])
                        S.add("dve", lambda e, i=i: e.scalar_tensor_tensor(
                            out=mid, in0=mid, scalar=Wd[:, i + 1:i + 2], in1=ge, op0=ALU.subtract, op1=ALU.add),
                            reads=["ge", "Wd", "mid"], writes=["mid"])
                    S.add("dve", lambda e: e.tensor_tensor(out=lo, in0=mid, in1=Wd[:, NIT:NIT + 1], op=ALU.subtract),
                          reads=["mid", "Wd"], writes=[lok])
                    thr = lo
                else:
                    thr = thr0
                S.add("dve", lambda e: e.tensor_scalar(
                    out=nm[:, 0:S_], in0=sc[:, 0:S_], scalar1=thr, scalar2=-30000.0, op0=ALU.is_lt, op1=ALU.mult),
                    reads=sck + [lok, "thr0"], writes=[nmk])

            def stageB(b):
                qs = slice(128 * b, 128 * b + 128)
                tq = b // 4
                nm = negm[b % 2]
                nmk = ("nm", b % 2)
                i4f = ident4.rearrange("p a b -> p (a b)")
                for ch in range(b + 1):
                    ks = slice(ch * 128, (ch + 1) * 128)
                    for par_ in range(2):
                        S.add("pe", lambda e, par_=par_, ks=ks: e.matmul(psum[:, 3 + par_, :], nm[:, ks], i4f, start=True, stop=False,
                                                                         skip_group_check=True),
                              reads=[nmk, "ident4"], writes=[("ps", 3 + par_)])
                    for hh in range(4):
                        for par_ in range(2):
                            hp = slice(64 * par_, 64 * par_ + 64)
                            S.add("pe", lambda e, hh=hh, par_=par_, hp=hp, ks=ks: e.matmul(
                                psum[:, 3 + par_, hh * 128:(hh + 1) * 128], kkT[hp, ks], qT[hp, hh, qs], start=False, stop=(hh == 3),
                                skip_group_check=True),
                                reads=[("kk", 0, ch // 4), ("q", hh, tq)], writes=[("ps", 3 + par_)])
                    for par_ in range(2):
                        e_ = ex[2 * (ch % 2) + par_]
                        S.add("act", lambda e, e_=e_, par_=par_: e.activation(
                            out=e_, in_=psum[:, 3 + par_, :].rearrange("p (a b) -> p a b", a=4), func=AF.Exp),
                            writes=[("ps", 3 + par_), ("ex", 2 * (ch % 2) + par_)])
                    for h in range(8):
                        ob = 5 + h // 4
                        S.add("pe", lambda e, h=h, ob=ob, ch=ch: e.matmul(
                            psum[:, ob, (h % 4) * 65:(h % 4) * 65 + 65], ex[2 * (ch % 2) + h % 2][:, h // 2, :], V1[:, ch, 0:65],
                            start=(ch == 0 and h % 4 == 0), stop=(ch == b), skip_group_check=True),
                            reads=[("ex", 2 * (ch % 2) + h % 2), "V1", "V1one"], writes=[("ps", ob)])
                S.add("act", lambda e: e.copy(out=otok[:, 0:4, :], in_=psum[:, 5, 0:260].rearrange("p (a b) -> p a b", a=4)),
                      writes=[("ps", 5), "otokA"])
                S.add("act", lambda e: e.copy(out=otok[:, 4:8, :], in_=psum[:, 6, 0:260].rearrange("p (a b) -> p a b", a=4)),
                      writes=[("ps", 6), "otokB"])
                S.add("dve", lambda e: e.reciprocal(out=rden, in_=otok[:, :, 64]), reads=["otokA", "otokB"], writes=["rden"])
                S.add("dve", lambda e: e.tensor_tensor(out=on, in0=otok[:, :, 0:64], in1=rden.unsqueeze(2).broadcast_to([128, 8, 64]),
                                                       op=ALU.mult), reads=["otokA", "otokB", "rden"], writes=["on"])
                onf = on.rearrange("p a b -> p (a b)")
                S.add("dve", lambda e: e.tensor_tensor(out=onsq, in0=onf, in1=onf, op=ALU.mult), reads=["on"], writes=["onsq"])
                S.add("dve", lambda e: e.tensor_scalar(out=onsq, in0=onsq, scalar1=1.0, scalar2=None, op0=ALU.mult, op1=ALU.add,
                                                       accum_out=ssq), reads=["onsq"], writes=["onsq", "ssq"])
                S.add("act", lambda e: e.activation(out=rq, in_=ssq, func=AF.Sqrt, scale=1.0 / 512, bias=EPS), reads=["ssq"], writes=["rq"])
                S.add("dve", lambda e: e.reciprocal(out=rq, in_=rq), reads=["rq"], writes=["rq"])
                S.add("dve", lambda e: e.tensor_scalar(out=onb, in0=onf, scalar1=rq, scalar2=None, op0=ALU.mult),
                      reads=["on", "rq"], writes=["onb"])
                for c in range(4):
                    S.add("pe", lambda e, c=c: e.transpose(pb[7][:, c * 128:(c + 1) * 128], onb[:, c * 128:(c + 1) * 128], identb),
                          reads=["onb", "identb"], writes=[("ps", 7)])
                for c in range(4):
                    S.add("act", lambda e, c=c: e.activation(out=attT[:, c, :], in_=pb[7][:, c * 128:(c + 1) * 128], func=AF.Copy,
                                                             scale=gatt[:, c:c + 1]),
                          reads=["PAR"], writes=[("ps", 7), "attT"])
                for dc in range(8):
                    bk = 3 + dc // 4
                    for kc in range(4):
                        S.add("pe", lambda e, dc=dc, kc=kc, bk=bk: e.matmul(
                            psum[:, bk, (dc % 4) * 128:(dc % 4 + 1) * 128], wo[:, kc, dc * 128:(dc + 1) * 128], attT[:, kc, :],
                            start=(kc == 0), stop=(kc == 3)), reads=["wo", "attT"], writes=[("ps", bk)])
                for dc in range(8):
                    bk = 3 + dc // 4
                    S.add("dve", lambda e, dc=dc, bk=bk: e.scalar_tensor_tensor(
                        out=XT[:, dc, qs], in0=psum[:, bk, (dc % 4) * 128:(dc % 4 + 1) * 128],
                        scalar=DER[:, l, s, 1, 2, dc:dc + 1], in1=XT[:, dc, qs], op0=ALU.mult, op1=ALU.add),
                        reads=["DER"], writes=[("ps", bk), ("XT", dc, tq)])

            stageA(0)
            stageAd(0)
            stageA(1)
            for b in range(16):
                if b + 2 < 16:
                    stageA(b + 2)
                if b + 1 < 16:
                    stageAd(b + 1)
                stageB(b)
            S.barrier()
            M.off = m0


        def ssm_part(l, s):
            I32 = mybir.dt.int32
            m0 = M.off
            TWO_PI = float(2 * np.pi)
            PI = float(np.pi)
            wssm = M.alloc([8, 256], BF16)
            Abuf = M.alloc([2, 16, 8, 16], BF16)
            U = M.alloc([16, 256], BF16)
            Bq = M.alloc([16, 128], BF16)
            Rm = Bq
            E = M.alloc([16, 16, 16], BF16)
            Bs = M.alloc([16, 128], BF16)
            W = M.alloc([16, 128], BF16)
            wglu = M.alloc([2, 512], BF16)
            wo = M.alloc([2, D], BF16)
            zf = M.alloc([2, 512], F32)
            XR = M.alloc([16, 8], F32)
            YR = M.alloc([16, 8], F32)
            m1 = M.off
            S.add("pool", lambda e: e.dma_start(out=wssm, in_=win_d[l][:, :, C_SSM:C_SSM + 256]), writes=["wssm"], dma=True)
            S.add("pool", lambda e: e.dma_start(out=wglu, in_=wglu_d[l]), writes=["wglu"], dma=True)
            S.add("pool", lambda e: e.dma_start(out=wo, in_=wout_d[l][:, 4:6, :]), writes=["wo"], dma=True)
            NP = 32
            tbase = M.off
            T = {n: M.alloc([16, NP], F32) for n in ("A", "KF", "LT", "MAG", "FR", "FR2")}
            ki_ = M.alloc([16, NP], F32).bitcast(I32)
            sm_ = {n: M.alloc([16], F32) for n in ("dt", "th", "lam", "nr", "den", "t1", "t2", "qr", "qi")}
            big = {n: M.alloc([16, 16], F32) for n in ("Q0", "t1", "t2", "BP", "BQ", "CP", "CQ")}
            tb = {"b1": M.view(tbase, [8, 8, 16], BF16), "b2": M.view(tbase + 4096, [8, 8, 16], BF16)}
            te = {"e1": M.view(tbase, [8, 16, 16], BF16), "e2": M.view(tbase + 4096, [8, 16, 16], BF16)}
            pr_ = lambda n: par(f"s{n}{l}")
            jall = par("sjall")
            sgnA, sgnC = par("ssgnA"), par("ssgnC")

            def V(eng, fn, r, w):
                S.add(eng, fn, reads=r, writes=w)

            def b3(ap2, n):
                return ap2.unsqueeze(2).broadcast_to([128, 16, n])

            def fl(ap3):
                return ap3.rearrange("p a b -> p (a b)")

            V("act", lambda e: e.activation(out=sm_["dt"], in_=pr_("ldt"), func=AF.Exp), ["PAR"], ["dt"])
            V("dve", lambda e: e.tensor_tensor(out=sm_["th"], in0=sm_["dt"], in1=pr_("aim"), op=ALU.mult), ["dt", "PAR"], ["th"])
            V("dve", lambda e: e.tensor_tensor(out=sm_["lam"], in0=sm_["dt"], in1=pr_("are"), op=ALU.mult), ["dt", "PAR"], ["lam"])
            jb = jall.unsqueeze(1).broadcast_to([128, 16, NP])
            V("dve", lambda e: e.tensor_tensor(out=T["A"], in0=b3(sm_["th"], NP), in1=jb, op=ALU.mult), ["th", "PAR"], ["A"])
            V("dve", lambda e: e.tensor_tensor(out=T["MAG"], in0=b3(sm_["lam"], NP), in1=jb, op=ALU.mult), ["lam", "PAR"], ["MAG"])
            V("act", lambda e: e.activation(out=T["MAG"], in_=T["MAG"], func=AF.Exp), ["MAG"], ["MAG"])
            V("dve", lambda e: e.tensor_scalar(out=T["A"], in0=T["A"], scalar1=1.0 / TWO_PI, scalar2=64.0, op0=ALU.mult, op1=ALU.add), ["A"], ["A"])
            V("dve", lambda e: e.tensor_copy(out=ki_, in_=T["A"]), ["A"], ["ki"])
            V("dve", lambda e: e.tensor_copy(out=T["KF"], in_=ki_), ["ki"], ["KF"])
            V("dve", lambda e: e.tensor_tensor(out=T["FR"], in0=T["A"], in1=T["KF"], op=ALU.subtract), ["A", "KF"], ["FR"])
            V("dve", lambda e: e.tensor_scalar(out=T["LT"], in0=T["FR"], scalar1=0.0, scalar2=None, op0=ALU.is_lt), ["FR"], ["LT"])
            V("dve", lambda e: e.tensor_tensor(out=T["FR"], in0=T["FR"], in1=T["LT"], op=ALU.add), ["FR", "LT"], ["FR"])
            V("dve", lambda e: e.tensor_scalar(out=T["FR2"], in0=T["FR"], scalar1=0.25, scalar2=None, op0=ALU.add), ["FR"], ["FR2"])
            V("dve", lambda e: e.tensor_scalar(out=T["LT"], in0=T["FR2"], scalar1=1.0, scalar2=None, op0=ALU.is_ge), ["FR2"], ["LT"])
            V("dve", lambda e: e.tensor_tensor(out=T["FR2"], in0=T["FR2"], in1=T["LT"], op=ALU.subtract), ["FR2", "LT"], ["FR2"])
            for nm in ("FR", "FR2"):
                V("dve", lambda e, nm=nm: e.tensor_scalar(out=T[nm], in0=T[nm], scalar1=TWO_PI, scalar2=-PI, op0=ALU.mult, op1=ALU.add), [nm], [nm])
                V("dve", lambda e, nm=nm: e.tensor_scalar(out=T[nm], in0=T[nm], scalar1=-3.1415925, scalar2=3.1415925, op0=ALU.max, op1=ALU.min), [nm], [nm])
                V("act", lambda e, nm=nm: e.activation(out=T[nm], in_=T[nm], func=AF.Sin), [nm], [nm])
                V("dve", lambda e, nm=nm: e.scalar_tensor_tensor(out=fl(T[nm]), in0=fl(T[nm]), scalar=-1.0, in1=fl(T["MAG"]),
                                                                 op0=ALU.mult, op1=ALU.mult), [nm, "MAG"], [nm])
            XA, YA = T["FR2"], T["FR"]
            XAk, YAk = "FR2", "FR"
            X1, Y1 = XA[:, :, 8], YA[:, :, 8]
            V("dve", lambda e: e.tensor_scalar(out=sm_["nr"], in0=X1, scalar1=-1.0, scalar2=None, op0=ALU.add), [XAk], ["nr"])
            V("dve", lambda e: e.tensor_tensor(out=sm_["den"], in0=pr_("are"), in1=pr_("are"), op=ALU.mult), ["PAR"], ["den"])
            V("dve", lambda e: e.tensor_tensor(out=sm_["t1"], in0=pr_("aim"), in1=pr_("aim"), op=ALU.mult), ["PAR"], ["st1"])
            V("dve", lambda e: e.tensor_tensor(out=sm_["den"], in0=sm_["den"], in1=sm_["t1"], op=ALU.add), ["den", "st1"], ["den"])
            V("dve", lambda e: e.reciprocal(out=sm_["den"], in_=sm_["den"]), ["den"], ["den"])
            V("dve", lambda e: e.tensor_tensor(out=sm_["t1"], in0=sm_["nr"], in1=pr_("are"), op=ALU.mult), ["nr", "PAR"], ["st1"])
            V("dve", lambda e: e.tensor_tensor(out=sm_["t2"], in0=Y1, in1=pr_("aim"), op=ALU.mult), [YAk, "PAR"], ["st2"])
            V("dve", lambda e: e.tensor_tensor(out=sm_["t1"], in0=sm_["t1"], in1=sm_["t2"], op=ALU.add), ["st1", "st2"], ["st1"])
            V("dve", lambda e: e.tensor_tensor(out=sm_["qr"], in0=sm_["t1"], in1=sm_["den"], op=ALU.mult), ["st1", "den"], ["qr"])
            V("dve", lambda e: e.tensor_tensor(out=sm_["t1"], in0=Y1, in1=pr_("are"), op=ALU.mult), [YAk, "PAR"], ["st1"])
            V("dve", lambda e: e.tensor_tensor(out=sm_["t2"], in0=sm_["nr"], in1=pr_("aim"), op=ALU.mult), ["nr", "PAR"], ["st2"])
            V("dve", lambda e: e.tensor_tensor(out=sm_["t1"], in0=sm_["t1"], in1=sm_["t2"], op=ALU.subtract), ["st1", "st2"], ["st1"])
            V("dve", lambda e: e.tensor_tensor(out=sm_["qi"], in0=sm_["t1"], in1=sm_["den"], op=ALU.mult), ["st1", "den"], ["qi"])
            bp3 = pr_("bp").rearrange("p (a b) -> p a b", a=16)
            bq3 = pr_("bq").rearrange("p (a b) -> p a b", a=16)
            V("dve", lambda e: e.tensor_scalar(out=big["Q0"], in0=bq3, scalar1=sgnA, scalar2=None, op0=ALU.mult), ["PAR"], ["Q0"])
            V("dve", lambda e: e.tensor_tensor(out=big["t1"], in0=bp3, in1=b3(sm_["qr"], 16), op=ALU.mult), ["PAR", "qr"], ["bt1"])
            V("dve", lambda e: e.tensor_tensor(out=big["t2"], in0=big["Q0"], in1=b3(sm_["qi"], 16), op=ALU.mult), ["Q0", "qi"], ["bt2"])
            V("dve", lambda e: e.tensor_tensor(out=big["BP"], in0=big["t1"], in1=big["t2"], op=ALU.add), ["bt1", "bt2"], ["BP"])
            V("dve", lambda e: e.tensor_tensor(out=big["t1"], in0=big["Q0"], in1=b3(sm_["qr"], 16), op=ALU.mult), ["Q0", "qr"], ["bt1"])
            V("dve", lambda e: e.tensor_tensor(out=big["t2"], in0=bp3, in1=b3(sm_["qi"], 16), op=ALU.mult), ["PAR", "qi"], ["bt2"])
            V("dve", lambda e: e.tensor_tensor(out=big["BQ"], in0=big["t1"], in1=big["t2"], op=ALU.subtract), ["bt1", "bt2"], ["BQ"])
            Bq4 = Bq.rearrange("p g (a b) -> p g a b", a=8)
            for gh in range(2):
                gs = slice(8 * gh, 8 * gh + 8)
                xb_ = XA[:, gs, 16:24].unsqueeze(3).broadcast_to([128, 8, 8, 16])
                yb_ = YA[:, gs, 16:24].unsqueeze(3).broadcast_to([128, 8, 8, 16])
                bpg = big["BP"][:, gs, :].unsqueeze(2).broadcast_to([128, 8, 8, 16])
                bqg = big["BQ"][:, gs, :].unsqueeze(2).broadcast_to([128, 8, 8, 16])
                V("dve", lambda e, xb_=xb_, bpg=bpg: e.tensor_tensor(out=tb["b1"], in0=bpg, in1=xb_, op=ALU.mult), ["BP", XAk, "Bq", "MAG", "A", "KF", "LT"], ["b1"])
                V("dve", lambda e, yb_=yb_, bqg=bqg: e.tensor_tensor(out=tb["b2"], in0=bqg, in1=yb_, op=ALU.mult), ["BQ", YAk, "Bq", "MAG", "A", "KF", "LT"], ["b2"])
                V("dve", lambda e, gs=gs: e.tensor_tensor(out=Bq4[:, gs], in0=tb["b1"], in1=tb["b2"], op=ALU.add), ["b1", "b2"], ["Bq"])
            cp3 = pr_("cp").rearrange("p (a b) -> p a b", a=16)
            cq3 = pr_("cq").rearrange("p (a b) -> p a b", a=16)
            V("dve", lambda e: e.tensor_scalar(out=big["CP"], in0=cp3, scalar1=sgnC, scalar2=None, op0=ALU.mult), ["PAR"], ["CP"])
            V("dve", lambda e: e.tensor_scalar(out=big["CQ"], in0=cq3, scalar1=-1.0, scalar2=None, op0=ALU.mult), ["PAR"], ["CQ"])
            for gh in range(2):
                gs = slice(8 * gh, 8 * gh + 8)
                xe_ = XA[:, gs, 0:16].unsqueeze(3).broadcast_to([128, 8, 16, 16])
                ye_ = YA[:, gs, 0:16].unsqueeze(3).broadcast_to([128, 8, 16, 16])
                cpg = big["CP"][:, gs, :].unsqueeze(2).broadcast_to([128, 8, 16, 16])
                cqg = big["CQ"][:, gs, :].unsqueeze(2).broadcast_to([128, 8, 16, 16])
                V("dve", lambda e, xe_=xe_, cpg=cpg: e.tensor_tensor(out=te["e1"], in0=cpg, in1=xe_, op=ALU.mult), ["CP", XAk, "E", "b1", "b2", "Bq"], ["e1"])
                V("dve", lambda e, ye_=ye_, cqg=cqg: e.tensor_tensor(out=te["e2"], in0=cqg, in1=ye_, op=ALU.mult), ["CQ", YAk, "E", "b1", "b2", "Bq"], ["e2"])
                V("dve", lambda e, gs=gs: e.tensor_tensor(out=E[:, gs], in0=te["e1"], in1=te["e2"], op=ALU.add), ["e1", "e2"], ["E"])
            V("dve", lambda e: e.tensor_copy(out=XR, in_=XA[:, :, 24:32]), [XAk], ["XR"])
            V("dve", lambda e: e.tensor_scalar(out=YR, in0=YA[:, :, 24:32], scalar1=sgnC, scalar2=None, op0=ALU.mult), [YAk, "PAR"], ["YR"])
            pb = [psum[:, bkk, :].bitcast(BF16) for bkk in range(8)]
            for g in range(16):
                bk = g // 8
                V("pe", lambda e, g=g, bk=bk: e.transpose(pb[bk][:, (g % 8) * 128:(g % 8 + 1) * 128], Bq[:, g, :], identb), ["Bq", "identb"], [("ps", bk)])
            for bk in range(2):
                V("act", lambda e, bk=bk: e.copy(out=Bs[:, 8 * bk:8 * bk + 8, :], in_=pb[bk].rearrange("p (a b) -> p a b", a=8)), [], [("ps", bk), "Bs"])
            tmk = par("stm")
            for g in range(16):
                bk = 2 + g // 4
                V("pe", lambda e, g=g, bk=bk: e.matmul(psum[:, bk, (g % 4) * 128:(g % 4 + 1) * 128], Bq[:, g, :],
                                                        E[:, g, 0:8, :].rearrange("p a b -> p (a b)"), start=True, stop=True), ["Bq", "E"], [("ps", bk)])
            for q4 in range(4):
                bk = 2 + q4
                V("dve", lambda e, q4=q4, bk=bk: e.tensor_tensor(
                    out=W[:, 4 * q4:4 * q4 + 4, :], in0=psum[:, bk, :].rearrange("p (a b) -> p a b", a=4),
                    in1=tmk.unsqueeze(1).broadcast_to([128, 4, 128]), op=ALU.mult), ["PAR"], [("ps", bk), "W"])
            for g in range(16):
                V("dve", lambda e, g=g: e.scalar_tensor_tensor(out=W[:, g, :], in0=identf, scalar=pr_("dcol")[:, g:g + 1], in1=W[:, g, :],
                                                              op0=ALU.mult, op1=ALU.add), ["W", "PAR", "identf"], ["W"])
            S.barrier()
            M.off = m1
            X32 = M.alloc([16, 257], F32)
            Xb = M.alloc([16, 257], BF16)
            for half in range(2):
                for sp_ in range(4):
                    bk = 4 * (half % 2) + sp_
                    for sg2 in range(2):
                        sig = 2 * sp_ + sg2
                        for k in range(8):
                            V("pe", lambda e, half=half, sig=sig, sg2=sg2, bk=bk, k=k: e.matmul(
                                psum[:, bk, sg2 * 256:(sg2 + 1) * 256], HT[:, k, 1024 * half + sig:1024 * (half + 1):8], wssm[:, k, :],
                                start=(k == 0), stop=(k == 7)), ["wssm", ("HT", k, 2 * half), ("HT", k, 2 * half + 1)], [("ps", bk)])
                    for sg2 in range(2):
                        sig = 2 * sp_ + sg2
                        src = psum[:, bk, sg2 * 256:(sg2 + 1) * 256].rearrange("p (a b) -> p a b", a=16)
                        dst = Abuf[:, half, :, sig, :]
                        if sg2 == 0:
                            V("act", lambda e, src=src, dst=dst: e.copy(out=dst, in_=src), [], [("ps", bk), ("Abuf", half)])
                        else:
                            V("dve", lambda e, src=src, dst=dst: e.tensor_copy(out=dst, in_=src), [], [("ps", bk), ("Abuf", half)])
            for half in range(2):
                for gh in range(2):
                    bk = 2 * half + gh
                    for gg in range(8):
                        g = 8 * gh + gg
                        V("pe", lambda e, half=half, g=g, gg=gg, bk=bk: e.transpose(
                            pb[bk][:, gg * 128:(gg + 1) * 128], Abuf[:, half, g].rearrange("p a b -> p (a b)"), identb),
                            [("Abuf", half), "identb"], [("ps", bk)])
                    dst = U[:, 8 * gh:8 * gh + 8, 128 * half:128 * (half + 1)]
                    src = pb[bk].rearrange("p (a b) -> p a b", a=8)
                    if gh == 0:
                        V("act", lambda e, src=src, dst=dst: e.copy(out=dst, in_=src), [], [("ps", bk), "U"])
                    else:
                        V("dve", lambda e, src=src, dst=dst: e.tensor_copy(out=dst, in_=src), [], [("ps", bk), "U"])
            V("dve", lambda e: e.memset(X32[:, :, 0:1], 0.0), [], ["X32z"])
            for g in range(16):
                bk = g // 2
                V("pe", lambda e, g=g, bk=bk: e.matmul(psum[:, bk, (g % 2) * 256:(g % 2 + 1) * 256], Bs[:, g, :], U[:, g, :], start=True, stop=True),
                  ["Bs", "U"], [("ps", bk)])
            for g2 in range(8):
                dst = X32[:, 2 * g2:2 * g2 + 2, 1:257]
                src = psum[:, g2, :].rearrange("p (a b) -> p a b", a=2)
                if g2 % 2 == 0:
                    V("act", lambda e, src=src, dst=dst: e.copy(out=dst, in_=src), [], [("ps", g2), "X32"])
                else:
                    V("dve", lambda e, src=src, dst=dst: e.tensor_copy(out=dst, in_=src), [], [("ps", g2), "X32"])
            V("act", lambda e: e.copy(out=Xb, in_=X32), ["X32", "X32z"], ["Xb"])
            swp = par("sswap")
            for lv in range(8):
                sh = 1 << lv
                n = 256 - sh
                for ph in range(2):
                    for chh in range(2):
                        pp = slice(64 * ph, 64 * ph + 64)
                        cs = slice(64 * chh, 64 * chh + 64)
                        src = identf if ph == chh else swp
                        coef = XR if ph == chh else YR
                        V("dve", lambda e, pp=pp, cs=cs, src=src, coef=coef, lv=lv: e.tensor_tensor(
                            out=Rm[pp, :, cs], in0=src[pp, cs].unsqueeze(1).broadcast_to([64, 16, 64]),
                            in1=coef[pp, :, lv:lv + 1].broadcast_to([64, 16, 64]), op=ALU.mult),
                            ["XR", "YR", "identf", "PAR"], ["Rm"])
                for g in range(16):
                    bk = g // 2
                    V("pe", lambda e, g=g, bk=bk, n=n: e.matmul(psum[:, bk, (g % 2) * 256:(g % 2) * 256 + n], Rm[:, g, :], Xb[:, g, 1:1 + n],
                                                                 start=True, stop=True), ["Rm", "Xb"], [("ps", bk)])
                for g2 in range(8):
                    V("dve", lambda e, g2=g2, n=n, sh=sh: e.tensor_tensor(
                        out=X32[:, 2 * g2:2 * g2 + 2, 1 + sh:257], in0=X32[:, 2 * g2:2 * g2 + 2, 1 + sh:257],
                        in1=psum[:, g2, :].rearrange("p (a b) -> p a b", a=2)[:, :, 0:n], op=ALU.add), ["X32"], [("ps", g2), "X32"])
                V("act", lambda e: e.copy(out=Xb, in_=X32), ["X32", "X32z"], ["Xb"])
            S.barrier()
            Ytok = Abuf.rearrange("p h g a b -> p h (g a b)").rearrange("p h (t c) -> p h t c", t=8)
            for half in range(2):
                for q4 in range(4):
                    bk = 4 * (half % 2) + q4
                    for gg in range(4):
                        g = 4 * q4 + gg
                        V("pe", lambda e, half=half, g=g, gg=gg, bk=bk: e.matmul(
                            psum[:, bk, gg * 128:(gg + 1) * 128], Xb[:, g, 128 * half:128 * half + 128],
                            E[:, g, 8:16, :].rearrange("p a b -> p (a b)"), start=True, stop=False), ["Xb", "E"], [("ps", bk)])
                        V("pe", lambda e, half=half, g=g, gg=gg, bk=bk: e.matmul(
                            psum[:, bk, gg * 128:(gg + 1) * 128], U[:, g, 128 * half:128 * half + 128], W[:, g, :],
                            start=False, stop=True), ["U", "W"], [("ps", bk)])
                    src = psum[:, bk, :].rearrange("p (g t c) -> p g t c", g=4, t=8)
                    dst = Ytok[:, half, :, 64 * q4:64 * q4 + 64].rearrange("p t (g c) -> p g t c", g=4)
                    V("act", lambda e, src=src, dst=dst: e.activation(out=dst, in_=src, func=AF.Gelu), [], [("ps", bk), ("Ytok", half)])
            S.barrier()
            M.off = m1
            YT = M.alloc([2, SEQ], BF16)
            for half in range(2):
                for cc in range(2):
                    bk = 2 * half + cc
                    for tau in range(8):
                        V("pe", lambda e, half=half, cc=cc, tau=tau, bk=bk: e.transpose(
                            pb[bk][:, tau * 128:(tau + 1) * 128], Ytok[:, half, tau, cc * 128:(cc + 1) * 128], identb),
                            [("Ytok", half), "identb"], [("ps", bk)])
                    dst = YT[:, cc, 1024 * half:1024 * (half + 1)].rearrange("p (k t) -> p t k", t=8)
                    src = pb[bk].rearrange("p (t k) -> p t k", t=8)
                    if cc == 0:
                        V("act", lambda e, src=src, dst=dst: e.copy(out=dst, in_=src), [], [("ps", bk), ("YT", half)])
                    else:
                        V("dve", lambda e, src=src, dst=dst: e.tensor_copy(out=dst, in_=src), [], [("ps", bk), ("YT", half)])
            ms = U.rearrange("p a b -> p (a b)").rearrange("p (c n) -> p c n", c=2)
            S.barrier()
            for t in range(4):
                ts_ = slice(t * 512, (t + 1) * 512)
                for oc in range(4):
                    for kc in range(2):
                        V("pe", lambda e, oc=oc, kc=kc, ts_=ts_: e.matmul(PS(oc), wglu[:, kc, oc * 128:(oc + 1) * 128], YT[:, kc, ts_],
                                                                         start=(kc == 0), stop=(kc == 1)), ["wglu", ("YT", t // 2)], [("ps", oc)])
                for cc in range(2):
                    sg = sgf[cc]
                    V("act", lambda e, sg=sg, cc=cc: e.activation(out=sg, in_=PS(2 + cc), func=AF.Sigmoid), [], [("ps", 2 + cc), ("sgf", cc)])
                    V("dve", lambda e, sg=sg, cc=cc: e.tensor_tensor(out=zf[:, cc, :], in0=sg, in1=PS(cc), op=ALU.mult),
                      [("sgf", cc)], [("ps", cc), "zf"])
                gnorm_tile(zf, "zf", 2, par(f"sgn{l}"), ms[:, :, ts_], ("ms", t), 6)
            wout_part(l, s, ms, lambda t: ("ms", t), 2, wo, "wo")
            S.barrier()
            M.off = m0

        def mixer(l, s):
            normmod(l, s, 1)
            if "conv" in MIXPARTS:
                conv_part(l, s)
            if "ssm" in MIXPARTS:
                ssm_part(l, s)
            if "att" in MIXPARTS:
                att_part(l, s)

        for s in range(2):
            load_x(s)
            for (l, kind) in phases:
                if kind == "ffn1":
                    ffn(l, s, 1)
                elif kind == "ffn2":
                    ffn(l, s, 2)
                else:
                    mixer(l, s)
            store_x(s, last)
        S.finish()
        nsem = S.emit(st)
        print(f"[build] ops={len(S.ops)} sems={nsem}")
    return nc


MIXPARTS = {"conv", "att", "ssm"}


ALL_PHASES = [(l, k) for l in range(2) for k in ("ffn1", "mix", "ffn2")]


def prep_weights(inp):
    w = {}
    for j, nm in ((1, "ffn1"), (2, "ffn2")):
        gu = inp[f"{nm}_w_gu"]
        g = gu[:, :, :DFF].reshape(2, 8, 128, NF, 128)
        u = gu[:, :, DFF:].reshape(2, 8, 128, NF, 128)
        cat = np.concatenate([g, u], axis=-1)
        w[f"wgu{j}"] = np.ascontiguousarray(cat.transpose(0, 3, 2, 1, 4)).reshape(2, NF, 128, 2048)
        w[f"wdn{j}"] = np.ascontiguousarray(inp[f"{nm}_w_down"]).reshape(2, NF, 128, D)
    w["mod_w"] = np.ascontiguousarray(inp["mod_w"])
    for l in range(2):
        wi = inp["w_in"][l]
        q, k, v, qi, ki, wv, ssm, ca, cg = (wi[:, 0:512], wi[:, 512:576], wi[:, 576:640], wi[:, 640:896], wi[:, 896:928],
                                            wi[:, 928:936], wi[:, 936:1192], wi[:, 1192:1448], wi[:, 1448:1704])
        cat = np.concatenate([q, k, k, ki, ki, ki, ki, qi, ca, cg, v, wv, ssm], axis=1)
        assert cat.shape[1] == NWIN
        w[f"win{l}"] = np.ascontiguousarray(cat.reshape(8, 128, NWIN).transpose(1, 0, 2))
        w[f"wout{l}"] = np.ascontiguousarray(inp["w_out"][l].reshape(8, 128, D).transpose(1, 0, 2))
        w[f"wpw{l}"] = np.ascontiguousarray(inp["conv_w_pw"][l].reshape(2, 128, 256).transpose(1, 0, 2))
        w[f"wglu{l}"] = np.ascontiguousarray(inp["ssm_w_glu"][l].reshape(2, 128, 512).transpose(1, 0, 2))
    return w


def run_phases(inp, xcur, phases, first, last, shared=None):
    shared = shared if shared is not None else prep_weights(inp)
    packs = [pack_small(inp, c) for c in range(8)]
    nc = build(phases, first, last, packs[0].off, packs[0].n)
    in_maps = []
    need = {"mod_w"}
    for l, k in phases:
        if k == "ffn1":
            need |= {"wgu1", "wdn1"}
        elif k == "ffn2":
            need |= {"wgu2", "wdn2"}
        else:
            need |= {f"win{l}", f"wout{l}", f"wpw{l}", f"wglu{l}"}
    shared = {k: v for k, v in shared.items() if k in need}
    for c in range(8):
        m = dict(shared)
        m["x"] = np.ascontiguousarray(xcur[2 * c:2 * c + 2])
        m["par"] = packs[c].array()
        in_maps.append(m)
    res = run_bass_kernel_spmd(nc, in_maps, core_ids=list(range(8)))
    return np.concatenate([r["y"] for r in res.results], axis=0)


def kernel(**inputs):
    inp = {k: np.asarray(v) for k, v in inputs.items()}
    x = np.ascontiguousarray(inp["x"], dtype=np.float32)
    return run_phases(inp, x, ALL_PHASES, True, True)
```

```python
import numpy as np
from contextlib import ExitStack
import concourse.bass as bass
import concourse.mybir as mybir
from concourse.bass_utils import run_bass_kernel_spmd

F32 = mybir.dt.float32
BF16 = mybir.dt.bfloat16
ALU = mybir.AluOpType
AF = mybir.ActivationFunctionType
AX = mybir.AxisListType

ROT = 2048
DMA_SLOTS = 12
DMA_ROT = 120

D = 1024
SEQ = 2048
DFF = 2816
NF = 22
NWIN = 1864
C_Q, C_KK, C_KI, C_QI, C_A, C_G, C_VW, C_SSM = 0, 512, 640, 768, 1024, 1280, 1536, 1608
EPS = 1e-6


class Op:
    __slots__ = ("eng", "fn", "reads", "writes", "dma", "deps", "inc", "cnt",
                 "dslot", "dgen", "dval", "waits", "clock", "idx")


class Sched:
    def __init__(self, nc):
        self.nc = nc
        self.ops = []
        self.last_w = {}
        self.readers = {}
        self.dma_n = 0
        self.dma_h = 0
        self.dma_prev = {}

    def add(self, eng, fn, reads=(), writes=(), dma=False):
        op = Op()
        op.eng, op.fn, op.dma = eng, fn, dma
        op.reads, op.writes = tuple(reads), tuple(writes)
        op.inc = False
        op.idx = len(self.ops)
        deps = {}
        for k in op.reads:
            w = self.last_w.get(k)
            if w is not None:
                deps[w.idx] = (w, True)
        for k in op.writes:
            w = self.last_w.get(k)
            if w is not None and w.idx not in deps:
                deps[w.idx] = (w, False)
            for r in self.readers.get(k, ()):
                if r.idx not in deps:
                    deps[r.idx] = (r, False)
        if dma:
            half = DMA_SLOTS // 2
            if eng == "pool":
                slot = self.dma_n % half
                self.dma_n += 1
            else:
                slot = half + self.dma_h % half
                self.dma_h += 1
            prev = self.dma_prev.get(slot)
            if prev is not None and prev.idx not in deps:
                deps[prev.idx] = (prev, True)
            self.dma_prev[slot] = op
            op.dslot = slot
            op.inc = True
        need = []
        for d, raw in deps.values():
            if d is op:
                continue
            if d.dma or op.dma or d.eng != eng:
                need.append(d)
            elif raw and eng != "pe":
                need.append(d)
        for d in need:
            d.inc = True
        op.deps = need
        for k in op.reads:
            self.readers.setdefault(k, []).append(op)
        for k in op.writes:
            self.last_w[k] = op
            self.readers[k] = []
        self.ops.append(op)
        return op

    def barrier(self):
        keys = set(self.last_w.keys()) | set(self.readers.keys())
        keys.discard("BAR")
        self.add("dve", lambda e: e.nop(), reads=list(keys), writes=["BAR"])
        for en in ("pe", "act", "pool", "sp"):
            self.add(en, lambda e: e.nop(), reads=["BAR"], writes=[("BARL", en)])
        bar = self.last_w["BAR"]
        self.last_w = {"BAR": bar}
        self.readers = {}
        self._bar = bar

    def finish(self, eng="sp"):
        keys = [k for k, w in self.last_w.items() if w.dma]
        self.add(eng, lambda e: e.nop(), reads=keys)

    def emit(self, stack):
        nc = self.nc
        engs = ["pe", "act", "dve", "pool", "sp"]
        cnt = {e: 0 for e in engs}
        dcount = {}
        for op in self.ops:
            if op.dma:
                g = dcount.get(op.dslot, 0)
                op.dgen, op.dval = g // DMA_ROT, 16 * (g % DMA_ROT + 1)
                dcount[op.dslot] = g + 1
            elif op.inc:
                cnt[op.eng] += 1
                op.cnt = cnt[op.eng]
        sems = {}

        def sem(name):
            if name not in sems:
                sems[name] = stack.enter_context(nc.semaphore(name))
            return sems[name]

        known = {e: {} for e in engs}
        for op in self.ops:
            kn = known[op.eng]
            waits = []
            for d in sorted(op.deps, key=lambda o: o.idx):
                if d.dma:
                    key = ("d", d.dslot, d.dgen)
                    val = d.dval
                else:
                    key = d.eng
                    val = d.cnt
                if kn.get(key, 0) >= val:
                    continue
                waits.append(d)
                for k2, v2 in d.clock.items():
                    if kn.get(k2, 0) < v2:
                        kn[k2] = v2
                kn[key] = val
            op.waits = waits
            if op.inc:
                op.clock = dict(kn)
                if op.dma:
                    op.clock[("d", op.dslot, op.dgen)] = op.dval
                else:
                    op.clock[op.eng] = op.cnt
            else:
                op.clock = None

        for op in self.ops:
            if op.inc:
                if op.dma:
                    sem(f"d{op.dslot}_{op.dgen}")
                else:
                    sem(f"{op.eng}_{(op.cnt - 1) // ROT}")

        def emit_engine(ename, e):
            for op in self.ops:
                if op.eng != ename:
                    continue
                for d in op.waits:
                    if d.dma:
                        e.wait_ge(sem(f"d{d.dslot}_{d.dgen}"), d.dval)
                    else:
                        c = d.cnt - 1
                        e.wait_ge(sem(f"{d.eng}_{c // ROT}"), c % ROT + 1)
                ins = op.fn(e)
                if op.inc:
                    if op.dma:
                        ins.then_inc(sem(f"d{op.dslot}_{op.dgen}"), 16)
                    else:
                        c = op.cnt - 1
                        ins.then_inc(sem(f"{op.eng}_{c // ROT}"), 1)

        block = stack.enter_context(nc.Block())

        @block.tensor
        def _(e):
            emit_engine("pe", e)

        @block.scalar
        def _(e):
            emit_engine("act", e)

        @block.vector
        def _(e):
            emit_engine("dve", e)

        @block.gpsimd
        def _(e):
            emit_engine("pool", e)

        @block.sync
        def _(e):
            emit_engine("sp", e)
        return len(sems)


class Mem:
    def __init__(self, pool, nbytes):
        self.pool, self.nbytes, self.off = pool, nbytes, 0

    def view(self, off, shape, dtype):
        o = self.off
        self.off = off
        v = self.alloc(shape, dtype)
        self.off = o
        return v

    def alloc(self, shape, dtype):
        isz = 4 if dtype == F32 else 2
        n = int(np.prod(shape))
        nb = (n * isz + 63) // 64 * 64
        assert self.off + nb <= self.nbytes, (self.off, nb, self.nbytes)
        v = self.pool[:, self.off // 2:(self.off + n * isz) // 2]
        self.off += nb
        if dtype == F32:
            v = v.bitcast(F32)
        if len(shape) > 1:
            names = [f"a{i}" for i in range(len(shape))]
            kw = {n: int(d) for n, d in zip(names[:-1], shape[:-1])}
            v = v.rearrange(f"p ({' '.join(names)}) -> p {' '.join(names)}", **kw)
        return v


def _fm(v, nch):
    return np.ascontiguousarray(v.reshape(nch, 128).T)


class Pack:
    def __init__(self):
        self.cols, self.off, self.n = [], {}, 0

    def add(self, name, arr):
        arr = np.asarray(arr, np.float32).reshape(128, -1)
        self.off[name] = (self.n, arr.shape[1])
        self.cols.append(arr)
        self.n += arr.shape[1]

    def array(self):
        return np.ascontiguousarray(np.concatenate(self.cols, axis=1))


def pack_small(inp, core):
    P = Pack()
    b0 = 2 * core
    P.add("cT", np.stack([_fm(inp["c"][b0 + s], 8) for s in range(2)], axis=-1))
    for l in range(2):
        P.add(f"modb{l}", _fm(inp["mod_b"][l], 72))
        for j, nm in enumerate(("ffn1_norm", "mix_norm", "ffn2_norm")):
            P.add(f"ng{l}{j}", _fm(inp[nm][l], 8))
    P.add("fin", _fm(inp["final_norm"], 8))
    for l in range(2):
        P.add(f"cw{l}", inp["conv_w_dw"][l].T.reshape(2, 128, 31).transpose(1, 0, 2))
        P.add(f"cb{l}", _fm(inp["conv_b_dw"][l], 2))
        P.add(f"clg{l}", _fm(inp["conv_ln_g"][l], 2))
        P.add(f"clb{l}", _fm(inp["conv_ln_b"][l], 2))
        P.add(f"cgn{l}", _fm(inp["conv_out_norm"][l], 2))
        P.add(f"sgn{l}", _fm(inp["ssm_out_norm"][l], 2))
        P.add(f"agn{l}", _fm(inp["att_out_norm"][l], 4))
        dup = lambda a: np.concatenate([a, a], axis=0)
        P.add(f"sare{l}", dup(inp["ssm_a_re"][l].T))
        P.add(f"saim{l}", dup(inp["ssm_a_im"][l].T))
        P.add(f"sldt{l}", np.broadcast_to(inp["ssm_log_dt"][l][None, :], (128, 16)))
        bre = inp["ssm_b_re"][l].transpose(1, 0, 2).reshape(64, 256)
        bim = inp["ssm_b_im"][l].transpose(1, 0, 2).reshape(64, 256)
        P.add(f"sbp{l}", np.concatenate([bre, bim], axis=0))
        P.add(f"sbq{l}", np.concatenate([bim, bre], axis=0))
        cre = inp["ssm_c_re"][l].transpose(2, 0, 1).reshape(64, 256)
        cim = inp["ssm_c_im"][l].transpose(2, 0, 1).reshape(64, 256)
        P.add(f"scp{l}", np.concatenate([cre, cim], axis=0))
        P.add(f"scq{l}", np.concatenate([cim, cre], axis=0))
        P.add(f"sdcol{l}", np.tile(inp["ssm_d"][l].T, (8, 1)))
    jall = np.concatenate([np.arange(-7, 9), np.arange(7, -1, -1), 8 * 2 ** np.arange(8)]).astype(np.float32)
    P.add("sjall", np.broadcast_to(jall[None, :], (128, 32)))
    sg = np.ones((128, 1), np.float32); sg[:64] = -1
    P.add("ssgnA", sg)
    P.add("ssgnC", -sg)
    sig = np.arange(128) // 16
    P.add("stm", (sig[None, :] >= sig[:, None]).astype(np.float32))
    sw = np.zeros((128, 128), np.float32); sw[np.arange(128), (np.arange(128) + 64) % 128] = 1
    P.add("sswap", sw)
    return P


def build(phases, first, last, poff, npar):
    nc = bass.Bass("TRN2", target_bir_lowering=False)
    x_d = nc.dram_tensor("x", [2, SEQ, D], F32, kind="ExternalInput").ap()
    par_d = nc.dram_tensor("par", [128, npar], F32, kind="ExternalInput").ap()
    modw_d = nc.dram_tensor("mod_w", [2, D, 9 * D], F32, kind="ExternalInput").ap()
    kinds = {k for _, k in phases}
    wgu_d = {j: nc.dram_tensor(f"wgu{j}", [2, NF, 128, 2048], F32, kind="ExternalInput").ap()
             for j in (1, 2) if f"ffn{j}" in kinds}
    wdn_d = {j: nc.dram_tensor(f"wdn{j}", [2, NF, 128, D], F32, kind="ExternalInput").ap()
             for j in (1, 2) if f"ffn{j}" in kinds}
    y_d = nc.dram_tensor("y", [2, SEQ, D], F32, kind="ExternalOutput").ap()
    mixl = sorted({l for l, k in phases if k == "mix"})
    win_d = {l: nc.dram_tensor(f"win{l}", [128, 8, NWIN], F32, kind="ExternalInput").ap() for l in mixl}
    wout_d = {l: nc.dram_tensor(f"wout{l}", [128, 8, D], F32, kind="ExternalInput").ap() for l in mixl}
    wpw_d = {l: nc.dram_tensor(f"wpw{l}", [128, 2, 256], F32, kind="ExternalInput").ap() for l in mixl}
    wglu_d = {l: nc.dram_tensor(f"wglu{l}", [128, 2, 512], F32, kind="ExternalInput").ap() for l in mixl}

    st = ExitStack()
    with st:
        S = Sched(nc)
        POOLB = 206 * 1024
        pool = st.enter_context(nc.sbuf_tensor("pool", [128, POOLB // 2], BF16))
        psum = st.enter_context(nc.psum_tensor("psum", [128, 8, 512], F32))
        M = Mem(pool, POOLB)

        def PS(b):
            return psum[:, b, :]

        XT = M.alloc([8, SEQ], F32)
        HT = M.alloc([8, SEQ], BF16)
        identf = M.alloc([128], F32)
        identb = M.alloc([128], BF16)
        onesb = M.alloc([128], BF16)
        PAR = M.alloc([npar], F32)
        MOD = M.alloc([2, 72, 2], F32)
        DER = M.alloc([2, 2, 3, 3, 8], F32)
        condb = M.alloc([8, 2], BF16)
        sq = [M.alloc([512], BF16) for _ in range(2)]
        rs = M.alloc([512], F32)
        rstd = M.alloc([512], F32)
        tmpf = [M.alloc([512], F32) for _ in range(2)]
        sgf = [M.alloc([512], F32) for _ in range(2)]
        big0 = M.off

        def par(name):
            o, n = poff[name]
            return PAR[:, o:o + n]

        S.add("sp", lambda e: e.dma_start(out=PAR, in_=par_d), writes=["PAR"], dma=True)
        S.add("pool", lambda e: e.memset(identf, 0.0), writes=["identf"])
        S.add("pool", lambda e: e.affine_select(out=identf, in_=identf, pattern=[[-1, 128]],
                                                compare_op=ALU.not_equal, fill=1.0, base=0,
                                                channel_multiplier=1), reads=["identf"], writes=["identf"])
        S.add("dve", lambda e: e.tensor_copy(out=identb, in_=identf), reads=["identf"], writes=["identb"])
        S.add("dve", lambda e: e.memset(onesb, 1.0), writes=["onesb"])

        S.add("act", lambda e: e.activation(out=condb, in_=par("cT").rearrange("p (a b) -> p a b", a=8),
                                            func=AF.Silu), reads=["PAR"], writes=["condb"])
        layers = sorted({l for l, _ in phases})
        mslab = [M.alloc([8, 1024], BF16) for _ in range(2)]
        for l in layers:
            for sl in range(9):
                buf = mslab[sl % 2]
                S.add("pool", lambda e, buf=buf, l=l, sl=sl: e.dma_start(
                    out=buf, in_=modw_d[l, :, sl * 1024:(sl + 1) * 1024].rearrange("(k p) n -> p k n", p=128)),
                    writes=[("mslab", sl % 2)], dma=True)
                for fc in range(8):
                    ch = sl * 8 + fc
                    for k in range(8):
                        S.add("pe", lambda e, buf=buf, fc=fc, k=k, ch=ch: e.matmul(
                            psum[:, 0, 2 * ch:2 * ch + 2], buf[:, k, fc * 128:(fc + 1) * 128], condb[:, k, :],
                            start=(k == 0), stop=(k == 7)),
                            reads=[("mslab", sl % 2), "condb"], writes=[("ps", 0)])
            for s in range(2):
                S.add("dve", lambda e, l=l, s=s: e.tensor_tensor(
                    out=MOD[:, l, :, s], in0=psum[:, 0, s:144:2], in1=par(f"modb{l}"), op=ALU.add),
                    reads=["PAR"], writes=[("ps", 0), "MOD"])
            for s in range(2):
                for j in range(3):
                    a_, sh_, g_ = DER[:, l, s, j, 0, :], DER[:, l, s, j, 1, :], DER[:, l, s, j, 2, :]
                    c0 = 3 * j * 8
                    S.add("dve", lambda e, a_=a_, l=l, s=s, j=j, c0=c0: e.scalar_tensor_tensor(
                        out=a_, in0=MOD[:, l, c0 + 8:c0 + 16, s], scalar=1.0, in1=par(f"ng{l}{j}"),
                        op0=ALU.add, op1=ALU.mult), reads=["MOD", "PAR"], writes=["DER"])
                    S.add("dve", lambda e, sh_=sh_, l=l, s=s, c0=c0: e.tensor_copy(
                        out=sh_, in_=MOD[:, l, c0:c0 + 8, s]), reads=["MOD"], writes=["DER"])
                    S.add("dve", lambda e, g_=g_, l=l, s=s, j=j, c0=c0: e.tensor_scalar(
                        out=g_, in0=MOD[:, l, c0 + 16:c0 + 24, s], scalar1=(1.0 if j == 1 else 0.5), scalar2=None,
                        op0=ALU.mult), reads=["MOD"], writes=["DER"])
        S.barrier()
        M.off = big0

        def normmod(l, s, j):
            rss = [rs, rstd]

            def stats(t):
                ts_ = slice(t * 512, (t + 1) * 512)
                bank = 6 + t % 2
                for c in range(8):
                    q_ = sq[c % 2]
                    S.add("act", lambda e, q_=q_, c=c, ts_=ts_: e.activation(out=q_, in_=XT[:, c, ts_], func=AF.Square),
                          reads=[("XT", c, t)], writes=[("sq", c % 2)])
                    S.add("pe", lambda e, q_=q_, c=c, bank=bank: e.matmul(PS(bank), onesb, q_, start=(c == 0), stop=(c == 7)),
                          reads=[("sq", c % 2), "onesb"], writes=[("ps", bank)])
                r_ = rss[t % 2]
                S.add("act", lambda e, r_=r_, bank=bank: e.activation(out=r_, in_=PS(bank), func=AF.Sqrt, scale=1.0 / D, bias=EPS),
                      writes=[("ps", bank), ("rs" if t % 2 == 0 else "rstd")])
                S.add("dve", lambda e, r_=r_: e.reciprocal(out=r_, in_=r_), reads=[("rs" if t % 2 == 0 else "rstd")], writes=[("rs" if t % 2 == 0 else "rstd")])

            def modulate(t):
                ts_ = slice(t * 512, (t + 1) * 512)
                r_ = rss[t % 2]
                for c in range(8):
                    tf = tmpf[c % 2]
                    S.add("dve", lambda e, tf=tf, c=c, ts_=ts_, r_=r_: e.scalar_tensor_tensor(
                        out=tf, in0=XT[:, c, ts_], scalar=DER[:, l, s, j, 0, c:c + 1], in1=r_,
                        op0=ALU.mult, op1=ALU.mult), reads=[("XT", c, t), ("rs" if t % 2 == 0 else "rstd"), "DER"], writes=[("tmpf", c % 2)])
                    S.add("act", lambda e, tf=tf, c=c, ts_=ts_: e.activation(
                        out=HT[:, c, ts_], in_=tf, func=AF.Identity, bias=DER[:, l, s, j, 1, c:c + 1], scale=1.0),
                        reads=[("tmpf", c % 2), "DER"], writes=[("HT", c, t)])

            stats(0)
            for t in range(4):
                if t + 1 < 4:
                    stats(t + 1)
                modulate(t)

        def ffn(l, s, which):
            j = 0 if which == 1 else 2
            normmod(l, s, j)
            m0 = M.off
            act = M.alloc([11, SEQ], BF16)
            wgu = [M.alloc([8, 256], BF16) for _ in range(3)]
            wdn = M.alloc([11, D], BF16)

            def load_gu(f):
                S.add("pool", lambda e, f=f: e.dma_start(
                    out=wgu[f % 3], in_=wgu_d[which][l, f].rearrange("p (k n) -> p k n", k=8)),
                    writes=[("wgu", f % 3)], dma=True)

            for f in range(2):
                load_gu(f)
            pi = 0
            for grp in range(2):
                for fl in range(11):
                    f = grp * 11 + fl
                    if f + 2 < NF:
                        load_gu(f + 2)
                    S.add("pool", lambda e, f=f, fl=fl: e.dma_start(out=wdn[:, fl, :], in_=wdn_d[which][l, f]),
                          writes=[("wdn", fl)], dma=True)
                    w = wgu[f % 3]
                    for t in range(4):
                        ts_ = slice(t * 512, (t + 1) * 512)
                        bg, bu = 2 * (pi % 2), 2 * (pi % 2) + 1
                        pi += 1
                        for gu, bk in ((0, bg), (1, bu)):
                            for k in range(8):
                                S.add("pe", lambda e, w=w, gu=gu, bk=bk, k=k, ts_=ts_: e.matmul(
                                    PS(bk), w[:, k, gu * 128:(gu + 1) * 128], HT[:, k, ts_],
                                    start=(k == 0), stop=(k == 7)),
                                    reads=[("wgu", f % 3), ("HT", k, t)], writes=[("ps", bk)])
                        sg = sgf[pi % 2]
                        S.add("act", lambda e, sg=sg, bg=bg: e.activation(out=sg, in_=PS(bg), func=AF.Silu),
                              writes=[("ps", bg), ("sgf", pi % 2)])
                        S.add("dve", lambda e, sg=sg, bu=bu, fl=fl, ts_=ts_: e.tensor_tensor(
                            out=act[:, fl, ts_], in0=sg, in1=PS(bu), op=ALU.mult),
                            reads=[("sgf", pi % 2)], writes=[("ps", bu), ("act", fl, t)])
                di = 0
                for dc in range(8):
                    for t in range(4):
                        ts_ = slice(t * 512, (t + 1) * 512)
                        bk = 4 + di % 2
                        di += 1
                        for fl in range(11):
                            S.add("pe", lambda e, fl=fl, dc=dc, bk=bk, ts_=ts_: e.matmul(
                                PS(bk), wdn[:, fl, dc * 128:(dc + 1) * 128], act[:, fl, ts_],
                                start=(fl == 0), stop=(fl == 10)),
                                reads=[("wdn", fl), ("act", fl, t)], writes=[("ps", bk)])
                        S.add("dve", lambda e, dc=dc, bk=bk, ts_=ts_: e.scalar_tensor_tensor(
                            out=XT[:, dc, ts_], in0=PS(bk), scalar=DER[:, l, s, j, 2, dc:dc + 1], in1=XT[:, dc, ts_],
                            op0=ALU.mult, op1=ALU.add), reads=["DER"], writes=[("ps", bk), ("XT", dc, t)])
            S.barrier()
            M.off = m0

        def load_x(s):
            m0 = M.off
            stg = [M.alloc([D], F32) for _ in range(2)]
            for i in range(16):
                sg_ = stg[i % 2]
                S.add("sp", lambda e, sg_=sg_, i=i: e.dma_start(out=sg_, in_=x_d[s, i * 128:(i + 1) * 128, :]),
                      writes=[("stg", i % 2)], dma=True)
                for h in range(2):
                    bk = 6 + (2 * i + h) % 2
                    for cc in range(4):
                        c = 4 * h + cc
                        S.add("pe", lambda e, sg_=sg_, c=c, cc=cc, bk=bk: e.transpose(
                            psum[:, bk, cc * 128:(cc + 1) * 128], sg_[:, c * 128:(c + 1) * 128], identf),
                            reads=[("stg", i % 2), "identf"], writes=[("ps", bk)])
                    eng = "act" if h == 0 else "dve"
                    dst = XT[:, 4 * h:4 * h + 4, i * 128:(i + 1) * 128]
                    src = psum[:, bk, :].rearrange("p (a b) -> p a b", a=4)
                    if eng == "act":
                        S.add("act", lambda e, dst=dst, src=src: e.copy(out=dst, in_=src),
                              writes=[("ps", bk)] + [("XT", 4 * h + cc, i // 4) for cc in range(4)])
                    else:
                        S.add("dve", lambda e, dst=dst, src=src: e.tensor_copy(out=dst, in_=src),
                              writes=[("ps", bk)] + [("XT", 4 * h + cc, i // 4) for cc in range(4)])
            S.barrier()
            M.off = m0

        def store_x(s, final):
            m0 = M.off
            stg = [M.alloc([D], F32) for _ in range(2)]
            xn = [M.alloc([8, 128], F32) for _ in range(2)]
            for t in range(4):
                ts_ = slice(t * 512, (t + 1) * 512)
                if final:
                    for c in range(8):
                        q_ = sq[c % 2]
                        S.add("act", lambda e, q_=q_, c=c, ts_=ts_: e.activation(out=q_, in_=XT[:, c, ts_], func=AF.Square),
                              reads=[("XT", c, t)], writes=[("sq", c % 2)])
                        S.add("pe", lambda e, q_=q_, c=c: e.matmul(PS(5), onesb, q_, start=(c == 0), stop=(c == 7)),
                              reads=[("sq", c % 2), "onesb"], writes=[("ps", 5)])
                    S.add("act", lambda e: e.activation(out=rs, in_=PS(5), func=AF.Sqrt, scale=1.0 / D, bias=EPS),
                          writes=[("ps", 5), "rs"])
                    S.add("dve", lambda e: e.reciprocal(out=rstd, in_=rs), reads=["rs"], writes=["rstd"])
                for ii in range(4):
                    i = 4 * t + ii
                    isl = slice(i * 128, (i + 1) * 128)
                    xb_ = xn[i % 2]
                    if final:
                        for c in range(8):
                            S.add("dve", lambda e, xb_=xb_, c=c, isl=isl, ii=ii: e.scalar_tensor_tensor(
                                out=xb_[:, c, :], in0=XT[:, c, isl], scalar=par("fin")[:, c:c + 1],
                                in1=rstd[:, ii * 128:(ii + 1) * 128], op0=ALU.mult, op1=ALU.mult),
                                reads=[("XT", c, t), "rstd", "PAR"], writes=[("xn", i % 2)])
                    sg_ = stg[i % 2]
                    for h in range(2):
                        bk = 6 + (2 * i + h) % 2
                        for cc in range(4):
                            c = 4 * h + cc
                            src = xb_[:, c, :] if final else XT[:, c, isl]
                            S.add("pe", lambda e, src=src, cc=cc, bk=bk: e.transpose(
                                psum[:, bk, cc * 128:(cc + 1) * 128], src, identf),
                                reads=[("xn", i % 2), ("XT", c, t), "identf"], writes=[("ps", bk)])
                        dst = sg_[:, h * 512:(h + 1) * 512]
                        if h == 0:
                            S.add("act", lambda e, dst=dst, bk=bk: e.copy(out=dst, in_=PS(bk)),
                                  writes=[("ps", bk), ("stg", i % 2, h)])
                        else:
                            S.add("dve", lambda e, dst=dst, bk=bk: e.tensor_copy(out=dst, in_=PS(bk)),
                                  writes=[("ps", bk), ("stg", i % 2, h)])
                    S.add("sp", lambda e, sg_=sg_, isl=isl: e.dma_start(out=y_d[s, isl, :], in_=sg_),
                          reads=[("stg", i % 2, 0), ("stg", i % 2, 1)], writes=[("y", s, i)], dma=True)
            S.barrier()
            M.off = m0


        NIT = 10

        def gnorm_tile(src, skey, nch, gn, dst, dkey, bank):
            for c in range(nch):
                q_ = sq[c % 2]
                S.add("act", lambda e, q_=q_, c=c: e.activation(out=q_, in_=src[:, c, :], func=AF.Square),
                      reads=[skey], writes=[("sq", c % 2)])
                S.add("pe", lambda e, q_=q_, c=c: e.matmul(PS(bank), onesb, q_, start=(c == 0), stop=(c == nch - 1)),
                      reads=[("sq", c % 2), "onesb"], writes=[("ps", bank)])
            S.add("act", lambda e: e.activation(out=rs, in_=PS(bank), func=AF.Sqrt, scale=1.0 / (128 * nch), bias=EPS),
                  writes=[("ps", bank), "rs"])
            S.add("dve", lambda e: e.reciprocal(out=rstd, in_=rs), reads=["rs"], writes=["rstd"])
            for c in range(nch):
                S.add("dve", lambda e, c=c: e.scalar_tensor_tensor(
                    out=dst[:, c, :], in0=src[:, c, :], scalar=gn[:, c:c + 1], in1=rstd, op0=ALU.mult, op1=ALU.mult),
                    reads=[skey, "rstd", "PAR"], writes=[dkey])

        def wout_part(l, s, mc, mkeyf, nch, wo, wokey):
            di = 0
            for dc in range(8):
                for t in range(4):
                    ts_ = slice(t * 512, (t + 1) * 512)
                    bk = 4 + di % 2
                    di += 1
                    for kc in range(nch):
                        S.add("pe", lambda e, kc=kc, dc=dc, bk=bk, ts_=ts_: e.matmul(
                            PS(bk), wo[:, kc, dc * 128:(dc + 1) * 128], mc[:, kc, ts_],
                            start=(kc == 0), stop=(kc == nch - 1)), reads=[wokey, mkeyf(t)], writes=[("ps", bk)])
                    S.add("dve", lambda e, dc=dc, bk=bk, ts_=ts_: e.scalar_tensor_tensor(
                        out=XT[:, dc, ts_], in0=PS(bk), scalar=DER[:, l, s, 1, 2, dc:dc + 1], in1=XT[:, dc, ts_],
                        op0=ALU.mult, op1=ALU.add), reads=["DER"], writes=[("ps", bk), ("XT", dc, t)])

        def conv_part(l, s):
            m0 = M.off
            wab = M.alloc([8, 512], BF16)
            ucv = M.alloc([2, 30 + SEQ], BF16)
            DWc = M.alloc([2, 31, 128], BF16)
            yacc = M.alloc([2, SEQ], F32)
            zs = M.alloc([2, SEQ], BF16)
            wpw = M.alloc([2, 256], BF16)
            wo = M.alloc([2, D], BF16)
            mc = DWc.rearrange("p a b c -> p (a b c)")[:, 0:2 * SEQ].rearrange("p (a b) -> p a b", a=2)
            ycf = M.alloc([2, 512], F32)
            mt = M.alloc([512], F32)
            msq = M.alloc([512], F32)
            d1 = [M.alloc([512], F32) for _ in range(2)]
            ybf = [M.alloc([512], BF16) for _ in range(2)]
            cw = par(f"cw{l}").rearrange("p (a b) -> p a b", a=2)
            S.add("pool", lambda e: e.dma_start(out=wab, in_=win_d[l][:, :, C_A:C_A + 512]), writes=["wab"], dma=True)
            S.add("pool", lambda e: e.dma_start(out=wpw, in_=wpw_d[l]), writes=["wpw"], dma=True)
            S.add("pool", lambda e: e.dma_start(out=wo, in_=wout_d[l][:, 6:8, :]), writes=["wo"], dma=True)
            S.add("dve", lambda e: e.memset(ucv[:, :, 0:30], 0.0), writes=["ucvpad"])
            pi = 0
            for cc in range(2):
                for t in range(4):
                    ts_ = slice(t * 512, (t + 1) * 512)
                    ba, bg = 2 * (pi % 2), 2 * (pi % 2) + 1
                    pi += 1
                    for (c0, bk) in ((cc * 128, ba), (256 + cc * 128, bg)):
                        for k in range(8):
                            S.add("pe", lambda e, c0=c0, bk=bk, k=k, ts_=ts_: e.matmul(
                                PS(bk), wab[:, k, c0:c0 + 128], HT[:, k, ts_], start=(k == 0), stop=(k == 7)),
                                reads=["wab", ("HT", k, t)], writes=[("ps", bk)])
                    sg = sgf[pi % 2]
                    S.add("act", lambda e, sg=sg, bg=bg: e.activation(out=sg, in_=PS(bg), func=AF.Sigmoid),
                          writes=[("ps", bg), ("sgf", pi % 2)])
                    S.add("dve", lambda e, sg=sg, ba=ba, cc=cc, t=t: e.tensor_tensor(
                        out=ucv[:, cc, 30 + t * 512:30 + (t + 1) * 512], in0=sg, in1=PS(ba), op=ALU.mult),
                        reads=[("sgf", pi % 2)], writes=[("ps", ba), ("ucv", cc)])
            for cc in range(2):
                for j in range(31):
                    S.add("pool", lambda e, cc=cc, j=j: e.tensor_scalar(out=DWc[:, cc, j, :], in0=identb, scalar1=cw[:, cc, j:j + 1],
                                                                        scalar2=None, op0=ALU.mult),
                          reads=["PAR", "identb"], writes=[("DWc", cc)])
            ci = 0
            for cc in range(2):
                for t in range(4):
                    bk = ci % 4
                    ci += 1
                    for j in range(31):
                        S.add("pe", lambda e, cc=cc, t=t, j=j, bk=bk: e.matmul(
                            PS(bk), DWc[:, cc, j, :], ucv[:, cc, j + t * 512:j + t * 512 + 512], start=(j == 0), stop=(j == 30)),
                            reads=[("DWc", cc), ("ucv", cc), "ucvpad"], writes=[("ps", bk)])
                    S.add("act", lambda e, cc=cc, t=t, bk=bk: e.activation(
                        out=yacc[:, cc, t * 512:(t + 1) * 512], in_=PS(bk), func=AF.Identity, bias=par(f"cb{l}")[:, cc:cc + 1], scale=1.0),
                        reads=["PAR"], writes=[("ps", bk), ("yacc", cc)])
            for t in range(4):
                ts_ = slice(t * 512, (t + 1) * 512)
                for cc in range(2):
                    S.add("act", lambda e, cc=cc, ts_=ts_: e.copy(out=ybf[cc], in_=yacc[:, cc, ts_]),
                          reads=[("yacc", cc)], writes=[("ybf", cc)])
                    S.add("pe", lambda e, cc=cc: e.matmul(PS(0), onesb, ybf[cc], start=(cc == 0), stop=(cc == 1)),
                          reads=[("ybf", cc), "onesb"], writes=[("ps", 0)])
                for cc in range(2):
                    S.add("act", lambda e, cc=cc, ts_=ts_: e.activation(out=sq[cc], in_=yacc[:, cc, ts_], func=AF.Square),
                          reads=[("yacc", cc)], writes=[("sq", cc)])
                    S.add("pe", lambda e, cc=cc: e.matmul(PS(1), onesb, sq[cc], start=(cc == 0), stop=(cc == 1)),
                          reads=[("sq", cc), "onesb"], writes=[("ps", 1)])
                S.add("dve", lambda e: e.tensor_scalar(out=mt, in0=PS(0), scalar1=1.0 / 256, scalar2=None, op0=ALU.mult),
                      writes=[("ps", 0), "mt"])
                S.add("dve", lambda e: e.tensor_tensor(out=msq, in0=mt, in1=mt, op=ALU.mult), reads=["mt"], writes=["msq"])
                S.add("dve", lambda e: e.scalar_tensor_tensor(out=msq, in0=PS(1), scalar=1.0 / 256, in1=msq,
                                                              op0=ALU.mult, op1=ALU.subtract),
                      reads=["msq"], writes=[("ps", 1), "msq"])
                S.add("act", lambda e: e.activation(out=rs, in_=msq, func=AF.Sqrt, scale=1.0, bias=EPS),
                      reads=["msq"], writes=["rs"])
                S.add("dve", lambda e: e.reciprocal(out=rstd, in_=rs), reads=["rs"], writes=["rstd"])
                for cc in range(2):
                    S.add("dve", lambda e, cc=cc, ts_=ts_: e.tensor_tensor(out=d1[cc], in0=yacc[:, cc, ts_], in1=mt, op=ALU.subtract),
                          reads=[("yacc", cc), "mt"], writes=[("d1", cc)])
                    S.add("dve", lambda e, cc=cc: e.tensor_tensor(out=d1[cc], in0=d1[cc], in1=rstd, op=ALU.mult),
                          reads=[("d1", cc), "rstd"], writes=[("d1", cc)])
                    S.add("act", lambda e, cc=cc, ts_=ts_: e.activation(
                        out=zs[:, cc, ts_], in_=d1[cc], func=AF.Silu, scale=par(f"clg{l}")[:, cc:cc + 1],
                        bias=par(f"clb{l}")[:, cc:cc + 1]), reads=[("d1", cc), "PAR"], writes=[("zs", t)])
            S.barrier()
            for t in range(4):
                ts_ = slice(t * 512, (t + 1) * 512)
                for oc in range(2):
                    for kc in range(2):
                        S.add("pe", lambda e, oc=oc, kc=kc, ts_=ts_: e.matmul(
                            PS(2 + oc), wpw[:, kc, oc * 128:(oc + 1) * 128], zs[:, kc, ts_], start=(kc == 0), stop=(kc == 1)),
                            reads=["wpw", ("zs", t)], writes=[("ps", 2 + oc)])
                    S.add("act", lambda e, oc=oc: e.copy(out=ycf[:, oc, :], in_=PS(2 + oc)), writes=[("ps", 2 + oc), "ycf"])
                gnorm_tile(ycf, "ycf", 2, par(f"cgn{l}"), mc[:, :, ts_], ("mc", t), 3)
            wout_part(l, s, mc, lambda t: ("mc", t), 2, wo, "wo")
            S.barrier()
            M.off = m0

        def att_part(l, s):
            m0 = M.off
            qT = M.alloc([4, SEQ], BF16)
            kkT = M.alloc([SEQ], BF16)
            kiT = M.alloc([SEQ], BF16)
            qiT = M.alloc([2, SEQ], BF16)
            V1 = M.alloc([16, 66], BF16)
            WI = M.alloc([16, 8], F32)
            wo = M.alloc([4, D], BF16)
            P2 = M.alloc([NIT + 1], F32)
            thr0 = M.alloc([1], F32)
            m1 = M.off
            wr = [M.alloc([8, 256], BF16) for _ in range(2)]
            S.add("pool", lambda e: e.dma_start(out=wo, in_=wout_d[l][:, 0:4, :]), writes=["wo"], dma=True)
            for i in range(NIT + 1):
                S.add("pool", lambda e, i=i: e.memset(P2[:, i:i + 1], 2.0 ** -(i + 1)), writes=["P2"])
            S.add("pool", lambda e: e.memset(thr0, -1e29), writes=["thr0"])
            S.add("pool", lambda e: e.memset(V1[:, :, 64:65], 1.0), writes=["V1one"])
            groups = [(C_Q, 256, [("q", 0), ("q", 1)]), (C_Q + 256, 256, [("q", 2), ("q", 3)]),
                      (C_KK, 256, [("kk", 0), ("ki", 0)]), (C_QI, 256, [("qi", 0), ("qi", 1)]), (C_VW, 72, None)]
            pi = 0
            for gi, (c0, n, dests) in enumerate(groups):
                w = wr[gi % 2]
                S.add("pool", lambda e, w=w, c0=c0, n=n: e.dma_start(out=w[:, :, 0:n], in_=win_d[l][:, :, c0:c0 + n]),
                      writes=[("wr", gi % 2)], dma=True)
                if dests is not None:
                    for ci, (kind, idx) in enumerate(dests):
                        for t in range(4):
                            ts_ = slice(t * 512, (t + 1) * 512)
                            bk = pi % 4
                            pi += 1
                            for k in range(8):
                                S.add("pe", lambda e, w=w, ci=ci, bk=bk, k=k, ts_=ts_: e.matmul(
                                    PS(bk), w[:, k, ci * 128:(ci + 1) * 128], HT[:, k, ts_], start=(k == 0), stop=(k == 7)),
                                    reads=[("wr", gi % 2), ("HT", k, t)], writes=[("ps", bk)])
                            dst = {"q": lambda: qT[:, idx, ts_], "kk": lambda: kkT[:, ts_], "ki": lambda: kiT[:, ts_],
                                   "qi": lambda: qiT[:, idx, ts_]}[kind]()
                            sc_ = 0.125 if kind == "q" else 1.0
                            if pi % 2 == 0:
                                S.add("act", lambda e, dst=dst, bk=bk, sc_=sc_: e.activation(
                                    out=dst, in_=PS(bk), func=AF.Copy, scale=sc_), writes=[("ps", bk), (kind, idx, t)])
                            else:
                                S.add("dve", lambda e, dst=dst, bk=bk, sc_=sc_: e.tensor_scalar(
                                    out=dst, in0=PS(bk), scalar1=sc_, scalar2=None, op0=ALU.mult),
                                    writes=[("ps", bk), (kind, idx, t)])
                else:
                    for i in range(16):
                        bk = pi % 4
                        pi += 1
                        for k in range(8):
                            S.add("pe", lambda e, w=w, bk=bk, k=k, i=i: e.matmul(
                                psum[:, bk, 0:72], HT[:, k, i * 128:(i + 1) * 128], w[:, k, 0:72], start=(k == 0), stop=(k == 7)),
                                reads=[("wr", gi % 2), ("HT", k, i // 4)], writes=[("ps", bk)])
                        S.add("dve", lambda e, bk=bk, i=i: e.tensor_copy(out=V1[:, i, 0:64], in_=psum[:, bk, 0:64]),
                              writes=[("ps", bk), "V1"])
                        S.add("act", lambda e, bk=bk, i=i: e.copy(out=WI[:, i, :], in_=psum[:, bk, 64:72]),
                              writes=[("ps", bk), "WI"])
            S.barrier()
            M.off = m1
            scs = [M.alloc([SEQ], F32) for _ in range(2)]
            rt = [M.alloc([512], BF16) for _ in range(3)]
            DW = [M.alloc([8, 128], BF16) for _ in range(2)]
            negm = [M.alloc([SEQ], BF16) for _ in range(2)]
            ident4 = M.alloc([4, 128], BF16)
            ex = [M.alloc([4, 128], BF16) for _ in range(4)]
            otok = M.alloc([8, 65], F32)
            on = sgf[0].rearrange("p (a b) -> p a b", a=8)
            onsq = tmpf[0]
            onb = rs.bitcast(BF16)[:, 0:512]
            attT = rstd.bitcast(BF16)[:, 0:512].rearrange("p (a b) -> p a b", a=4)
            sm = M.alloc([48], F32)
            mid, cnt, ge, mx, mn, w0, ssq, rq = [sm[:, i:i + 1] for i in range(8)]
            los = [sm[:, 8:9], sm[:, 9:10]]
            Wd = sm[:, 12:12 + NIT + 1]
            rden = M.alloc([8], F32)
            gatt = par(f"agn{l}")
            pb = [psum[:, bkk, :].bitcast(BF16) for bkk in range(8)]
            for i4 in range(4):
                S.add("pool", lambda e, i4=i4: e.tensor_copy(out=ident4[:, i4, :], in_=identb), reads=["identb"], writes=["ident4"])
            cntr = {"ri": 0}

            def stageA(b):
                S_ = 128 * (b + 1)
                qs = slice(128 * b, 128 * b + 128)
                nseg = (S_ + 511) // 512
                tq = b // 4
                nm = negm[b % 2]
                nmk = ("nm", b % 2)
                dw = DW[b % 2]
                sc = scs[b % 2]
                for h in range(8):
                    S.add("pool", lambda e, dw=dw, h=h: e.tensor_scalar(out=dw[:, h, :], in0=identb, scalar1=WI[:, b, h:h + 1],
                                                                       scalar2=None, op0=ALU.mult),
                          reads=["WI", "identb"], writes=[("DW", b % 2)])
                for sg_i in range(nseg):
                    c0, c1 = sg_i * 512, min(S_, sg_i * 512 + 512)
                    n = c1 - c0
                    accb = 2
                    prev = None
                    for h in range(9):
                        if h < 8:
                            ri = cntr["ri"]
                            cntr["ri"] += 1
                            pr = slice(32 * (h % 4), 32 * (h % 4) + 32)
                            bk = ri % 2
                            r_ = rt[ri % 3]
                            rk = ("rt", ri % 3)
                            S.add("pe", lambda e, pr=pr, h=h, bk=bk, c0=c0, c1=c1, n=n: e.matmul(
                                psum[:, bk, 0:n], qiT[pr, h // 4, qs], kiT[pr, c0:c1], start=True, stop=True,
                                tile_position=(32 * (h % 4), 0)),
                                reads=[("qi", h // 4, tq), ("ki", 0, sg_i)], writes=[("ps", bk)])
                            S.add("act", lambda e, r_=r_, bk=bk, n=n: e.activation(out=r_[:, 0:n], in_=psum[:, bk, 0:n], func=AF.Relu),
                                  writes=[("ps", bk), rk])
                        if prev is not None:
                            ph, pr_t, prk = prev
                            S.add("pe", lambda e, ph=ph, pr_t=pr_t, n=n: e.matmul(
                                psum[:, accb, 0:n], dw[:, ph, :], pr_t[:, 0:n], start=(ph == 0), stop=(ph == 7)),
                                reads=[("DW", b % 2), prk], writes=[("ps", accb)])
                        prev = (h, r_, rk) if h < 8 else None
                    S.add("act", lambda e, n=n, c0=c0, c1=c1: e.copy(out=sc[:, c0:c1], in_=psum[:, accb, 0:n]),
                          writes=[("ps", accb), ("sc", b % 2, sg_i)])

            def stageAd(b):
                S_ = 128 * (b + 1)
                nseg = (S_ + 511) // 512
                nm = negm[b % 2]
                nmk = ("nm", b % 2)
                sc = scs[b % 2]
                sck = [("sc", b % 2, i) for i in range(nseg)]
                lo = los[b % 2]
                lok = ("lo", b % 2)
                if b >= 2:
                    S.add("dve", lambda e: e.tensor_reduce(out=mx, in_=sc[:, 0:S_], axis=AX.X, op=ALU.max), reads=sck, writes=["mx"])
                    S.add("dve", lambda e: e.tensor_reduce(out=mn, in_=sc[:, 0:S_], axis=AX.X, op=ALU.min), reads=sck, writes=["mn"])
                S.add("dve", lambda e: e.memset(sc[0:64, S_ - 64:S_], -1e30), reads=sck, writes=sck)
                if b >= 2:
                    S.add("dve", lambda e: e.tensor_tensor(out=w0, in0=mx, in1=mn, op=ALU.subtract), reads=["mx", "mn"], writes=["w0"])
                    S.add("dve", lambda e: e.tensor_scalar(out=Wd, in0=P2, scalar1=w0, scalar2=None, op0=ALU.mult),
                          reads=["w0", "P2"], writes=["Wd"])
                    S.add("dve", lambda e: e.tensor_tensor(out=mid, in0=mn, in1=Wd[:, 0:1], op=ALU.add), reads=["mn", "Wd"], writes=["mid"])
                    for i in range(NIT):
                        S.add("dve", lambda e: e.tensor_scalar(
                            out=nm[:, 0:S_], in0=sc[:, 0:S_], scalar1=mid, scalar2=None, op0=ALU.is_ge, op1=ALU.add,
                            accum_out=cnt), reads=sck + ["mid"], writes=[nmk, "cnt"])
                        S.add("dve", lambda e, i=i: e.tensor_scalar(out=ge, in0=cnt, scalar1=255.5, scalar2=Wd[:, i:i + 1],
                                                                    op0=ALU.is_ge, op1=ALU.mult), reads=["cnt", "Wd"], writes=["ge"])
                        S.add("dve", lambda e, i=i: e.scalar_tensor_tensor(
                            out=mid, in0=mid, scalar=Wd[:, i + 1:i + 2], in1=ge, op0=ALU.subtract, op1=ALU.add),
                            reads=["ge", "Wd", "mid"], writes=["mid"])
                    S.add("dve", lambda e: e.tensor_tensor(out=lo, in0=mid, in1=Wd[:, NIT:NIT + 1], op=ALU.subtract),
                          reads=["mid", "Wd"], writes=[lok])
                    thr = lo
                else:
                    thr = thr0
                S.add("dve", lambda e: e.tensor_scalar(
                    out=nm[:, 0:S_], in0=sc[:, 0:S_], scalar1=thr, scalar2=-30000.0, op0=ALU.is_lt, op1=ALU.mult),
                    reads=sck + [lok, "thr0"], writes=[nmk])

            def stageB(b):
                qs = slice(128 * b, 128 * b + 128)
                tq = b // 4
                nm = negm[b % 2]
                nmk = ("nm", b % 2)
                i4f = ident4.rearrange("p a b -> p (a b)")
                for ch in range(b + 1):
                    ks = slice(ch * 128, (ch + 1) * 128)
                    for par_ in range(2):
                        S.add("pe", lambda e, par_=par_, ks=ks: e.matmul(psum[:, 3 + par_, :], nm[:, ks], i4f, start=True, stop=False,
                                                                         skip_group_check=True),
                              reads=[nmk, "ident4"], writes=[("ps", 3 + par_)])
                    for hh in range(4):
                        for par_ in range(2):
                            hp = slice(64 * par_, 64 * par_ + 64)
                            S.add("pe", lambda e, hh=hh, par_=par_, hp=hp, ks=ks: e.matmul(
                                psum[:, 3 + par_, hh * 128:(hh + 1) * 128], kkT[hp, ks], qT[hp, hh, qs], start=False, stop=(hh == 3),
                                skip_group_check=True),
                                reads=[("kk", 0, ch // 4), ("q", hh, tq)], writes=[("ps", 3 + par_)])
                    for par_ in range(2):
                        e_ = ex[2 * (ch % 2) + par_]
                        S.add("act", lambda e, e_=e_, par_=par_: e.activation(
                            out=e_, in_=psum[:, 3 + par_, :].rearrange("p (a b) -> p a b", a=4), func=AF.Exp),
                            writes=[("ps", 3 + par_), ("ex", 2 * (ch % 2) + par_)])
                    for h in range(8):
                        ob = 5 + h // 4
                        S.add("pe", lambda e, h=h, ob=ob, ch=ch: e.matmul(
                            psum[:, ob, (h % 4) * 65:(h % 4) * 65 + 65], ex[2 * (ch % 2) + h % 2][:, h // 2, :], V1[:, ch, 0:65],
                            start=(ch == 0 and h % 4 == 0), stop=(ch == b), skip_group_check=True),
                            reads=[("ex", 2 * (ch % 2) + h % 2), "V1", "V1one"], writes=[("ps", ob)])
                S.add("act", lambda e: e.copy(out=otok[:, 0:4, :], in_=psum[:, 5, 0:260].rearrange("p (a b) -> p a b", a=4)),
                      writes=[("ps", 5), "otokA"])
                S.add("act", lambda e: e.copy(out=otok[:, 4:8, :], in_=psum[:, 6, 0:260].rearrange("p (a b) -> p a b", a=4)),
                      writes=[("ps", 6), "otokB"])
                S.add("dve", lambda e: e.reciprocal(out=rden, in_=otok[:, :, 64]), reads=["otokA", "otokB"], writes=["rden"])
                S.add("dve", lambda e: e.tensor_tensor(out=on, in0=otok[:, :, 0:64], in1=rden.unsqueeze(2).broadcast_to([128, 8, 64]),
                                                       op=ALU.mult), reads=["otokA", "otokB", "rden"], writes=["on"])
                onf = on.rearrange("p a b -> p (a b)")
                S.add("dve", lambda e: e.tensor_tensor(out=onsq, in0=onf, in1=onf, op=ALU.mult), reads=["on"], writes=["onsq"])
                S.add("dve", lambda e: e.tensor_scalar(out=onsq, in0=onsq, scalar1=1.0, scalar2=None, op0=ALU.mult, op1=ALU.add,
                                                       accum_out=ssq), reads=["onsq"], writes=["onsq", "ssq"])
                S.add("act", lambda e: e.activation(out=rq, in_=ssq, func=AF.Sqrt, scale=1.0 / 512, bias=EPS), reads=["ssq"], writes=["rq"])
                S.add("dve", lambda e: e.reciprocal(out=rq, in_=rq), reads=["rq"], writes=["rq"])
                S.add("dve", lambda e: e.tensor_scalar(out=onb, in0=onf, scalar1=rq, scalar2=None, op0=ALU.mult),
                      reads=["on", "rq"], writes=["onb"])
                for c in range(4):
                    S.add("pe", lambda e, c=c: e.transpose(pb[7][:, c * 128:(c + 1) * 128], onb[:, c * 128:(c + 1) * 128], identb),
                          reads=["onb", "identb"], writes=[("ps", 7)])
                for c in range(4):
                    S.add("act", lambda e, c=c: e.activation(out=attT[:, c, :], in_=pb[7][:, c * 128:(c + 1) * 128], func=AF.Copy,
                                                             scale=gatt[:, c:c + 1]),
                          reads=["PAR"], writes=[("ps", 7), "attT"])
                for dc in range(8):
                    bk = 3 + dc // 4
                    for kc in range(4):
                        S.add("pe", lambda e, dc=dc, kc=kc, bk=bk: e.matmul(
                            psum[:, bk, (dc % 4) * 128:(dc % 4 + 1) * 128], wo[:, kc, dc * 128:(dc + 1) * 128], attT[:, kc, :],
                            start=(kc == 0), stop=(kc == 3)), reads=["wo", "attT"], writes=[("ps", bk)])
                for dc in range(8):
                    bk = 3 + dc // 4
                    S.add("dve", lambda e, dc=dc, bk=bk: e.scalar_tensor_tensor(
                        out=XT[:, dc, qs], in0=psum[:, bk, (dc % 4) * 128:(dc % 4 + 1) * 128],
                        scalar=DER[:, l, s, 1, 2, dc:dc + 1], in1=XT[:, dc, qs], op0=ALU.mult, op1=ALU.add),
                        reads=["DER"], writes=[("ps", bk), ("XT", dc, tq)])

            stageA(0)
            stageAd(0)
            stageA(1)
            for b in range(16):
                if b + 2 < 16:
                    stageA(b + 2)
                if b + 1 < 16:
                    stageAd(b + 1)
                stageB(b)
            S.barrier()
            M.off = m0


        def ssm_part(l, s):
            I32 = mybir.dt.int32
            m0 = M.off
            TWO_PI = float(2 * np.pi)
            PI = float(np.pi)
            wssm = M.alloc([8, 256], BF16)
            Abuf = M.alloc([2, 16, 8, 16], BF16)
            U = M.alloc([16, 256], BF16)
            Bq = M.alloc([16, 128], BF16)
            Rm = Bq
            E = M.alloc([16, 16, 16], BF16)
            Bs = M.alloc([16, 128], BF16)
            W = M.alloc([16, 128], BF16)
            wglu = M.alloc([2, 512], BF16)
            wo = M.alloc([2, D], BF16)
            zf = M.alloc([2, 512], F32)
            XR = M.alloc([16, 8], F32)
            YR = M.alloc([16, 8], F32)
            m1 = M.off
            S.add("pool", lambda e: e.dma_start(out=wssm, in_=win_d[l][:, :, C_SSM:C_SSM + 256]), writes=["wssm"], dma=True)
            S.add("pool", lambda e: e.dma_start(out=wglu, in_=wglu_d[l]), writes=["wglu"], dma=True)
            S.add("pool", lambda e: e.dma_start(out=wo, in_=wout_d[l][:, 4:6, :]), writes=["wo"], dma=True)
            NP = 32
            tbase = M.off
            T = {n: M.alloc([16, NP], F32) for n in ("A", "KF", "LT", "MAG", "FR", "FR2")}
            ki_ = M.alloc([16, NP], F32).bitcast(I32)
            sm_ = {n: M.alloc([16], F32) for n in ("dt", "th", "lam", "nr", "den", "t1", "t2", "qr", "qi")}
            big = {n: M.alloc([16, 16], F32) for n in ("Q0", "t1", "t2", "BP", "BQ", "CP", "CQ")}
            tb = {"b1": M.view(tbase, [8, 8, 16], BF16), "b2": M.view(tbase + 4096, [8, 8, 16], BF16)}
            te = {"e1": M.view(tbase, [8, 16, 16], BF16), "e2": M.view(tbase + 4096, [8, 16, 16], BF16)}
            pr_ = lambda n: par(f"s{n}{l}")
            jall = par("sjall")
            sgnA, sgnC = par("ssgnA"), par("ssgnC")

            def V(eng, fn, r, w):
                S.add(eng, fn, reads=r, writes=w)

            def b3(ap2, n):
                return ap2.unsqueeze(2).broadcast_to([128, 16, n])

            def fl(ap3):
                return ap3.rearrange("p a b -> p (a b)")

            V("act", lambda e: e.activation(out=sm_["dt"], in_=pr_("ldt"), func=AF.Exp), ["PAR"], ["dt"])
            V("dve", lambda e: e.tensor_tensor(out=sm_["th"], in0=sm_["dt"], in1=pr_("aim"), op=ALU.mult), ["dt", "PAR"], ["th"])
            V("dve", lambda e: e.tensor_tensor(out=sm_["lam"], in0=sm_["dt"], in1=pr_("are"), op=ALU.mult), ["dt", "PAR"], ["lam"])
            jb = jall.unsqueeze(1).broadcast_to([128, 16, NP])
            V("dve", lambda e: e.tensor_tensor(out=T["A"], in0=b3(sm_["th"], NP), in1=jb, op=ALU.mult), ["th", "PAR"], ["A"])
            V("dve", lambda e: e.tensor_tensor(out=T["MAG"], in0=b3(sm_["lam"], NP), in1=jb, op=ALU.mult), ["lam", "PAR"], ["MAG"])
            V("act", lambda e: e.activation(out=T["MAG"], in_=T["MAG"], func=AF.Exp), ["MAG"], ["MAG"])
            V("dve", lambda e: e.tensor_scalar(out=T["A"], in0=T["A"], scalar1=1.0 / TWO_PI, scalar2=64.0, op0=ALU.mult, op1=ALU.add), ["A"], ["A"])
            V("dve", lambda e: e.tensor_copy(out=ki_, in_=T["A"]), ["A"], ["ki"])
            V("dve", lambda e: e.tensor_copy(out=T["KF"], in_=ki_), ["ki"], ["KF"])
            V("dve", lambda e: e.tensor_tensor(out=T["FR"], in0=T["A"], in1=T["KF"], op=ALU.subtract), ["A", "KF"], ["FR"])
            V("dve", lambda e: e.tensor_scalar(out=T["LT"], in0=T["FR"], scalar1=0.0, scalar2=None, op0=ALU.is_lt), ["FR"], ["LT"])
            V("dve", lambda e: e.tensor_tensor(out=T["FR"], in0=T["FR"], in1=T["LT"], op=ALU.add), ["FR", "LT"], ["FR"])
            V("dve", lambda e: e.tensor_scalar(out=T["FR2"], in0=T["FR"], scalar1=0.25, scalar2=None, op0=ALU.add), ["FR"], ["FR2"])
            V("dve", lambda e: e.tensor_scalar(out=T["LT"], in0=T["FR2"], scalar1=1.0, scalar2=None, op0=ALU.is_ge), ["FR2"], ["LT"])
            V("dve", lambda e: e.tensor_tensor(out=T["FR2"], in0=T["FR2"], in1=T["LT"], op=ALU.subtract), ["FR2", "LT"], ["FR2"])
            for nm in ("FR", "FR2"):
                V("dve", lambda e, nm=nm: e.tensor_scalar(out=T[nm], in0=T[nm], scalar1=TWO_PI, scalar2=-PI, op0=ALU.mult, op1=ALU.add), [nm], [nm])
                V("dve", lambda e, nm=nm: e.tensor_scalar(out=T[nm], in0=T[nm], scalar1=-3.1415925, scalar2=3.1415925, op0=ALU.max, op1=ALU.min), [nm], [nm])
                V("act", lambda e, nm=nm: e.activation(out=T[nm], in_=T[nm], func=AF.Sin), [nm], [nm])
                V("dve", lambda e, nm=nm: e.scalar_tensor_tensor(out=fl(T[nm]), in0=fl(T[nm]), scalar=-1.0, in1=fl(T["MAG"]),
                                                                 op0=ALU.mult, op1=ALU.mult), [nm, "MAG"], [nm])
            XA, YA = T["FR2"], T["FR"]
            XAk, YAk = "FR2", "FR"
            X1, Y1 = XA[:, :, 8], YA[:, :, 8]
            V("dve", lambda e: e.tensor_scalar(out=sm_["nr"], in0=X1, scalar1=-1.0, scalar2=None, op0=ALU.add), [XAk], ["nr"])
            V("dve", lambda e: e.tensor_tensor(out=sm_["den"], in0=pr_("are"), in1=pr_("are"), op=ALU.mult), ["PAR"], ["den"])
            V("dve", lambda e: e.tensor_tensor(out=sm_["t1"], in0=pr_("aim"), in1=pr_("aim"), op=ALU.mult), ["PAR"], ["st1"])
            V("dve", lambda e: e.tensor_tensor(out=sm_["den"], in0=sm_["den"], in1=sm_["t1"], op=ALU.add), ["den", "st1"], ["den"])
            V("dve", lambda e: e.reciprocal(out=sm_["den"], in_=sm_["den"]), ["den"], ["den"])
            V("dve", lambda e: e.tensor_tensor(out=sm_["t1"], in0=sm_["nr"], in1=pr_("are"), op=ALU.mult), ["nr", "PAR"], ["st1"])
            V("dve", lambda e: e.tensor_tensor(out=sm_["t2"], in0=Y1, in1=pr_("aim"), op=ALU.mult), [YAk, "PAR"], ["st2"])
            V("dve", lambda e: e.tensor_tensor(out=sm_["t1"], in0=sm_["t1"], in1=sm_["t2"], op=ALU.add), ["st1", "st2"], ["st1"])
            V("dve", lambda e: e.tensor_tensor(out=sm_["qr"], in0=sm_["t1"], in1=sm_["den"], op=ALU.mult), ["st1", "den"], ["qr"])
            V("dve", lambda e: e.tensor_tensor(out=sm_["t1"], in0=Y1, in1=pr_("are"), op=ALU.mult), [YAk, "PAR"], ["st1"])
            V("dve", lambda e: e.tensor_tensor(out=sm_["t2"], in0=sm_["nr"], in1=pr_("aim"), op=ALU.mult), ["nr", "PAR"], ["st2"])
            V("dve", lambda e: e.tensor_tensor(out=sm_["t1"], in0=sm_["t1"], in1=sm_["t2"], op=ALU.subtract), ["st1", "st2"], ["st1"])
            V("dve", lambda e: e.tensor_tensor(out=sm_["qi"], in0=sm_["t1"], in1=sm_["den"], op=ALU.mult), ["st1", "den"], ["qi"])
            bp3 = pr_("bp").rearrange("p (a b) -> p a b", a=16)
            bq3 = pr_("bq").rearrange("p (a b) -> p a b", a=16)
            V("dve", lambda e: e.tensor_scalar(out=big["Q0"], in0=bq3, scalar1=sgnA, scalar2=None, op0=ALU.mult), ["PAR"], ["Q0"])
            V("dve", lambda e: e.tensor_tensor(out=big["t1"], in0=bp3, in1=b3(sm_["qr"], 16), op=ALU.mult), ["PAR", "qr"], ["bt1"])
            V("dve", lambda e: e.tensor_tensor(out=big["t2"], in0=big["Q0"], in1=b3(sm_["qi"], 16), op=ALU.mult), ["Q0", "qi"], ["bt2"])
            V("dve", lambda e: e.tensor_tensor(out=big["BP"], in0=big["t1"], in1=big["t2"], op=ALU.add), ["bt1", "bt2"], ["BP"])
            V("dve", lambda e: e.tensor_tensor(out=big["t1"], in0=big["Q0"], in1=b3(sm_["qr"], 16), op=ALU.mult), ["Q0", "qr"], ["bt1"])
            V("dve", lambda e: e.tensor_tensor(out=big["t2"], in0=bp3, in1=b3(sm_["qi"], 16), op=ALU.mult), ["PAR", "qi"], ["bt2"])
            V("dve", lambda e: e.tensor_tensor(out=big["BQ"], in0=big["t1"], in1=big["t2"], op=ALU.subtract), ["bt1", "bt2"], ["BQ"])
            Bq4 = Bq.rearrange("p g (a b) -> p g a b", a=8)
            for gh in range(2):
                gs = slice(8 * gh, 8 * gh + 8)
                xb_ = XA[:, gs, 16:24].unsqueeze(3).broadcast_to([128, 8, 8, 16])
                yb_ = YA[:, gs, 16:24].unsqueeze(3).broadcast_to([128, 8, 8, 16])
                bpg = big["BP"][:, gs, :].unsqueeze(2).broadcast_to([128, 8, 8, 16])
                bqg = big["BQ"][:, gs, :].unsqueeze(2).broadcast_to([128, 8, 8, 16])
                V("dve", lambda e, xb_=xb_, bpg=bpg: e.tensor_tensor(out=tb["b1"], in0=bpg, in1=xb_, op=ALU.mult), ["BP", XAk, "Bq", "MAG", "A", "KF", "LT"], ["b1"])
                V("dve", lambda e, yb_=yb_, bqg=bqg: e.tensor_tensor(out=tb["b2"], in0=bqg, in1=yb_, op=ALU.mult), ["BQ", YAk, "Bq", "MAG", "A", "KF", "LT"], ["b2"])
                V("dve", lambda e, gs=gs: e.tensor_tensor(out=Bq4[:, gs], in0=tb["b1"], in1=tb["b2"], op=ALU.add), ["b1", "b2"], ["Bq"])
            cp3 = pr_("cp").rearrange("p (a b) -> p a b", a=16)
            cq3 = pr_("cq").rearrange("p (a b) -> p a b", a=16)
            V("dve", lambda e: e.tensor_scalar(out=big["CP"], in0=cp3, scalar1=sgnC, scalar2=None, op0=ALU.mult), ["PAR"], ["CP"])
            V("dve", lambda e: e.tensor_scalar(out=big["CQ"], in0=cq3, scalar1=-1.0, scalar2=None, op0=ALU.mult), ["PAR"], ["CQ"])
            for gh in range(2):
                gs = slice(8 * gh, 8 * gh + 8)
                xe_ = XA[:, gs, 0:16].unsqueeze(3).broadcast_to([128, 8, 16, 16])
                ye_ = YA[:, gs, 0:16].unsqueeze(3).broadcast_to([128, 8, 16, 16])
                cpg = big["CP"][:, gs, :].unsqueeze(2).broadcast_to([128, 8, 16, 16])
                cqg = big["CQ"][:, gs, :].unsqueeze(2).broadcast_to([128, 8, 16, 16])
                V("dve", lambda e, xe_=xe_, cpg=cpg: e.tensor_tensor(out=te["e1"], in0=cpg, in1=xe_, op=ALU.mult), ["CP", XAk, "E", "b1", "b2", "Bq"], ["e1"])
                V("dve", lambda e, ye_=ye_, cqg=cqg: e.tensor_tensor(out=te["e2"], in0=cqg, in1=ye_, op=ALU.mult), ["CQ", YAk, "E", "b1", "b2", "Bq"], ["e2"])
                V("dve", lambda e, gs=gs: e.tensor_tensor(out=E[:, gs], in0=te["e1"], in1=te["e2"], op=ALU.add), ["e1", "e2"], ["E"])
            V("dve", lambda e: e.tensor_copy(out=XR, in_=XA[:, :, 24:32]), [XAk], ["XR"])
            V("dve", lambda e: e.tensor_scalar(out=YR, in0=YA[:, :, 24:32], scalar1=sgnC, scalar2=None, op0=ALU.mult), [YAk, "PAR"], ["YR"])
            pb = [psum[:, bkk, :].bitcast(BF16) for bkk in range(8)]
            for g in range(16):
                bk = g // 8
                V("pe", lambda e, g=g, bk=bk: e.transpose(pb[bk][:, (g % 8) * 128:(g % 8 + 1) * 128], Bq[:, g, :], identb), ["Bq", "identb"], [("ps", bk)])
            for bk in range(2):
                V("act", lambda e, bk=bk: e.copy(out=Bs[:, 8 * bk:8 * bk + 8, :], in_=pb[bk].rearrange("p (a b) -> p a b", a=8)), [], [("ps", bk), "Bs"])
            tmk = par("stm")
            for g in range(16):
                bk = 2 + g // 4
                V("pe", lambda e, g=g, bk=bk: e.matmul(psum[:, bk, (g % 4) * 128:(g % 4 + 1) * 128], Bq[:, g, :],
                                                        E[:, g, 0:8, :].rearrange("p a b -> p (a b)"), start=True, stop=True), ["Bq", "E"], [("ps", bk)])
            for q4 in range(4):
                bk = 2 + q4
                V("dve", lambda e, q4=q4, bk=bk: e.tensor_tensor(
                    out=W[:, 4 * q4:4 * q4 + 4, :], in0=psum[:, bk, :].rearrange("p (a b) -> p a b", a=4),
                    in1=tmk.unsqueeze(1).broadcast_to([128, 4, 128]), op=ALU.mult), ["PAR"], [("ps", bk), "W"])
            for g in range(16):
                V("dve", lambda e, g=g: e.scalar_tensor_tensor(out=W[:, g, :], in0=identf, scalar=pr_("dcol")[:, g:g + 1], in1=W[:, g, :],
                                                              op0=ALU.mult, op1=ALU.add), ["W", "PAR", "identf"], ["W"])
            S.barrier()
            M.off = m1
            X32 = M.alloc([16, 257], F32)
            Xb = M.alloc([16, 257], BF16)
            for half in range(2):
                for sp_ in range(4):
                    bk = 4 * (half % 2) + sp_
                    for sg2 in range(2):
                        sig = 2 * sp_ + sg2
                        for k in range(8):
                            V("pe", lambda e, half=half, sig=sig, sg2=sg2, bk=bk, k=k: e.matmul(
                                psum[:, bk, sg2 * 256:(sg2 + 1) * 256], HT[:, k, 1024 * half + sig:1024 * (half + 1):8], wssm[:, k, :],
                                start=(k == 0), stop=(k == 7)), ["wssm", ("HT", k, 2 * half), ("HT", k, 2 * half + 1)], [("ps", bk)])
                    for sg2 in range(2):
                        sig = 2 * sp_ + sg2
                        src = psum[:, bk, sg2 * 256:(sg2 + 1) * 256].rearrange("p (a b) -> p a b", a=16)
                        dst = Abuf[:, half, :, sig, :]
                        if sg2 == 0:
                            V("act", lambda e, src=src, dst=dst: e.copy(out=dst, in_=src), [], [("ps", bk), ("Abuf", half)])
                        else:
                            V("dve", lambda e, src=src, dst=dst: e.tensor_copy(out=dst, in_=src), [], [("ps", bk), ("Abuf", half)])
            for half in range(2):
                for gh in range(2):
                    bk = 2 * half + gh
                    for gg in range(8):
                        g = 8 * gh + gg
                        V("pe", lambda e, half=half, g=g, gg=gg, bk=bk: e.transpose(
                            pb[bk][:, gg * 128:(gg + 1) * 128], Abuf[:, half, g].rearrange("p a b -> p (a b)"), identb),
                            [("Abuf", half), "identb"], [("ps", bk)])
                    dst = U[:, 8 * gh:8 * gh + 8, 128 * half:128 * (half + 1)]
                    src = pb[bk].rearrange("p (a b) -> p a b", a=8)
                    if gh == 0:
                        V("act", lambda e, src=src, dst=dst: e.copy(out=dst, in_=src), [], [("ps", bk), "U"])
                    else:
                        V("dve", lambda e, src=src, dst=dst: e.tensor_copy(out=dst, in_=src), [], [("ps", bk), "U"])
            V("dve", lambda e: e.memset(X32[:, :, 0:1], 0.0), [], ["X32z"])
            for g in range(16):
                bk = g // 2
                V("pe", lambda e, g=g, bk=bk: e.matmul(psum[:, bk, (g % 2) * 256:(g % 2 + 1) * 256], Bs[:, g, :], U[:, g, :], start=True, stop=True),
                  ["Bs", "U"], [("ps", bk)])
            for g2 in range(8):
                dst = X32[:, 2 * g2:2 * g2 + 2, 1:257]
                src = psum[:, g2, :].rearrange("p (a b) -> p a b", a=2)
                if g2 % 2 == 0:
                    V("act", lambda e, src=src, dst=dst: e.copy(out=dst, in_=src), [], [("ps", g2), "X32"])
                else:
                    V("dve", lambda e, src=src, dst=dst: e.tensor_copy(out=dst, in_=src), [], [("ps", g2), "X32"])
            V("act", lambda e: e.copy(out=Xb, in_=X32), ["X32", "X32z"], ["Xb"])
            swp = par("sswap")
            for lv in range(8):
                sh = 1 << lv
                n = 256 - sh
                for ph in range(2):
                    for chh in range(2):
                        pp = slice(64 * ph, 64 * ph + 64)
                        cs = slice(64 * chh, 64 * chh + 64)
                        src = identf if ph == chh else swp
                        coef = XR if ph == chh else YR
                        V("dve", lambda e, pp=pp, cs=cs, src=src, coef=coef, lv=lv: e.tensor_tensor(
                            out=Rm[pp, :, cs], in0=src[pp, cs].unsqueeze(1).broadcast_to([64, 16, 64]),
                            in1=coef[pp, :, lv:lv + 1].broadcast_to([64, 16, 64]), op=ALU.mult),
                            ["XR", "YR", "identf", "PAR"], ["Rm"])
                for g in range(16):
                    bk = g // 2
                    V("pe", lambda e, g=g, bk=bk, n=n: e.matmul(psum[:, bk, (g % 2) * 256:(g % 2) * 256 + n], Rm[:, g, :], Xb[:, g, 1:1 + n],
                                                                 start=True, stop=True), ["Rm", "Xb"], [("ps", bk)])
                for g2 in range(8):
                    V("dve", lambda e, g2=g2, n=n, sh=sh: e.tensor_tensor(
                        out=X32[:, 2 * g2:2 * g2 + 2, 1 + sh:257], in0=X32[:, 2 * g2:2 * g2 + 2, 1 + sh:257],
                        in1=psum[:, g2, :].rearrange("p (a b) -> p a b", a=2)[:, :, 0:n], op=ALU.add), ["X32"], [("ps", g2), "X32"])
                V("act", lambda e: e.copy(out=Xb, in_=X32), ["X32", "X32z"], ["Xb"])
            S.barrier()
            Ytok = Abuf.rearrange("p h g a b -> p h (g a b)").rearrange("p h (t c) -> p h t c", t=8)
            for half in range(2):
                for q4 in range(4):
                    bk = 4 * (half % 2) + q4
                    for gg in range(4):
                        g = 4 * q4 + gg
                        V("pe", lambda e, half=half, g=g, gg=gg, bk=bk: e.matmul(
                            psum[:, bk, gg * 128:(gg + 1) * 128], Xb[:, g, 128 * half:128 * half + 128],
                            E[:, g, 8:16, :].rearrange("p a b -> p (a b)"), start=True, stop=False), ["Xb", "E"], [("ps", bk)])
                        V("pe", lambda e, half=half, g=g, gg=gg, bk=bk: e.matmul(
                            psum[:, bk, gg * 128:(gg + 1) * 128], U[:, g, 128 * half:128 * half + 128], W[:, g, :],
                            start=False, stop=True), ["U", "W"], [("ps", bk)])
                    src = psum[:, bk, :].rearrange("p (g t c) -> p g t c", g=4, t=8)
                    dst = Ytok[:, half, :, 64 * q4:64 * q4 + 64].rearrange("p t (g c) -> p g t c", g=4)
                    V("act", lambda e, src=src, dst=dst: e.activation(out=dst, in_=src, func=AF.Gelu), [], [("ps", bk), ("Ytok", half)])
            S.barrier()
            M.off = m1
            YT = M.alloc([2, SEQ], BF16)
            for half in range(2):
                for cc in range(2):
                    bk = 2 * half + cc
                    for tau in range(8):
                        V("pe", lambda e, half=half, cc=cc, tau=tau, bk=bk: e.transpose(
                            pb[bk][:, tau * 128:(tau + 1) * 128], Ytok[:, half, tau, cc * 128:(cc + 1) * 128], identb),
                            [("Ytok", half), "identb"], [("ps", bk)])
                    dst = YT[:, cc, 1024 * half:1024 * (half + 1)].rearrange("p (k t) -> p t k", t=8)
                    src = pb[bk].rearrange("p (t k) -> p t k", t=8)
                    if cc == 0:
                        V("act", lambda e, src=src, dst=dst: e.copy(out=dst, in_=src), [], [("ps", bk), ("YT", half)])
                    else:
                        V("dve", lambda e, src=src, dst=dst: e.tensor_copy(out=dst, in_=src), [], [("ps", bk), ("YT", half)])
            ms = U.rearrange("p a b -> p (a b)").rearrange("p (c n) -> p c n", c=2)
            S.barrier()
            for t in range(4):
                ts_ = slice(t * 512, (t + 1) * 512)
                for oc in range(4):
                    for kc in range(2):
                        V("pe", lambda e, oc=oc, kc=kc, ts_=ts_: e.matmul(PS(oc), wglu[:, kc, oc * 128:(oc + 1) * 128], YT[:, kc, ts_],
                                                                         start=(kc == 0), stop=(kc == 1)), ["wglu", ("YT", t // 2)], [("ps", oc)])
                for cc in range(2):
                    sg = sgf[cc]
                    V("act", lambda e, sg=sg, cc=cc: e.activation(out=sg, in_=PS(2 + cc), func=AF.Sigmoid), [], [("ps", 2 + cc), ("sgf", cc)])
                    V("dve", lambda e, sg=sg, cc=cc: e.tensor_tensor(out=zf[:, cc, :], in0=sg, in1=PS(cc), op=ALU.mult),
                      [("sgf", cc)], [("ps", cc), "zf"])
                gnorm_tile(zf, "zf", 2, par(f"sgn{l}"), ms[:, :, ts_], ("ms", t), 6)
            wout_part(l, s, ms, lambda t: ("ms", t), 2, wo, "wo")
            S.barrier()
            M.off = m0

        def mixer(l, s):
            normmod(l, s, 1)
            if "conv" in MIXPARTS:
                conv_part(l, s)
            if "ssm" in MIXPARTS:
                ssm_part(l, s)
            if "att" in MIXPARTS:
                att_part(l, s)

        for s in range(2):
            load_x(s)
            for (l, kind) in phases:
                if kind == "ffn1":
                    ffn(l, s, 1)
                elif kind == "ffn2":
                    ffn(l, s, 2)
                else:
                    mixer(l, s)
            store_x(s, last)
        S.finish()
        nsem = S.emit(st)
        print(f"[build] ops={len(S.ops)} sems={nsem}")
    return nc


MIXPARTS = {"conv", "att", "ssm"}


ALL_PHASES = [(l, k) for l in range(2) for k in ("ffn1", "mix", "ffn2")]


def prep_weights(inp):
    w = {}
    for j, nm in ((1, "ffn1"), (2, "ffn2")):
        gu = inp[f"{nm}_w_gu"]
        g = gu[:, :, :DFF].reshape(2, 8, 128, NF, 128)
        u = gu[:, :, DFF:].reshape(2, 8, 128, NF, 128)
        cat = np.concatenate([g, u], axis=-1)
        w[f"wgu{j}"] = np.ascontiguousarray(cat.transpose(0, 3, 2, 1, 4)).reshape(2, NF, 128, 2048)
        w[f"wdn{j}"] = np.ascontiguousarray(inp[f"{nm}_w_down"]).reshape(2, NF, 128, D)
    w["mod_w"] = np.ascontiguousarray(inp["mod_w"])
    for l in range(2):
        wi = inp["w_in"][l]
        q, k, v, qi, ki, wv, ssm, ca, cg = (wi[:, 0:512], wi[:, 512:576], wi[:, 576:640], wi[:, 640:896], wi[:, 896:928],
                                            wi[:, 928:936], wi[:, 936:1192], wi[:, 1192:1448], wi[:, 1448:1704])
        cat = np.concatenate([q, k, k, ki, ki, ki, ki, qi, ca, cg, v, wv, ssm], axis=1)
        assert cat.shape[1] == NWIN
        w[f"win{l}"] = np.ascontiguousarray(cat.reshape(8, 128, NWIN).transpose(1, 0, 2))
        w[f"wout{l}"] = np.ascontiguousarray(inp["w_out"][l].reshape(8, 128, D).transpose(1, 0, 2))
        w[f"wpw{l}"] = np.ascontiguousarray(inp["conv_w_pw"][l].reshape(2, 128, 256).transpose(1, 0, 2))
        w[f"wglu{l}"] = np.ascontiguousarray(inp["ssm_w_glu"][l].reshape(2, 128, 512).transpose(1, 0, 2))
    return w


def run_phases(inp, xcur, phases, first, last, shared=None):
    shared = shared if shared is not None else prep_weights(inp)
    packs = [pack_small(inp, c) for c in range(8)]
    nc = build(phases, first, last, packs[0].off, packs[0].n)
    in_maps = []
    need = {"mod_w"}
    for l, k in phases:
        if k == "ffn1":
            need |= {"wgu1", "wdn1"}
        elif k == "ffn2":
            need |= {"wgu2", "wdn2"}
        else:
            need |= {f"win{l}", f"wout{l}", f"wpw{l}", f"wglu{l}"}
    shared = {k: v for k, v in shared.items() if k in need}
    for c in range(8):
        m = dict(shared)
        m["x"] = np.ascontiguousarray(xcur[2 * c:2 * c + 2])
        m["par"] = packs[c].array()
        in_maps.append(m)
    res = run_bass_kernel_spmd(nc, in_maps, core_ids=list(range(8)))
    return np.concatenate([r["y"] for r in res.results], axis=0)


def kernel(**inputs):
    inp = {k: np.asarray(v) for k, v in inputs.items()}
    x = np.ascontiguousarray(inp["x"], dtype=np.float32)
    return run_phases(inp, x, ALL_PHASES, True, True)
```
